# Optimizing a Trainium2 kernel written in Bass

```python
import math
import jax, jax.numpy as jnp
from jax import lax
import numpy as np

D_MODEL = 2048
BATCH = 1
SEQ = 8192
DEPTH = 4

GRID_W = 64
CTX_LEN = 256
N_MIXERS = 3
RMS_EPS = 1e-6
CONV_WIDTH = 3
DIFF_HEAD_DIM = 128
DIFF_HEADS = D_MODEL // (2 * DIFF_HEAD_DIM)
DIFF_SUBLN_EPS = 1e-5
DIFF_LAMBDA_A = 0.8
DIFF_LAMBDA_B = 0.6
DIFF_LAMBDA_C = 0.3
ROPE_THETA = 10000.0
QUERY_BLOCK = 128
GLA_HEADS = 4
GLA_DK = D_MODEL // (2 * GLA_HEADS)
GLA_DV = D_MODEL // GLA_HEADS
GLA_GATE_RANK = 16
GLA_TAU = 16.0
GLA_CHUNK = 64
N_EXPERTS = 16
EXPERT_FF = (3 * D_MODEL) // 4
CAPACITY_FACTOR = 2

kernel_name = 'hybrid_dit_shortconv_diffattn_gla_ecmoe'


def _n_layers_of(kind):
    return sum(1 for i in range(DEPTH) if i % N_MIXERS == kind)


def _rmsnorm(x, g, eps=RMS_EPS):
    xf = x.astype(jnp.float32)
    y = xf * lax.rsqrt(jnp.mean(xf * xf, axis=-1, keepdims=True) + eps)
    return (y * g.astype(jnp.float32)).astype(x.dtype)


def _modulation(cvec, w, b):
    return jnp.split(jax.nn.silu(cvec) @ w + b, 6, axis=-1)


def _modulate(h, shift, scale):
    return h * (1 + scale) + shift


def _short_conv(h, w_in, w_dw, w_out):
    L = h.shape[1]
    b_gate, c_gate, u = jnp.split(h @ w_in, 3, axis=-1)
    pad = CONV_WIDTH // 2
    v = jnp.pad(c_gate * u, ((0, 0), (pad, pad), (0, 0)))
    conv = v[:, 0:L] * w_dw[0]
    for k in range(1, CONV_WIDTH):
        conv = conv + v[:, k:k + L] * w_dw[k]
    return (b_gate * conv) @ w_out


def _rope_half(x, pos):
    half = x.shape[-1] // 2
    inv = ROPE_THETA ** (-jnp.arange(half, dtype=jnp.float32) / half)
    ang = pos.astype(jnp.float32)[:, None] * inv
    shape = (1, ang.shape[0]) + (1,) * (x.ndim - 3) + (half,)
    cos = jnp.cos(ang).reshape(shape)
    sin = jnp.sin(ang).reshape(shape)
    x1 = x[..., :half].astype(jnp.float32)
    x2 = x[..., half:].astype(jnp.float32)
    return jnp.concatenate([x1 * cos - x2 * sin, x2 * cos + x1 * sin], axis=-1).astype(x.dtype)


def _rope_2d(x, row, col):
    r = x.shape[-1] // 2
    return jnp.concatenate([_rope_half(x[..., :r], row), _rope_half(x[..., r:], col)], axis=-1)


def _diff_softmax_mix(q, keys, vals, lam):
    s = jnp.einsum('bqhtd,bkhtd->bhtqk', q, keys).astype(jnp.float32) * (DIFF_HEAD_DIM ** -0.5)
    p = jax.nn.softmax(s, axis=-1)
    w = p[:, :, 0] - lam * p[:, :, 1]
    return jnp.einsum('bhqk,bkhe->bqhe', w, vals.astype(jnp.float32))


def _diff_attention(h_lat, h_ctx, row, col, w_qkv, lam_params, subln, w_out, lambda_init, with_ctx_out):
    def project(h):
        B, L, _ = h.shape
        q, k, v = jnp.split(h @ w_qkv, 3, axis=-1)
        return (q.reshape(B, L, DIFF_HEADS, 2, DIFF_HEAD_DIM),
                k.reshape(B, L, DIFF_HEADS, 2, DIFF_HEAD_DIM),
                v.reshape(B, L, DIFF_HEADS, 2 * DIFF_HEAD_DIM))

    ql, kl, vl = project(h_lat)
    ql = _rope_2d(ql, row, col)
    kl = _rope_2d(kl, row, col)
    qc, kc, vc = project(h_ctx)
    lf = lam_params.astype(jnp.float32)
    lam = jnp.exp(jnp.sum(lf[0] * lf[1])) - jnp.exp(jnp.sum(lf[2] * lf[3])) + lambda_init
    keys = jnp.concatenate([kc, kl], axis=1)
    vals = jnp.concatenate([vc, vl], axis=1)
    B, L = ql.shape[0], ql.shape[1]
    nb = L // QUERY_BLOCK
    qb = jnp.moveaxis(ql.reshape(B, nb, QUERY_BLOCK, DIFF_HEADS, 2, DIFF_HEAD_DIM), 1, 0)
    ol = lax.map(lambda q: _diff_softmax_mix(q, keys, vals, lam), qb)
    ol = jnp.moveaxis(ol, 0, 1).reshape(B, L, DIFF_HEADS, 2 * DIFF_HEAD_DIM)

    def finish(o):
        o = _rmsnorm(o, subln, DIFF_SUBLN_EPS) * (1.0 - lambda_init)
        return o.reshape(o.shape[0], o.shape[1], -1).astype(h_lat.dtype) @ w_out

    y_lat = finish(ol)
    y_ctx = finish(_diff_softmax_mix(qc, kc, vc, lam)) if with_ctx_out else None
    return y_lat, y_ctx


def _gla_log_gate(h, w1, w2, b):
    B, L, _ = h.shape
    z = ((h @ w1) @ w2 + b).astype(jnp.float32)
    return (jax.nn.log_sigmoid(z) / GLA_TAU).reshape(B, L, GLA_HEADS, GLA_DK)


def _gla_chunk_scan(q, k, v, log_a, s0):
    B, L, H, dk = q.shape
    dv = v.shape[-1]
    n = L // GLA_CHUNK

    def chunks(t):
        return jnp.moveaxis(t.reshape(B, n, GLA_CHUNK, H, t.shape[-1]), 1, 0)

    causal = jnp.tril(jnp.ones((GLA_CHUNK, GLA_CHUNK), dtype=bool))[None, :, :, None, None]

    def step(S, inp):
        qc, kc, vc, gc = inp
        b = jnp.cumsum(gc, axis=1)
        rel = jnp.where(causal, b[:, :, None] - b[:, None, :], -jnp.inf)
        a = jnp.sum(qc[:, :, None] * kc[:, None, :] * jnp.exp(rel), axis=-1)
        o = (jnp.einsum('bijh,bjhe->bihe', a, vc)
             + jnp.einsum('bihd,bhde->bihe', qc * jnp.exp(b), S))
        b_end = b[:, -1]
        S = (jnp.exp(b_end)[..., None] * S
             + jnp.einsum('bjhd,bjhe->bhde', kc * jnp.exp(b_end[:, None] - b), vc))
        return S, o

    s_fin, o = lax.scan(step, s0, (chunks(q), chunks(k), chunks(v), chunks(log_a)))
    return jnp.moveaxis(o, 0, 1).reshape(B, L, H, dv), s_fin


def _gla(h_lat, h_ctx, w_in, gate_w1, gate_w2, gate_b, onorm, w_out, with_ctx_out):
    splits = [GLA_HEADS * GLA_DK, 2 * GLA_HEADS * GLA_DK, 2 * GLA_HEADS * GLA_DK + GLA_HEADS * GLA_DV]

    def project(h):
        B, L, _ = h.shape
        q, k, v, g = jnp.split(h @ w_in, splits, axis=-1)
        q = q.reshape(B, L, GLA_HEADS, GLA_DK).astype(jnp.float32) * (GLA_DK ** -0.5)
        k = k.reshape(B, L, GLA_HEADS, GLA_DK).astype(jnp.float32)
        v = v.reshape(B, L, GLA_HEADS, GLA_DV).astype(jnp.float32)
        la_f = _gla_log_gate(h, gate_w1[0], gate_w2[0], gate_b[0])
        la_b = _gla_log_gate(h, gate_w1[1], gate_w2[1], gate_b[1])
        return q, k, v, g, la_f, la_b

    def rev(t):
        return t[:, ::-1]

    qc, kc, vc, gc, lcf, lcb = project(h_ctx)
    ql, kl, vl, gl, llf, llb = project(h_lat)
    s0 = jnp.zeros((h_lat.shape[0], GLA_HEADS, GLA_DK, GLA_DV), jnp.float32)
    oc_f, s_cf = _gla_chunk_scan(qc, kc, vc, lcf, s0)
    oc_b, s_cb = _gla_chunk_scan(rev(qc), rev(kc), rev(vc), rev(lcb), s0)
    ol_f, _ = _gla_chunk_scan(ql, kl, vl, llf, s_cf)
    ol_b, _ = _gla_chunk_scan(rev(ql), rev(kl), rev(vl), rev(llb), s_cb)

    def finish(o, g):
        B, L = o.shape[0], o.shape[1]
        o = _rmsnorm(o, onorm) * jax.nn.silu(g.astype(jnp.float32)).reshape(B, L, GLA_HEADS, GLA_DV)
        return o.reshape(B, L, -1).astype(h_lat.dtype) @ w_out

    y_lat = finish(ol_f + rev(ol_b), gl)
    y_ctx = finish(oc_f + rev(oc_b), gc) if with_ctx_out else None
    return y_lat, y_ctx


def _ec_moe(h, router_w, w_gate, w_up, w_down):
    B, N, D = h.shape
    cap = max(1, (CAPACITY_FACTOR * N) // N_EXPERTS)
    affinity = jax.nn.softmax((h @ router_w).astype(jnp.float32), axis=-1)
    gate, idx = lax.top_k(jnp.swapaxes(affinity, 1, 2), cap)
    xin = jax.vmap(lambda hb, ib: hb[ib])(h, idx)
    a = jnp.einsum('becd,edf->becf', xin, w_gate)
    u = jnp.einsum('becd,edf->becf', xin, w_up)
    y = jnp.einsum('becf,efd->becd', jax.nn.silu(a) * u, w_down) * gate[..., None].astype(h.dtype)

    def scatter(yb, ib):
        return jnp.zeros((N, D), h.dtype).at[ib.reshape(-1)].add(yb.reshape(-1, D))

    return jax.vmap(scatter)(y, idx)


def setup_inputs(seed: int = 0) -> dict:
    key = jax.random.key(seed)
    ks = iter(jax.random.split(key, 40))

    def nrm(shape, scale):
        return jax.random.normal(next(ks), shape, jnp.float32) * scale

    D = D_MODEL
    nA, nB, nC = _n_layers_of(0), _n_layers_of(1), _n_layers_of(2)
    return {
        'x': nrm((BATCH, SEQ, D), 1.0),
        'c': nrm((BATCH, D), 1.0),
        'ctx': nrm((BATCH, CTX_LEN, D), 1.0),
        'c_ctx': nrm((D,), 1.0),
        'mod_w': nrm((DEPTH, D, 6 * D), 0.5 * D ** -0.5),
        'mod_b': nrm((DEPTH, 6 * D), 0.02),
        'norm_mix': 1.0 + nrm((DEPTH, D), 0.02),
        'norm_ffn': 1.0 + nrm((DEPTH, D), 0.02),
        'conv_w_in': nrm((nA, D, 3 * D), D ** -0.5),
        'conv_w_dw': nrm((nA, CONV_WIDTH, D), CONV_WIDTH ** -0.5),
        'conv_w_out': nrm((nA, D, D), D ** -0.5),
        'diff_w_qkv': nrm((nB, D, 3 * D), D ** -0.5),
        'diff_lambda': nrm((nB, 4, DIFF_HEAD_DIM), 0.1),
        'diff_subln': 1.0 + nrm((nB, 2 * DIFF_HEAD_DIM), 0.02),
        'diff_w_out': nrm((nB, D, D), D ** -0.5),
        'gla_w_in': nrm((nC, D, 2 * GLA_HEADS * GLA_DK + 2 * GLA_HEADS * GLA_DV), D ** -0.5),
        'gla_gate_w1': nrm((nC, 2, D, GLA_GATE_RANK), D ** -0.5),
        'gla_gate_w2': nrm((nC, 2, GLA_GATE_RANK, GLA_HEADS * GLA_DK), GLA_GATE_RANK ** -0.5),
        'gla_gate_b': nrm((nC, 2, GLA_HEADS * GLA_DK), 0.5),
        'gla_onorm': 1.0 + nrm((nC, GLA_DV), 0.02),
        'gla_w_out': nrm((nC, D, D), D ** -0.5),
        'router_w': nrm((DEPTH, D, N_EXPERTS), D ** -0.5),
        'exp_w_gate': nrm((DEPTH, N_EXPERTS, D, EXPERT_FF), D ** -0.5),
        'exp_w_up': nrm((DEPTH, N_EXPERTS, D, EXPERT_FF), D ** -0.5),
        'exp_w_down': nrm((DEPTH, N_EXPERTS, EXPERT_FF, D), EXPERT_FF ** -0.5),
        'final_norm': 1.0 + nrm((D,), 0.02),
    }


def reference(x, c, ctx, c_ctx, mod_w, mod_b, norm_mix, norm_ffn, conv_w_in, conv_w_dw, conv_w_out,
              diff_w_qkv, diff_lambda, diff_subln, diff_w_out, gla_w_in, gla_gate_w1, gla_gate_w2,
              gla_gate_b, gla_onorm, gla_w_out, router_w, exp_w_gate, exp_w_up, exp_w_down, final_norm):
    rows = x.shape[1] // GRID_W
    row = jnp.repeat(jnp.arange(rows, dtype=jnp.int32), GRID_W)
    col = jnp.tile(jnp.arange(GRID_W, dtype=jnp.int32), rows)
    c_lat = c[:, None, :]
    c_pre = c_ctx[None, None, :]
    for i in range(DEPTH):
        kind, j = i % N_MIXERS, i // N_MIXERS
        ctx_next = i < DEPTH - 1
        ctx_read = ctx_next or kind != 0
        sh1, sc1, g1, sh2, sc2, g2 = _modulation(c_lat, mod_w[i], mod_b[i])
        h_lat = _modulate(_rmsnorm(x, norm_mix[i]), sh1, sc1)
        h_ctx = None
        if ctx_read:
            csh1, csc1, cg1, csh2, csc2, cg2 = _modulation(c_pre, mod_w[i], mod_b[i])
            h_ctx = _modulate(_rmsnorm(ctx, norm_mix[i]), csh1, csc1)
        if kind == 0:
            y_lat = _short_conv(h_lat, conv_w_in[j], conv_w_dw[j], conv_w_out[j])
            y_ctx = _short_conv(h_ctx, conv_w_in[j], conv_w_dw[j], conv_w_out[j]) if ctx_next else None
        elif kind == 1:
            lambda_init = DIFF_LAMBDA_A - DIFF_LAMBDA_B * math.exp(-DIFF_LAMBDA_C * i)
            y_lat, y_ctx = _diff_attention(h_lat, h_ctx, row, col, diff_w_qkv[j], diff_lambda[j],
                                           diff_subln[j], diff_w_out[j], lambda_init, ctx_next)
        else:
            y_lat, y_ctx = _gla(h_lat, h_ctx, gla_w_in[j], gla_gate_w1[j], gla_gate_w2[j], gla_gate_b[j],
                                gla_onorm[j], gla_w_out[j], ctx_next)
        x = x + g1 * y_lat
        x = x + g2 * _ec_moe(_modulate(_rmsnorm(x, norm_ffn[i]), sh2, sc2),
                             router_w[i], exp_w_gate[i], exp_w_up[i], exp_w_down[i])
        if ctx_next:
            ctx = ctx + cg1 * y_ctx
            ctx = ctx + cg2 * _ec_moe(_modulate(_rmsnorm(ctx, norm_ffn[i]), csh2, csc2),
                                      router_w[i], exp_w_gate[i], exp_w_up[i], exp_w_down[i])
    return _rmsnorm(x, final_norm)
```

```python
import numpy as np
import concourse.bass as bass
import concourse.mybir as mybir
from concourse.bass_utils import run_bass_kernel_spmd

F32 = mybir.dt.float32
BF16 = mybir.dt.bfloat16
I32 = mybir.dt.int32
U32 = mybir.dt.uint32
AF = mybir.ActivationFunctionType
ALU = mybir.AluOpType
AX = mybir.AxisListType


class Sched:
    ENG = ("pe", "act", "dve", "pool", "sp")

    def __init__(self, nc, es, n_dma_sems=12, same_engine_sync=True):
        self.nc = nc
        self.same = same_engine_sync
        self.thunks = {e: [] for e in self.ENG}
        self.sem = {e: es.enter_context(nc.semaphore("c_" + e)) for e in self.ENG}
        self.cnt = {e: 0 for e in self.ENG}
        self.seen = {e: {} for e in self.ENG}
        self.dsems = {}
        for q in ("sp", "pool", "act"):
            self.dsems[q] = [
                [es.enter_context(nc.semaphore("d_%s%d" % (q, i))), 0] for i in range(n_dma_sems)
            ]
        self.dnext = {q: 0 for q in self.dsems}
        self.lastw = {}
        self.reads = {}
        self.n_wait = 0

    def _need(self, eng, evs):
        best = {}
        for ev in evs:
            if ev is None:
                continue
            sem, name, val = ev
            if name == "c_" + eng and not self.same:
                continue
            if name == "c_pe" and eng == "pe":
                continue
            if self.seen[eng].get(name, 0) >= val:
                continue
            if name not in best or best[name][1] < val:
                best[name] = (sem, val)
        for name, (sem, val) in best.items():
            self.seen[eng][name] = val
            self.thunks[eng].append(lambda e, sem=sem, val=val: e.wait_ge(sem, val))
            self.n_wait += 1

    def _deps(self, reads, writes):
        evs = []
        for k in reads:
            evs.append(self.lastw.get(k))
        for k in writes:
            evs.append(self.lastw.get(k))
            evs.extend(self.reads.get(k, ()))
        return evs

    def _commit(self, ev, reads, writes):
        for k in reads:
            self.reads.setdefault(k, []).append(ev)
        for k in writes:
            self.lastw[k] = ev
            self.reads[k] = []

    def op(self, eng, fn, reads=(), writes=()):
        self._need(eng, self._deps(reads, writes))
        self.cnt[eng] += 1
        sem = self.sem[eng]
        self.thunks[eng].append(lambda e, fn=fn, sem=sem: fn(e).then_inc(sem, 1))
        ev = (sem, "c_" + eng, self.cnt[eng])
        self._commit(ev, reads, writes)
        return ev

    def dma(self, q, out, in_, reads=(), writes=(), **kw):
        slot = self.dsems[q][self.dnext[q] % len(self.dsems[q])]
        self.dnext[q] += 1
        sem, val = slot
        name = sem.name if hasattr(sem, "name") else str(id(sem))
        name = "d_%s_%d" % (q, (self.dnext[q] - 1) % len(self.dsems[q]))
        evs = self._deps(reads, writes)
        if val > 0:
            evs.append((sem, name, val))
        self._need(q, evs)
        slot[1] = val + 16
        self.thunks[q].append(
            lambda e, out=out, in_=in_, sem=sem, kw=kw: e.dma_start(out=out, in_=in_, **kw).then_inc(sem, 16)
        )
        ev = (sem, name, val + 16)
        self._commit(ev, reads, writes)
        return ev

    def dma_custom(self, q, fn, reads=(), writes=()):
        slot = self.dsems[q][self.dnext[q] % len(self.dsems[q])]
        name = "d_%s_%d" % (q, self.dnext[q] % len(self.dsems[q]))
        self.dnext[q] += 1
        sem, val = slot
        evs = self._deps(reads, writes)
        if val > 0:
            evs.append((sem, name, val))
        self._need(q, evs)
        slot[1] = val + 16
        self.thunks[q].append(lambda e, fn=fn, sem=sem: fn(e).then_inc(sem, 16))
        ev = (sem, name, val + 16)
        self._commit(ev, reads, writes)
        return ev

    def wait_all(self, eng):
        evs = []
        for k, ev in self.lastw.items():
            evs.append(ev)
        for k, l in self.reads.items():
            evs.extend(l)
        self._need(eng, evs)

    def emit(self):
        nc = self.nc
        with nc.Block() as block:
            @block.tensor
            def _(e):
                for t in self.thunks["pe"]:
                    t(e)

            @block.scalar
            def _(e):
                for t in self.thunks["act"]:
                    t(e)

            @block.vector
            def _(e):
                for t in self.thunks["dve"]:
                    t(e)

            @block.gpsimd
            def _(e):
                for t in self.thunks["pool"]:
                    t(e)

            @block.sync
            def _(e):
                for t in self.thunks["sp"]:
                    t(e)
        self.thunks = {e: [] for e in self.ENG}


import contextlib
import math
import numpy as np
import ml_dtypes

D = 2048
CTX = 256
FF = 1536
EPS = 1e-6


def prod(t):
    r = 1
    for a in t:
        r *= a
    return r


class Mem:
    def __init__(self, prog):
        self.prog = prog

    def alloc(self, free, dt, parts=128):
        if isinstance(free, int):
            free = (free,)
        n = prod(free)
        p = self.prog
        p.uid += 1
        t = p.pes.enter_context(p.nc.sbuf_tensor("b%d" % p.uid, [128, n], dt))
        v = t[:parts, :]
        if len(free) == 2:
            v = v.rearrange("p (a b) -> p a b", a=free[0])
        elif len(free) == 3:
            v = v.rearrange("p (a b c) -> p a b c", a=free[0], b=free[1])
        return v


class Cfg:
    def __init__(self, LAT=8192, E=16, DEPTH=4, CF=2):
        self.LAT = LAT
        self.E = E
        self.DEPTH = DEPTH
        self.NTOK = CTX + LAT
        self.cap_l = CF * LAT // E
        self.cap_c = CF * CTX // E
        self.nA = sum(1 for i in range(DEPTH) if i % 3 == 0)
        self.nB = sum(1 for i in range(DEPTH) if i % 3 == 1)
        self.nC = sum(1 for i in range(DEPTH) if i % 3 == 2)


class Prog:
    def __init__(self, cfg):
        self.cfg = cfg
        self.nc = bass.Bass("TRN2", target_bir_lowering=False)
        self.din = {}
        self.uid = 0

    def inp(self, name, shape, dt=F32):
        self.din[name] = self.nc.dram_tensor(name, list(shape), dt, kind="ExternalInput").ap()
        return self.din[name]

    def scratch(self, name, shape, dt):
        return self.nc.dram_tensor(name, list(shape), dt, kind="Internal").ap()

    def barrier(self):
        S = self.S
        evs = []
        for k, ev in S.lastw.items():
            evs.append(ev)
        for k, l in S.reads.items():
            evs.extend(l)
        for e in S.ENG:
            S._need(e, evs)
        S.lastw.clear()
        S.reads.clear()

    def begin_phase(self):
        self.pes = contextlib.ExitStack()
        self.nphase = getattr(self, "nphase", 0) + 1
        self.ps = [self.pes.enter_context(self.nc.psum_tensor("ps%d_%d" % (self.nphase, i), [128, 512], F32)) for i in range(8)]

    def end_phase(self, last=False):
        self.barrier()
        self.S.emit()
        self.pes.close()
        if not last:
            self.begin_phase()

    def act(self, out, in_, func, reads, writes, **kw):
        self.S.op("act", lambda e: e.activation(out=out, in_=in_, func=func, **kw), reads, writes)

    def ts(self, eng, out, in0, s1, s2, op0, op1, reads, writes, **kw):
        if op1 is None:
            self.S.op(eng, lambda e: e.tensor_scalar(out=out, in0=in0, scalar1=s1, scalar2=None, op0=op0, **kw), reads, writes)
        else:
            self.S.op(eng, lambda e: e.tensor_scalar(out=out, in0=in0, scalar1=s1, scalar2=s2, op0=op0, op1=op1, **kw), reads, writes)

    def tt(self, eng, out, in0, in1, op, reads, writes):
        self.S.op(eng, lambda e: e.tensor_tensor(out=out, in0=in0, in1=in1, op=op), reads, writes)

    def stt(self, out, in0, scalar, in1, op0, op1, reads, writes):
        self.S.op("dve", lambda e: e.scalar_tensor_tensor(out=out, in0=in0, scalar=scalar, in1=in1, op0=op0, op1=op1), reads, writes)

    def rsqrt(self, ap, key):
        self.S.op("act", lambda e: e.activation(out=ap, in_=ap, func=AF.Sqrt), [key], [key])
        self.S.op("dve", lambda e: e.reciprocal(out=ap, in_=ap), [key], [key])

    def copy(self, eng, out, in_, reads, writes):
        if eng == "act":
            self.S.op("act", lambda e: e.activation(out=out, in_=in_, func=AF.Copy), reads, writes)
        else:
            self.S.op(eng, lambda e: e.tensor_copy(out=out, in_=in_), reads, writes)

    def memset(self, eng, out, val, writes):
        self.S.op(eng, lambda e: e.memset(out, val), (), writes)

    def mm(self, out, lhsT, rhs, start, stop, reads, writes):
        self.S.op("pe", lambda e: e.matmul(out, lhsT=lhsT, rhs=rhs, start=start, stop=stop), reads, writes)

    def tr(self, out, in_, ident, reads, writes):
        self.S.op("pe", lambda e: e.transpose(out=out, in_=in_, identity=ident), reads, writes)

    def dma(self, q, out, in_, reads, writes, **kw):
        self.S.dma(q, out, in_, reads, writes, **kw)

    def ring(self, name, n, free, dt, parts=128):
        tiles = [self.mem.alloc(free, dt, parts) for _ in range(n)]
        st = {"i": 0}

        def nxt():
            i = st["i"] % n
            st["i"] += 1
            return tiles[i], (name, i)
        return nxt

    def psring(self, name, idxs, bf16=False):
        st = {"i": 0}

        def nxt():
            i = idxs[st["i"] % len(idxs)]
            st["i"] += 1
            t = self.ps[i][:]
            if bf16:
                t = t.bitcast(BF16)
            return t, ("ps", i)
        return nxt

    def load_w(self, dst, key, w_ap, ncols):
        kc = w_ap.shape[0] // 128
        src = w_ap.rearrange("(k p) n -> p k n", p=128)
        step = max(1, kc // 4)
        for k0 in range(0, kc, step):
            k1 = min(kc, k0 + step)
            self.S.dma("pool", dst[:, k0:k1, 0:ncols], src[:, k0:k1, :], (), [key])

    def build(self):
        cfg = self.cfg
        nc = self.nc
        LAT, E, DEPTH, NTOK = cfg.LAT, cfg.E, cfg.DEPTH, cfg.NTOK
        inp = self.inp
        inp("xin", [LAT, D])
        inp("ctxin", [CTX, D])
        inp("ccols", [128, 32])
        inp("mod_w", [DEPTH, D, 6 * D])
        inp("mod_b", [DEPTH, 6 * D])
        inp("norm_mix", [DEPTH, D])
        inp("norm_ffn", [DEPTH, D])
        inp("final_norm", [1, D])
        inp("conv_w_in_r", [cfg.nA, D, 3 * D])
        inp("conv_dw_cols", [cfg.nA, 128, 48])
        inp("conv_w_out", [cfg.nA, D, D])
        inp("router_w", [DEPTH, D, E])
        inp("exp_w_gate", [DEPTH, E, D, FF])
        inp("exp_w_up", [DEPTH, E, D, FF])
        inp("exp_w_down", [DEPTH, E, FF, D])
        inp("ident", [128, 128], BF16)
        if cfg.nB:
            inp("diff_w_qkv", [cfg.nB, D, 3 * D])
            inp("diff_lambda", [cfg.nB, 1, 512])
            inp("diff_subln", [cfg.nB, 256])
            inp("diff_w_out", [cfg.nB, D, D])
            inp("ropeC", [128, NTOK])
            inp("ropeS", [128, NTOK])
            inp("ropeP", [128, 128], BF16)
            self.PM = self.scratch("PM", [NTOK, D], BF16)
        if cfg.nC:
            inp("gla_w_in", [cfg.nC, D, 3 * D])
            inp("gla_gate_w1", [cfg.nC, 2, D, 16])
            inp("gla_gate_w2", [cfg.nC, 2, 16, 1024])
            inp("gla_gate_b", [cfg.nC, 2, 1024])
            inp("gla_onorm", [cfg.nC, 512])
            inp("gla_w_out", [cfg.nC, D, D])
            inp("gmask", [2, 128, 128])
            inp("gtri", [2, 128, 128])
            self.Gm = self.scratch("Gm", [NTOK, D], BF16)
            self.SP = self.scratch("SP", [2, NTOK, 1024], F32)
            self.OF = self.scratch("OF", [NTOK, D], F32)
            if not cfg.nB:
                self.PM = self.scratch("PM", [NTOK, D], BF16)
        self.out = nc.dram_tensor("out", [LAT, D], F32, kind="ExternalOutput").ap()
        self.X = self.scratch("X", [NTOK, D], F32)
        self.modv = self.scratch("modv", [DEPTH, 2, 6 * D], F32)
        self.Hm = self.scratch("Hm", [NTOK, D], BF16)
        self.VT = self.scratch("VT", [D, NTOK], BF16)
        self.BT = self.scratch("BT", [D, NTOK], BF16)
        self.PT = self.scratch("PT", [D, NTOK], BF16)
        self.idx_d = self.scratch("idx_d", [2, E, max(cfg.cap_l, 128)], U32)
        self.gate_d = self.scratch("gate_d", [2, E, max(cfg.cap_l, 128)], F32)
        with contextlib.ExitStack() as es:
            self.S = Sched(nc, es)
            self.mem = Mem(self)
            self.begin_phase()
            self.body()
            self.end_phase(last=True)
        return nc

    def body(self):
        cfg = self.cfg
        self.setup()
        for i in range(cfg.DEPTH):
            kind, j = i % 3, i // 3
            ctx_on = i < cfg.DEPTH - 1
            self.cur_blocks = self.blocks(ctx_on)
            self.phase_mod(i)
            if kind == 0:
                self.phase_conv(i, j, ctx_on)
                self.phase_outproj(i, self.PT, True, self.din["conv_w_out"][j], ctx_on)
            elif kind == 1:
                self.phase_attn(i, j, ctx_on)
                self.phase_outproj(i, self.PM, False, self.din["diff_w_out"][j], ctx_on)
            else:
                self.phase_gla(i, j, ctx_on)
                self.phase_outproj(i, self.PM, False, self.din["gla_w_out"][j], ctx_on)
            self.phase_moe(i, ctx_on)
        self.phase_final()

    def blocks(self, ctx_on):
        b = []
        if ctx_on:
            b.append((0, CTX, 1))
        for r in range(0, self.cfg.LAT, 1024):
            b.append((CTX + r, min(1024, self.cfg.LAT - r), 0))
        return b

    def setup(self):
        self.dma("sp", self.X[0:CTX, :], self.din["ctxin"], (), ["X"])
        LAT = self.cfg.LAT
        for r in range(0, LAT, 1024):
            self.dma("sp", self.X[CTX + r:CTX + r + 1024, :], self.din["xin"][r:r + 1024, :], (), ["X"])
        z = self.mem.alloc(D, BF16)
        self.memset("pool", z, 0.0, ["z"])
        for t in range(CTX // 128):
            self.dma("sp", self.Hm[t * 128:(t + 1) * 128, :], z, ["z"], ["Hm"])
        self.end_phase()

    def phase_mod(self, i):
        m = self.mem
        cc = m.alloc(32, F32)
        sc = m.alloc((16, 2), BF16)
        bias = m.alloc(6 * D, F32, parts=2)
        res = m.alloc(6 * D, F32, parts=2)
        wring = self.ring("modw", 2, (16, 512), BF16)
        pring = self.psring("modp", [0, 1])
        self.dma("sp", cc, self.din["ccols"], (), ["cc"])
        self.act(sc.rearrange("p k v -> p (k v)"), cc, AF.Silu, ["cc"], ["sc"])
        for v in range(2):
            self.dma("sp", bias[v:v + 1, :], self.din["mod_b"][i:i + 1, :], (), ["mbias"])
        for cb in range(6 * D // 512):
            wt, wk = wring()
            self.load_w(wt, wk, self.din["mod_w"][i][:, cb * 512:(cb + 1) * 512], 512)
            pt, pk = pring()
            for k in range(16):
                self.mm(pt[0:2, :], sc[:, k, :], wt[:, k, :], k == 0, k == 15, ["sc", wk], [pk])
            self.tt("dve", res[:, cb * 512:(cb + 1) * 512], pt[0:2, :], bias[:, cb * 512:(cb + 1) * 512], ALU.add,
                    [pk, "mbias"], ["mres"])
        self.dma("sp", self.modv[i], res, ["mres"], ["modv"])
        self.end_phase()

    def load_mod_bc(self, i, which, v, tag):
        m = self.mem
        a_sh, a_sc = (0, 1) if which == "mix" else (3, 4)
        gs = m.alloc(D, F32)
        sh = m.alloc(D, F32)
        tmp = m.alloc(D, F32)
        norm = self.din["norm_mix" if which == "mix" else "norm_ffn"]
        self.dma("sp", gs, self.modv[i, v, a_sc * D:(a_sc + 1) * D].partition_broadcast(128), ["modv"], [tag + "gs"])
        self.dma("sp", tmp, norm[i, :].partition_broadcast(128), (), [tag + "tmp"])
        self.dma("sp", sh, self.modv[i, v, a_sh * D:(a_sh + 1) * D].partition_broadcast(128), ["modv"], [tag + "sh"])
        self.stt(gs, gs, 1.0, tmp, ALU.add, ALU.mult, [tag + "gs", tag + "tmp"], [tag + "gs"])
        return gs, sh, tag + "gs", tag + "sh"

    def load_gate_bc(self, i, which, v, tag):
        a = 2 if which == "mix" else 5
        g = self.mem.alloc(D, F32)
        self.dma("sp", g, self.modv[i, v, a * D:(a + 1) * D].partition_broadcast(128), ["modv"], [tag])
        return g, tag

    def norm_setup(self, i, which, ctx_on):
        m = self.mem
        r = {}
        r["ident"] = m.alloc(128, BF16)
        self.dma("sp", r["ident"], self.din["ident"], (), ["ident"])
        r["mod"] = {0: self.load_mod_bc(i, which, 0, "mL")}
        if ctx_on:
            r["mod"][1] = self.load_mod_bc(i, which, 1, "mC")
        r["xring"] = self.ring("nx", 2, D, F32)
        r["tring"] = self.ring("ntmp", 2, D, F32)
        r["hring"] = self.ring("nh", 2, D, BF16)
        r["ss"] = self.ring("nss", 2, 1, F32)
        r["rs"] = self.ring("nrs", 2, 1, F32)
        r["junk"] = m.alloc(D, BF16)
        r["ptr"] = self.psring("ntr", [6, 7], bf16=True)
        return r

    def norm_block(self, r, blk, hT, hkey, hm_store=False):
        row0, ntok, v = blk
        gs, sh, gsk, shk = r["mod"][v]
        for t in range(ntok // 128):
            xs, xk = r["xring"]()
            self.dma("sp", xs, self.X[row0 + t * 128:row0 + (t + 1) * 128, :], ["X"], [xk])
            ss, sk = r["ss"]()
            rs, rk = r["rs"]()
            self.act(r["junk"], xs, AF.Square, [xk], ["njunk", sk], accum_out=ss)
            self.ts("dve", rs, ss, 1.0 / D, EPS, ALU.mult, ALU.add, [sk], [rk])
            self.rsqrt(rs, rk)
            tmp, tk = r["tring"]()
            self.stt(tmp, xs, rs[:, 0:1], gs, ALU.mult, ALU.mult, [xk, rk, gsk], [tk])
            hb, hk = r["hring"]()
            self.tt("pool", hb, tmp, sh, ALU.add, [tk, shk], [hk])
            if hm_store:
                self.dma("sp", self.Hm[row0 + t * 128:row0 + (t + 1) * 128, :], hb, [hk], ["Hm"])
            self.transpose_tile(r, hb, hk, 128, hT, hkey, t * 128)

    def transpose_tile(self, r, hb, hk, nrow, hT, hkey, col0):
        for g in range(2):
            pt, pk = r["ptr"]()
            for kk in range(8):
                k = g * 8 + kk
                self.tr(pt[:, kk * 128:kk * 128 + nrow], hb[0:nrow, k * 128:(k + 1) * 128], r["ident"][0:nrow, 0:nrow],
                        [hk, "ident"], [pk])
            src = pt.rearrange("p (k t) -> p k t", k=8)[:, :, 0:nrow]
            self.copy("act" if g == 0 else "dve", hT[:, g * 8:(g + 1) * 8, col0:col0 + nrow], src, [pk], [hkey])

    def phase_conv(self, i, j, ctx_on):
        m = self.mem
        r = self.norm_setup(i, "mix", ctx_on)
        hT = m.alloc((16, 1024), BF16)
        wring = self.ring("cw", 2, (16, 384), BF16)
        pring = self.psring("cp", [0, 1, 2, 3, 4, 5])
        ub = self.ring("cub", 2, 512, F32)
        vst = self.ring("cvst", 2, 512, BF16)
        bst = self.ring("cbst", 2, 512, BF16)
        win = self.din["conv_w_in_r"][j]
        for blk in self.cur_blocks:
            row0, ntok, v = blk
            self.norm_block(r, blk, hT, "hT")
            for n in range(16):
                wt, wk = wring()
                self.load_w(wt, wk, win[:, n * 384:(n + 1) * 384], 384)
                for m0 in range(0, ntok, 512):
                    mw = min(512, ntok - m0)
                    pB, kB = pring()
                    pC, kC = pring()
                    pU, kU = pring()
                    for (pp, pk, c0) in ((pB, kB, 0), (pC, kC, 128), (pU, kU, 256)):
                        for k in range(16):
                            self.mm(pp[:, 0:mw], wt[:, k, c0:c0 + 128], hT[:, k, m0:m0 + mw], k == 0, k == 15,
                                    [wk, "hT"], [pk])
                    u, uk = ub()
                    self.copy("act", u[:, 0:mw], pU[:, 0:mw], [kU], [uk])
                    vs, vk = vst()
                    self.tt("dve", vs[:, 0:mw], pC[:, 0:mw], u[:, 0:mw], ALU.mult, [kC, uk], [vk])
                    bs, bk = bst()
                    self.copy("act", bs[:, 0:mw], pB[:, 0:mw], [kB], [bk])
                    self.dma("sp", self.VT[n * 128:(n + 1) * 128, row0 + m0:row0 + m0 + mw], vs[:, 0:mw], [vk], ["VT"])
                    self.dma("sp", self.BT[n * 128:(n + 1) * 128, row0 + m0:row0 + m0 + mw], bs[:, 0:mw], [bk], ["BT"])
        self.end_phase()
        dw = m.alloc(48, F32)
        self.dma("sp", dw, self.din["conv_dw_cols"][j], (), ["dw"])
        PW = 2048
        vring = self.ring("c2v", 2, PW + 2, BF16)
        bring = self.ring("c2b", 2, PW, BF16)
        aring = self.ring("c2a", 2, PW, F32)
        gring = self.ring("c2g", 2, PW, BF16)
        segs = []
        if ctx_on:
            segs.append((0, CTX))
        segs.append((CTX, self.cfg.NTOK))
        for n in range(16):
            for (s0, s1) in segs:
                for t0 in range(s0, s1, PW):
                    w = min(PW, s1 - t0)
                    vp, vk = vring()
                    lo = t0 - 1
                    hi = t0 + w + 1
                    c0 = 0
                    if lo < s0:
                        self.memset("pool", vp[:, 0:1], 0.0, [vk])
                        lo = s0
                        c0 = 1
                    if hi > s1:
                        self.memset("pool", vp[:, w + 1:w + 2], 0.0, [vk])
                        hi = s1
                    self.dma("sp", vp[:, c0:c0 + hi - lo], self.VT[n * 128:(n + 1) * 128, lo:hi], ["VT"], [vk])
                    bp, bk = bring()
                    self.dma("sp", bp[:, 0:w], self.BT[n * 128:(n + 1) * 128, t0:t0 + w], ["BT"], [bk])
                    ac, ak = aring()
                    self.ts("dve", ac[:, 0:w], vp[:, 1:w + 1], dw[:, 16 + n:17 + n], None, ALU.mult, None, [vk, "dw"], [ak])
                    self.stt(ac[:, 0:w], vp[:, 0:w], dw[:, n:n + 1], ac[:, 0:w], ALU.mult, ALU.add, [vk, "dw", ak], [ak])
                    self.stt(ac[:, 0:w], vp[:, 2:w + 2], dw[:, 32 + n:33 + n], ac[:, 0:w], ALU.mult, ALU.add, [vk, "dw", ak], [ak])
                    gp, gk = gring()
                    self.tt("pool", gp[:, 0:w], ac[:, 0:w], bp[:, 0:w], ALU.mult, [ak, bk], [gk])
                    self.dma("sp", self.PT[n * 128:(n + 1) * 128, t0:t0 + w], gp[:, 0:w], [gk], ["PT"])
        self.end_phase()

    def phase_outproj(self, i, src, src_fm, w_ap, ctx_on):
        m = self.mem
        w = m.alloc((16, D), BF16)
        self.load_w(w, "ow", w_ap, D)
        gate = {0: self.load_gate_bc(i, "mix", 0, "ogL")}
        if ctx_on:
            gate[1] = self.load_gate_bc(i, "mix", 1, "ogC")
        aT = m.alloc((16, 1024), BF16)
        xring = self.ring("ox", 2, D, F32)
        tring = self.ring("ot", 2, 512, F32)
        pring = self.psring("op", [0, 1, 2, 3])
        if not src_fm:
            ident = m.alloc(128, BF16)
            self.dma("sp", ident, self.din["ident"], (), ["ident"])
            r = {"ident": ident, "ptr": self.psring("otr", [6, 7], bf16=True)}
            hring = self.ring("oh", 2, D, BF16)
        for blk in self.cur_blocks:
            row0, ntok, v = blk
            g, gk = gate[v]
            if src_fm:
                srcv = src.rearrange("(k p) t -> p k t", p=128)
                for k0 in range(0, 16, 4):
                    self.dma("sp", aT[:, k0:k0 + 4, 0:ntok], srcv[:, k0:k0 + 4, row0:row0 + ntok], ["PT"], ["aT"])
            else:
                for t in range(ntok // 128):
                    hb, hk = hring()
                    self.dma("sp", hb, src[row0 + t * 128:row0 + (t + 1) * 128, :], ["PM"], [hk])
                    self.transpose_tile(r, hb, hk, 128, aT, "aT", t * 128)
            for t in range(ntok // 128):
                xs, xk = xring()
                self.dma("sp", xs, self.X[row0 + t * 128:row0 + (t + 1) * 128, :], ["X"], [xk])
                for cb in range(4):
                    pt, pk = pring()
                    for k in range(16):
                        self.mm(pt, aT[:, k, t * 128:(t + 1) * 128], w[:, k, cb * 512:(cb + 1) * 512], k == 0, k == 15,
                                ["aT", "ow"], [pk])
                    tmp, tk = tring()
                    self.tt("dve", tmp, pt, g[:, cb * 512:(cb + 1) * 512], ALU.mult, [pk, gk], [tk])
                    self.tt("pool", xs[:, cb * 512:(cb + 1) * 512], xs[:, cb * 512:(cb + 1) * 512], tmp, ALU.add, [xk, tk], [xk])
                self.dma("sp", self.X[row0 + t * 128:row0 + (t + 1) * 128, :], xs, [xk], ["X"])
        self.end_phase()


    def phase_attn(self, i, j, ctx_on):
        cfg = self.cfg
        m = self.mem
        NTOK = cfg.NTOK
        NT = NTOK // 128
        QT, KT, Vm = self.VT, self.BT, self.Hm
        wqkv = self.din["diff_w_qkv"][j]
        r = self.norm_setup(i, "mix", True)
        hT = m.alloc((16, 1024), BF16)
        Pm = m.alloc(128, BF16)
        self.dma("sp", Pm, self.din["ropeP"], (), ["Pm"])
        cring = self.ring("rc", 2, 512, F32)
        sring = self.ring("rs", 2, 512, F32)
        wring = self.ring("aw", 2, (16, 512), BF16)
        xbr = self.ring("axb", 2, 512, BF16)
        t1r = self.ring("at1", 2, 512, F32)
        t2r = self.ring("at2", 2, 512, F32)
        ror = self.ring("aro", 2, 512, BF16)
        vsr = self.ring("avs", 2, 512, BF16)
        pa = self.psring("apa", [0, 1])
        pp = self.psring("app", [2, 3])
        pv = self.psring("apv", [4, 5])
        for blk in self.blocks(True):
            row0, ntok, v = blk
            self.norm_block(r, blk, hT, "hT")
            for wb in range(8):
                wt, wk = wring()
                self.load_w(wt, wk, wqkv[:, wb * 512:(wb + 1) * 512], 512)
                for m0 in range(0, ntok, 512):
                    mw = min(512, ntok - m0)
                    ct, ck = cring()
                    st, sk = sring()
                    self.dma("sp", ct[:, 0:mw], self.din["ropeC"][:, row0 + m0:row0 + m0 + mw], (), [ck])
                    self.dma("sp", st[:, 0:mw], self.din["ropeS"][:, row0 + m0:row0 + m0 + mw], (), [sk])
                    for c4 in range(4):
                        c = wb * 4 + c4
                        a_, ak = pa()
                        for k in range(16):
                            self.mm(a_[:, 0:mw], wt[:, k, c4 * 128:(c4 + 1) * 128], hT[:, k, m0:m0 + mw], k == 0, k == 15, [wk, "hT"], [ak])
                        xb, xk = xbr()
                        self.copy("act", xb[:, 0:mw], a_[:, 0:mw], [ak], [xk])
                        p_, pk = pp()
                        self.mm(p_[:, 0:mw], Pm, xb[:, 0:mw], True, True, ["Pm", xk], [pk])
                        t1, t1k = t1r()
                        self.tt("pool", t1[:, 0:mw], xb[:, 0:mw], ct[:, 0:mw], ALU.mult, [xk, ck], [t1k])
                        t2, t2k = t2r()
                        self.tt("dve", t2[:, 0:mw], p_[:, 0:mw], st[:, 0:mw], ALU.mult, [pk, sk], [t2k])
                        ro, rk = ror()
                        self.tt("dve", ro[:, 0:mw], t1[:, 0:mw], t2[:, 0:mw], ALU.add, [t1k, t2k], [rk])
                        dst = QT if c < 16 else KT
                        self.dma("sp", dst[(c % 16) * 128:(c % 16 + 1) * 128, row0 + m0:row0 + m0 + mw], ro[:, 0:mw], [rk], ["QK"])
            for vb in range(4):
                wt, wk = wring()
                self.load_w(wt, wk, wqkv[:, 4096 + vb * 512:4096 + (vb + 1) * 512], 512)
                for t in range(ntok // 128):
                    v_, vk = pv()
                    for k in range(16):
                        self.mm(v_, hT[:, k, t * 128:(t + 1) * 128], wt[:, k, :], k == 0, k == 15, ["hT", wk], [vk])
                    vs, vsk = vsr()
                    self.copy("act", vs, v_, [vk], [vsk])
                    self.dma("sp", Vm[row0 + t * 128:row0 + (t + 1) * 128, vb * 512:(vb + 1) * 512], vs, [vsk], ["Vm"])
        self.end_phase()
        lambda_init = 0.8 - 0.6 * math.exp(-0.3 * i)
        lp = m.alloc(512, F32, parts=1)
        self.dma("sp", lp, self.din["diff_lambda"][j], (), ["lp"])
        pr = m.alloc(256, F32, parts=1)
        sm = m.alloc(2, F32, parts=1)
        lam1 = m.alloc(1, F32, parts=1)
        self.tt("dve", pr[:, 0:128], lp[:, 0:128], lp[:, 128:256], ALU.mult, ["lp"], ["pr"])
        self.tt("dve", pr[:, 128:256], lp[:, 256:384], lp[:, 384:512], ALU.mult, ["lp"], ["pr"])
        self.S.op("dve", lambda e: e.reduce_sum(out=sm[:, 0:1], in_=pr[:, 0:128], axis=AX.X), ["pr"], ["sm"])
        self.S.op("dve", lambda e: e.reduce_sum(out=sm[:, 1:2], in_=pr[:, 128:256], axis=AX.X), ["pr"], ["sm"])
        self.act(sm, sm, AF.Exp, ["sm"], ["sm"])
        self.tt("dve", lam1, sm[:, 0:1], sm[:, 1:2], ALU.subtract, ["sm"], ["lam1"])
        self.ts("dve", lam1, lam1, -1.0, -lambda_init, ALU.mult, ALU.add, ["lam1"], ["lam1"])
        ones1 = m.alloc(128, F32, parts=1)
        self.memset("pool", ones1, 1.0, ["ones1"])
        neglam = m.alloc(1, F32)
        pl, plk = pa()
        self.mm(pl[:, 0:1], ones1, lam1, True, True, ["ones1", "lam1"], [plk])
        self.copy("dve", neglam, pl[:, 0:1], [plk], ["neglam"])
        sub = m.alloc(256, F32)
        self.dma("sp", sub, self.din["diff_subln"][j, :].partition_broadcast(128), (), ["sub"])
        self.ts("dve", sub, sub, 1.0 - lambda_init, None, ALU.mult, None, ["sub"], ["sub"])
        kring = self.ring("kth", 2, (2, NTOK), BF16)
        vring = self.ring("vh", 2, (NT, 257), BF16)
        for _ in range(2):
            vh, vhk = vring()
            self.memset("pool", vh[:, :, 256:257], 1.0, [vhk])
        qring = self.ring("qt", 2, (2, 256), BF16)
        ptr = self.ring("pT", 4, 256, BF16)
        psc = self.psring("psc", [4, 5, 6, 7])
        tar = self.ring("tA", 2, 256, F32)
        orr = self.ring("ao", 2, 256, F32)
        osr = self.ring("aos", 2, 256, BF16)
        smr = self.ring("asm", 2, 4, F32)
        junk = m.alloc(256, BF16)
        scale = 1.0 / math.sqrt(128.0)
        qblocks = []
        if ctx_on:
            qblocks.append((0, 0, 2))
        for q0 in range(CTX, NTOK, 256):
            qblocks.append((q0, 0, NT))
        for h in range(8):
            kth, kk = kring()
            vh, vhk = vring()
            for t in range(2):
                self.dma("sp", kth[:, t, :], KT[(2 * h + t) * 128:(2 * h + t + 1) * 128, :], ["QK"], [kk])
            vsrc = Vm[:, h * 256:(h + 1) * 256].rearrange("(kt p) c -> p kt c", p=128)
            for k0 in range(0, NT, 8):
                k1 = min(NT, k0 + 8)
                self.dma("sp", vh[:, k0:k1, 0:256], vsrc[:, k0:k1, :], ["Vm"], [vhk])
            for (q0, kt0, kt1) in qblocks:
                qt, qk = qring()
                for t in range(2):
                    self.dma("sp", qt[:, t, :], QT[(2 * h + t) * 128:(2 * h + t + 1) * 128, q0:q0 + 256], ["QK"], [qk])
                for t in range(2):
                    for kt in range(kt0, kt1):
                        s_, sk_ = psc()
                        self.mm(s_[:, 0:256], kth[:, t, kt * 128:(kt + 1) * 128], qt[:, t, :], True, True, [kk, qk], [sk_])
                        pT, pTk = ptr()
                        self.act(pT, s_[:, 0:256], AF.Exp, [sk_], [pTk], scale=scale)
                        for qi in range(2):
                            b = t * 2 + qi
                            self.mm(self.ps[b][:, 0:257], pT[:, qi * 128:(qi + 1) * 128], vh[:, kt, :], kt == kt0, kt == kt1 - 1,
                                    [pTk, vhk], [("ps", b)])
                for qi in range(2):
                    O0 = self.ps[qi]
                    O1 = self.ps[2 + qi]
                    k0_, k1_ = ("ps", qi), ("ps", 2 + qi)
                    s4, s4k = smr()
                    self.S.op("dve", lambda e, s4=s4, O0=O0: e.reciprocal(out=s4[:, 0:1], in_=O0[:, 256:257]), [k0_], [s4k])
                    self.S.op("dve", lambda e, s4=s4, O1=O1: e.reciprocal(out=s4[:, 1:2], in_=O1[:, 256:257]), [k1_], [s4k])
                    self.tt("dve", s4[:, 1:2], s4[:, 1:2], neglam, ALU.mult, [s4k, "neglam"], [s4k])
                    tA, tAk = tar()
                    self.ts("dve", tA, O0[:, 0:256], s4[:, 0:1], None, ALU.mult, None, [k0_, s4k], [tAk])
                    o, ok = orr()
                    self.stt(o, O1[:, 0:256], s4[:, 1:2], tA, ALU.mult, ALU.add, [k1_, s4k, tAk], [ok])
                    self.act(junk, o, AF.Square, [ok], ["ajunk", s4k], accum_out=s4[:, 2:3])
                    self.ts("dve", s4[:, 2:3], s4[:, 2:3], 1.0 / 256, 1e-5, ALU.mult, ALU.add, [s4k], [s4k])
                    self.rsqrt(s4[:, 2:3], s4k)
                    os_, osk = osr()
                    self.stt(os_, o, s4[:, 2:3], sub, ALU.mult, ALU.mult, [ok, s4k, "sub"], [osk])
                    self.dma("sp", self.PM[q0 + qi * 128:q0 + (qi + 1) * 128, h * 256:(h + 1) * 256], os_, [osk], ["PM"])
        self.end_phase()


    def phase_gla(self, i, j, ctx_on):
        cfg = self.cfg
        m = self.mem
        NTOK = cfg.NTOK
        NT = NTOK // 128
        QT, KT, Vm, Gm, SP, OF = self.VT, self.BT, self.Hm, self.Gm, self.SP, self.OF
        win = self.din["gla_w_in"][j]
        r = self.norm_setup(i, "mix", True)
        hT = m.alloc((16, 1024), BF16)
        wring = self.ring("gw", 2, (16, 512), BF16)
        w1 = [m.alloc((16, 16), BF16) for _ in range(2)]
        w2 = [m.alloc(1024, BF16, parts=16) for _ in range(2)]
        bb = [m.alloc(1024, F32) for _ in range(2)]
        one = m.alloc(1, F32)
        self.memset("pool", one, 1.0, ["one"])
        for dr in range(2):
            self.load_w(w1[dr], "gw1", self.din["gla_gate_w1"][j, dr], 16)
            self.S.dma("pool", w2[dr], self.din["gla_gate_w2"][j, dr], (), ["gw2"])
            self.dma("sp", bb[dr], self.din["gla_gate_b"][j, dr, :].partition_broadcast(128), (), ["gbb"])
        str_ = self.ring("gst", 2, 512, BF16)
        t1r = self.ring("gt1", 2, 1024, BF16, parts=16)
        zr = self.ring("gz", 2, 512, F32)
        spr = self.ring("gsp", 2, 512, F32)
        pa = self.psring("gpa", [0, 1, 2, 3])
        pz = self.psring("gpz", [4, 5])
        for blk in self.blocks(True):
            row0, ntok, v = blk
            self.norm_block(r, blk, hT, "hT")
            for wb in range(4):
                wt, wk = wring()
                self.load_w(wt, wk, win[:, wb * 512:(wb + 1) * 512], 512)
                for m0 in range(0, ntok, 512):
                    mw = min(512, ntok - m0)
                    for c4 in range(4):
                        c = wb * 4 + c4
                        a_, ak = pa()
                        for k in range(16):
                            self.mm(a_[:, 0:mw], wt[:, k, c4 * 128:(c4 + 1) * 128], hT[:, k, m0:m0 + mw], k == 0, k == 15, [wk, "hT"], [ak])
                        st, sk = str_()
                        if c < 8:
                            self.act(st[:, 0:mw], a_[:, 0:mw], AF.Copy, [ak], [sk], scale=1.0 / 16.0)
                        else:
                            self.copy("dve", st[:, 0:mw], a_[:, 0:mw], [ak], [sk])
                        dst = QT if c < 8 else KT
                        self.dma("sp", dst[(c % 8) * 128:(c % 8 + 1) * 128, row0 + m0:row0 + m0 + mw], st[:, 0:mw], [sk], ["QK"])
            for vb in range(8):
                wt, wk = wring()
                self.load_w(wt, wk, win[:, 2048 + vb * 512:2048 + (vb + 1) * 512], 512)
                for t in range(ntok // 128):
                    a_, ak = pa()
                    for k in range(16):
                        self.mm(a_, hT[:, k, t * 128:(t + 1) * 128], wt[:, k, :], k == 0, k == 15, ["hT", wk], [ak])
                    st, sk = str_()
                    self.copy("act" if t % 2 else "dve", st, a_, [ak], [sk])
                    dst = Vm if vb < 4 else Gm
                    self.dma("sp", dst[row0 + t * 128:row0 + (t + 1) * 128, (vb % 4) * 512:(vb % 4 + 1) * 512], st, [sk], ["VG"])
            for dr in range(2):
                t1, t1k = t1r()
                for m0 in range(0, ntok, 512):
                    mw = min(512, ntok - m0)
                    a_, ak = pz()
                    for k in range(16):
                        self.mm(a_[0:16, 0:mw], w1[dr][:, k, :], hT[:, k, m0:m0 + mw], k == 0, k == 15, ["gw1", "hT"], [ak])
                    self.copy("dve", t1[:, m0:m0 + mw], a_[0:16, 0:mw], [ak], [t1k])
                for t in range(ntok // 128):
                    for cb in range(2):
                        a_, ak = pa()
                        self.mm(a_, t1[:, t * 128:(t + 1) * 128], w2[dr][:, cb * 512:(cb + 1) * 512], True, True, [t1k, "gw2"], [ak])
                        z, zk = zr()
                        self.tt("dve", z, a_, bb[dr][:, cb * 512:(cb + 1) * 512], ALU.add, [ak, "gbb"], [zk])
                        self.act(z, z, AF.Exp, [zk], [zk], scale=-1.0)
                        sp, spk = spr()
                        self.act(sp, z, AF.Ln, [zk, "one"], [spk], bias=one[:, 0:1])
                        self.dma("sp", SP[dr, row0 + t * 128:row0 + (t + 1) * 128, cb * 512:(cb + 1) * 512], sp, [spk], ["SPd"])
        self.end_phase()
        ident = m.alloc(128, BF16)
        self.dma("sp", ident, self.din["ident"], (), ["ident"])
        onb = m.alloc(512, F32)
        self.dma("sp", onb, self.din["gla_onorm"][j, :].partition_broadcast(128), (), ["onb"])
        mask = [m.alloc(128, F32) for _ in range(2)]
        tri = [m.alloc(128, F32) for _ in range(2)]
        for dr in range(2):
            self.dma("sp", mask[dr], self.din["gmask"][dr], (), ["gmask"])
            self.dma("sp", tri[dr], self.din["gtri"][dr], (), ["gtri"])
        Sf = m.alloc((8, 512), F32)
        Sb = m.alloc((8, 512), BF16)
        qr = self.ring("sq", 2, (8, 128), BF16)
        kr = self.ring("sk", 2, (8, 128), BF16)
        vr = self.ring("sv", 2, D, BF16)
        gr = self.ring("sg", 2, D, BF16)
        spr2 = self.ring("ssp", 2, 1024, F32)
        ofr = self.ring("sof", 2, D, F32)
        pmr = self.ring("spm", 2, D, BF16)
        ebr = self.ring("seb", 4, 128, F32)
        enr = self.ring("sen", 4, 128, F32)
        qtr = self.ring("sqt", 4, (2, 128), BF16)
        ktr = self.ring("skt", 4, (2, 128), BF16)
        khr = self.ring("skh", 4, 128, BF16)
        khm = self.ring("skhm", 2, 256, BF16)
        atr = self.ring("sat", 2, 128, BF16)
        ostr = self.ring("sos", 2, 512, F32)
        onr = self.ring("son", 2, 512, F32)
        sgr = self.ring("ssg", 2, 512, F32)
        s4r = self.ring("ss4", 2, 2, F32)
        junk = m.alloc(512, BF16)
        pb = self.psring("spb", [0, 1])
        ptr_ = self.psring("sptr", [2], bf16=True)
        pA = self.psring("spA", [3])
        pO = self.psring("spO", [4, 5])
        pS = self.psring("spS", [6, 7])
        QTv = QT.rearrange("(c p) t -> p c t", p=128)
        KTv = KT.rearrange("(c p) t -> p c t", p=128)
        for dr in range(2):
            self.memset("pool", Sf, 0.0, ["Sf"])
            self.memset("pool", Sb, 0.0, ["Sb"])
            if dr == 0:
                order = list(range(NT))
            else:
                order = [1, 0] + list(range(NT - 1, 1, -1))
            endc = 127 if dr == 0 else 0
            for tt_ in order:
                rows = slice(tt_ * 128, (tt_ + 1) * 128)
                q_, qk = qr()
                k_, kk = kr()
                v_, vk = vr()
                sp_, spk = spr2()
                self.dma("sp", q_, QTv[:, 0:8, rows], ["QK"], [qk])
                self.dma("sp", k_, KTv[:, 0:8, rows], ["QK"], [kk])
                self.dma("sp", v_, Vm[rows, :], ["VG"], [vk])
                self.dma("sp", sp_, SP[dr, rows, :], ["SPd"], [spk])
                if dr == 1:
                    g_, gk = gr()
                    of_, ofk = ofr()
                    self.dma("sp", g_, Gm[rows, :], ["VG"], [gk])
                    self.dma("sp", of_, OF[rows, :], ["OFd"], [ofk])
                    pm_, pmk = pmr()
                else:
                    of_, ofk = ofr()
                for h in range(4):
                    qt, qtk = qtr()
                    kt, ktk = ktr()
                    kh, khk = khm()
                    ebs = []
                    for dc in range(2):
                        c = h * 2 + dc
                        b_, bk = pb()
                        self.mm(b_[:, 0:128], sp_[:, c * 128:(c + 1) * 128], tri[dr], True, True, [spk, "gtri"], [bk])
                        eb, ebk = ebr()
                        en, enk = enr()
                        self.act(eb, b_[:, 0:128], AF.Exp, [bk], [ebk])
                        self.act(en, b_[:, 0:128], AF.Exp, [bk], [enk], scale=-1.0)
                        self.tt("dve", qt[:, dc, :], q_[:, c, :], eb, ALU.mult, [qk, ebk], [qtk])
                        self.tt("pool", kt[:, dc, :], k_[:, c, :], en, ALU.mult, [kk, enk], [ktk])
                        khT, khTk = khr()
                        self.ts("dve", khT, kt[:, dc, :], eb[:, endc:endc + 1], None, ALU.mult, None, [ktk, ebk], [khTk])
                        p_, pk = ptr_()
                        self.tr(p_[:, 0:128], khT, ident, [khTk, "ident"], [pk])
                        self.copy("act", kh[:, dc * 128:(dc + 1) * 128], p_[:, 0:128], [pk], [khk])
                        ebs.append((eb, ebk))
                    a_, ak = pA()
                    for dc in range(2):
                        self.mm(a_[:, 0:128], kt[:, dc, :], qt[:, dc, :], dc == 0, dc == 1, [ktk, qtk], [ak])
                    at, atk = atr()
                    self.tt("dve", at, a_[:, 0:128], mask[dr], ALU.mult, [ak, "gmask"], [atk])
                    o_, ok = pO()
                    self.mm(o_, at, v_[:, h * 512:(h + 1) * 512], True, False, [atk, vk], [ok])
                    for dc in range(2):
                        self.mm(o_, qt[:, dc, :], Sb[:, h * 2 + dc, :], False, dc == 1, [qtk, "Sb"], [ok])
                    for dc in range(2):
                        c = h * 2 + dc
                        s_, sk = pS()
                        self.mm(s_, kh[:, dc * 128:(dc + 1) * 128], v_[:, h * 512:(h + 1) * 512], True, True, [khk, vk], [sk])
                        eb, ebk = ebs[dc]
                        self.stt(Sf[:, c, :], Sf[:, c, :], eb[:, endc:endc + 1], s_, ALU.mult, ALU.add, ["Sf", ebk, sk, ok], ["Sf"])
                        self.copy("act", Sb[:, c, :], Sf[:, c, :], ["Sf"], ["Sb"])
                    if dr == 0:
                        self.copy("act", of_[:, h * 512:(h + 1) * 512], o_, [ok], [ofk])
                    else:
                        os_, osk = ostr()
                        self.tt("dve", os_, o_, of_[:, h * 512:(h + 1) * 512], ALU.add, [ok, ofk], [osk])
                        s4, s4k = s4r()
                        self.act(junk, os_, AF.Square, [osk], ["gjunk", s4k], accum_out=s4[:, 0:1])
                        self.ts("dve", s4[:, 0:1], s4[:, 0:1], 1.0 / 512, EPS, ALU.mult, ALU.add, [s4k], [s4k])
                        self.rsqrt(s4[:, 0:1], s4k)
                        on, onk = onr()
                        self.stt(on, os_, s4[:, 0:1], onb, ALU.mult, ALU.mult, [osk, s4k, "onb"], [onk])
                        sg, sgk = sgr()
                        self.act(sg, g_[:, h * 512:(h + 1) * 512], AF.Silu, [gk], [sgk])
                        self.tt("pool", pm_[:, h * 512:(h + 1) * 512], on, sg, ALU.mult, [onk, sgk], [pmk])
                if dr == 0:
                    self.dma("sp", OF[rows, :], of_, [ofk], ["OFd"])
                else:
                    self.dma("sp", self.PM[rows, :], pm_, [pmk], ["PM"])
        self.end_phase()

    def phase_moe(self, i, ctx_on):
        cfg = self.cfg
        m = self.mem
        E = cfg.E
        NTOK = cfg.NTOK
        LAT = cfg.LAT
        aff = m.alloc(NTOK, F32, parts=E)
        r = self.norm_setup(i, "ffn", ctx_on)
        hT = m.alloc((16, 1024), BF16)
        rw = m.alloc((16, E), BF16)
        self.load_w(rw, "rw", self.din["router_w"][i], E)
        ones = m.alloc(E, BF16, parts=E)
        self.memset("pool", ones, 1.0, ["ones"])
        ex = self.ring("mex", 2, 512, BF16, parts=E)
        exf = self.ring("mexf", 2, 512, F32, parts=E)
        rc = self.ring("mrc", 2, 512, F32, parts=E)
        pring = self.psring("mp", [0, 1])
        pring2 = self.psring("mp2", [2, 3])
        for blk in self.cur_blocks:
            row0, ntok, v = blk
            self.norm_block(r, blk, hT, "hT", hm_store=True)
            for m0 in range(0, ntok, 512):
                mw = min(512, ntok - m0)
                pt, pk = pring()
                for k in range(16):
                    self.mm(pt[0:E, 0:mw], rw[:, k, :], hT[:, k, m0:m0 + mw], k == 0, k == 15, ["rw", "hT"], [pk])
                ef, efk = exf()
                self.act(ef[:, 0:mw], pt[0:E, 0:mw], AF.Exp, [pk], [efk])
                e_, ek = ex()
                self.copy("dve", e_[:, 0:mw], ef[:, 0:mw], [efk], [ek])
                lo, lk = ex()
                self.tt("dve", lo[:, 0:mw], ef[:, 0:mw], e_[:, 0:mw], ALU.subtract, [efk, ek], [lk])
                p2, pk2 = pring2()
                self.mm(p2[0:E, 0:mw], ones, e_[:, 0:mw], True, False, ["ones", ek], [pk2])
                self.mm(p2[0:E, 0:mw], ones, lo[:, 0:mw], False, True, ["ones", lk], [pk2])
                rr, rk = rc()
                self.S.op("dve", lambda e, rr=rr, p2=p2, mw=mw: e.reciprocal(out=rr[:, 0:mw], in_=p2[0:E, 0:mw]), [pk2], [rk])
                self.tt("dve", aff[:, row0 + m0:row0 + m0 + mw], ef[:, 0:mw], rr[:, 0:mw], ALU.mult, [efk, rk], ["aff"])
        self.barrier()
        segs = [(0, CTX, LAT, cfg.cap_l)]
        if ctx_on:
            segs.append((1, 0, CTX, cfg.cap_c))
        res = {}
        for (sid, c0, n, cap) in segs:
            vals = m.alloc(cap, F32, parts=E)
            idx = m.alloc(cap, U32, parts=E)
            src = aff[:, c0:c0 + n]
            vk, ik = "tv%d" % sid, "ti%d" % sid
            for rr_ in range(cap // 8):
                sl = slice(rr_ * 8, rr_ * 8 + 8)
                self.S.op("dve", lambda e, vals=vals, sl=sl, src=src: e.max(out=vals[:, sl], in_=src), ["aff"], [vk])
                self.S.op("dve", lambda e, vals=vals, idx=idx, sl=sl, src=src: e.max_index(out=idx[:, sl], in_max=vals[:, sl], in_values=src),
                          ["aff", vk], [ik])
                self.S.op("dve", lambda e, vals=vals, sl=sl, src=src: e.match_replace(out=src, in_to_replace=vals[:, sl], in_values=src, imm_value=-1.0),
                          [vk, ik], ["aff"])
            self.dma("sp", self.gate_d[sid, :, 0:cap], vals, [vk], ["gate_d"])
            self.dma("sp", self.idx_d[sid, :, 0:cap], idx, [ik], ["idx_d"])
            res[sid] = (c0, n, cap)
        self.end_phase()
        res2 = {}
        for sid in sorted(res):
            c0, n, cap = res[sid]
            gs = min(128, cap)
            ng = cap // gs
            idxT = m.alloc((E, ng), U32)
            gateT = m.alloc((E, ng), F32)
            for e_ in range(E):
                self.dma("sp", idxT[0:gs, e_, :], self.idx_d[sid, e_, 0:cap].rearrange("(g p) -> p g", p=gs), (), ["idxT%d" % sid],
                         allow_slow_non_contiguous=True)
                self.dma("sp", gateT[0:gs, e_, :], self.gate_d[sid, e_, 0:cap].rearrange("(g p) -> p g", p=gs), (), ["gateT%d" % sid],
                         allow_slow_non_contiguous=True)
            res2[sid] = (idxT, gateT, gs, ng, c0, n)
        res = res2
        groups = []
        slot = 0
        for sid in sorted(res):
            idxT, gateT, gs, ng, c0, n = res[sid]
            for g in range(ng):
                groups.append((sid, g, gs, slot))
                slot += gs
        NS = slot
        ident = m.alloc(128, BF16)
        self.dma("sp", ident, self.din["ident"], (), ["ident"])
        r = {"ident": ident, "ptr": self.psring("mtr", [6, 7], bf16=True)}
        g2 = {0: self.load_gate_bc(i, "ffn", 0, "g2L")}
        if ctx_on:
            g2[1] = self.load_gate_bc(i, "ffn", 1, "g2C")
        XT = m.alloc((16, NS), BF16)
        zT = m.alloc((12, NS), BF16)
        FB = 256
        wgr = self.ring("wg", 2, (16, FB), BF16)
        wur = self.ring("wu", 2, (16, FB), BF16)
        wd = m.alloc((12, D), BF16)
        xgr = self.ring("xg", 2, D, BF16)
        ysr = self.ring("ys", 2, D, F32)
        sar = self.ring("sa", 2, 512, F32)
        pA = self.psring("pA", [0, 1])
        pU = self.psring("pU", [2, 3])
        pY = self.psring("pY", [4, 5])
        mtiles = []
        nl = cfg.cap_l
        for s0 in range(0, nl, 512):
            mtiles.append((s0, min(512, nl - s0)))
        if ctx_on:
            mtiles.append((nl, NS - nl))
        for e in range(E):
            for (sid, g, gs, s0) in groups:
                idxT, gateT, _, _, c0, n = res[sid]
                xg, xk = xgr()
                srcrows = self.Hm
                ia = idxT[0:gs, e, g:g + 1]
                self.S.dma_custom("pool", lambda en, xg=xg, gs=gs, srcrows=srcrows, ia=ia, c0=c0: en.indirect_dma_start(
                    out=xg[0:gs, :], out_offset=None, in_=srcrows, in_offset=bass.IndirectOffsetOnAxis(ap=ia, axis=0),
                    element_offset=c0 * D),
                    ["Hm", "idxT%d" % sid], [xk])
                self.transpose_tile(r, xg, xk, gs, XT, "XT", s0)
            wdv = self.din["exp_w_down"][i, e].rearrange("(c p) n -> p c n", p=128)
            for c0_ in range(0, 12, 3):
                self.S.dma("pool", wd[:, c0_:c0_ + 3, :], wdv[:, c0_:c0_ + 3, :], (), ["wd"])
            for fb in range(FF // FB):
                wg, wgk = wgr()
                wu, wuk = wur()
                self.load_w(wg, wgk, self.din["exp_w_gate"][i, e][:, fb * FB:(fb + 1) * FB], FB)
                self.load_w(wu, wuk, self.din["exp_w_up"][i, e][:, fb * FB:(fb + 1) * FB], FB)
                for c in range(FB // 128):
                    fc = fb * (FB // 128) + c
                    for (s0, w_) in mtiles:
                        a_, ak = pA()
                        u_, uk = pU()
                        for k in range(16):
                            self.mm(a_[:, 0:w_], wg[:, k, c * 128:(c + 1) * 128], XT[:, k, s0:s0 + w_], k == 0, k == 15, [wgk, "XT"], [ak])
                        for k in range(16):
                            self.mm(u_[:, 0:w_], wu[:, k, c * 128:(c + 1) * 128], XT[:, k, s0:s0 + w_], k == 0, k == 15, [wuk, "XT"], [uk])
                        sa, sk = sar()
                        self.act(sa[:, 0:w_], a_[:, 0:w_], AF.Silu, [ak], [sk])
                        self.tt("dve", zT[:, fc, s0:s0 + w_], sa[:, 0:w_], u_[:, 0:w_], ALU.mult, [sk, uk], ["zT"])
            for (sid, g, gs, s0) in groups:
                idxT, gateT, _, _, c0, n = res[sid]
                gbc, gbk = g2[sid]
                ys, yk = ysr()
                for db in range(4):
                    y_, ypk = pY()
                    for c in range(12):
                        self.mm(y_[0:gs, :], zT[:, c, s0:s0 + gs], wd[:, c, db * 512:(db + 1) * 512], c == 0, c == 11, ["zT", "wd"], [ypk])
                    self.stt(ys[0:gs, db * 512:(db + 1) * 512], y_[0:gs, :], gateT[0:gs, e, g:g + 1], gbc[0:gs, db * 512:(db + 1) * 512],
                             ALU.mult, ALU.mult, [ypk, "gateT%d" % sid, gbk], [yk])
                dst = self.X
                ia = idxT[0:gs, e, g:g + 1]
                self.S.dma_custom("pool", lambda en, ys=ys, gs=gs, dst=dst, ia=ia, c0=c0: en.indirect_dma_start(
                    out=dst, out_offset=bass.IndirectOffsetOnAxis(ap=ia, axis=0), in_=ys[0:gs, :], in_offset=None, compute_op=ALU.add,
                    element_offset=c0 * D),
                    [yk, "idxT%d" % sid], ["X"])
        self.end_phase()

    def phase_final(self):
        m = self.mem
        g = m.alloc(D, F32)
        self.dma("sp", g, self.din["final_norm"][0, :].partition_broadcast(128), (), ["fg"])
        xring = self.ring("fx", 2, D, F32)
        oring = self.ring("fo", 2, D, F32)
        ss = self.ring("fss", 2, 1, F32)
        junk = m.alloc(D, BF16)
        for t in range(self.cfg.LAT // 128):
            xs, xk = xring()
            self.dma("sp", xs, self.X[CTX + t * 128:CTX + (t + 1) * 128, :], ["X"], [xk])
            s, sk = ss()
            self.act(junk, xs, AF.Square, [xk], ["fjunk", sk], accum_out=s)
            self.ts("dve", s, s, 1.0 / D, EPS, ALU.mult, ALU.add, [sk], [sk])
            self.rsqrt(s, sk)
            o, ok = oring()
            self.stt(o, xs, s[:, 0:1], g, ALU.mult, ALU.mult, [xk, sk, "fg"], [ok])
            self.dma("sp", self.out[t * 128:(t + 1) * 128, :], o, [ok], ["out"])


def prep_inputs(inp, cfg):
    f32 = np.float32
    d = {}
    d["xin"] = np.ascontiguousarray(inp["x"][0], dtype=f32)
    d["ctxin"] = np.ascontiguousarray(inp["ctx"][0], dtype=f32)
    cl = np.asarray(inp["c"][0], f32).reshape(16, 128).T
    cc = np.asarray(inp["c_ctx"], f32).reshape(16, 128).T
    d["ccols"] = np.ascontiguousarray(np.stack([cl, cc], axis=2).reshape(128, 32))
    for k in ("mod_w", "mod_b", "norm_mix", "norm_ffn", "router_w", "exp_w_gate", "exp_w_up", "exp_w_down", "conv_w_out"):
        d[k] = np.ascontiguousarray(inp[k], dtype=f32)
    d["final_norm"] = np.asarray(inp["final_norm"], f32).reshape(1, D)
    w = np.asarray(inp["conv_w_in"], f32)
    nA = w.shape[0]
    parts = [w[:, :, s * D:(s + 1) * D].reshape(nA, D, 16, 1, 128) for s in range(3)]
    d["conv_w_in_r"] = np.ascontiguousarray(np.concatenate(parts, axis=3).reshape(nA, D, 3 * D))
    dw = np.asarray(inp["conv_w_dw"], f32).reshape(nA, 3, 16, 128)
    d["conv_dw_cols"] = np.ascontiguousarray(dw.transpose(0, 3, 1, 2).reshape(nA, 128, 48))
    d["ident"] = np.eye(128, dtype=f32).astype(ml_dtypes.bfloat16)
    if cfg.nB:
        d["diff_w_qkv"] = np.ascontiguousarray(inp["diff_w_qkv"], dtype=f32)
        d["diff_lambda"] = np.asarray(inp["diff_lambda"], f32).reshape(cfg.nB, 1, 512)
        d["diff_subln"] = np.asarray(inp["diff_subln"], f32)
        d["diff_w_out"] = np.ascontiguousarray(inp["diff_w_out"], dtype=f32)
        n = np.arange(cfg.LAT)
        pos = [n // 64, n % 64]
        inv = 10000.0 ** (-np.arange(32, dtype=np.float64) / 32)
        C = np.ones((128, cfg.NTOK), np.float64)
        Sn = np.zeros((128, cfg.NTOK), np.float64)
        P = np.zeros((128, 128), np.float64)
        for dd in range(128):
            sct, within = dd // 64, dd % 64
            ang = pos[sct].astype(np.float64) * inv[within % 32]
            ang = (pos[sct].astype(np.float32) * inv[within % 32].astype(np.float32)).astype(np.float64)
            C[dd, CTX:] = np.cos(ang)
            Sn[dd, CTX:] = np.sin(ang)
            if within < 32:
                P[dd + 32, dd] = -1.0
            else:
                P[dd - 32, dd] = 1.0
        d["ropeC"] = C.astype(f32)
        d["ropeS"] = Sn.astype(f32)
        d["ropeP"] = P.astype(f32).astype(ml_dtypes.bfloat16)
    if cfg.nC:
        for k in ("gla_w_in", "gla_gate_w1", "gla_gate_w2", "gla_gate_b", "gla_onorm", "gla_w_out"):
            d[k] = np.ascontiguousarray(inp[k], dtype=f32)
        jj, ii = np.meshgrid(np.arange(128), np.arange(128), indexing="ij")
        mk = np.stack([(jj <= ii), (jj >= ii)]).astype(f32)
        d["gmask"] = mk
        d["gtri"] = (mk * (-1.0 / 16.0)).astype(f32)
    return d


def kernel(**inputs):
    cfg = Cfg()
    prog = Prog(cfg)
    nc = prog.build()
    d = prep_inputs(inputs, cfg)
    d = {k: v for k, v in d.items() if k in prog.din}
    res = run_bass_kernel_spmd(nc, [d], core_ids=[0])
    out = np.asarray(res.results[0]["out"], dtype=np.float32)
    return out.reshape(1, cfg.LAT, D)
```

```python
import numpy as np
import concourse.bass as bass
import concourse.mybir as mybir
from concourse.bass_utils import run_bass_kernel_spmd

F32 = mybir.dt.float32
BF16 = mybir.dt.bfloat16
I32 = mybir.dt.int32
U32 = mybir.dt.uint32
AF = mybir.ActivationFunctionType
ALU = mybir.AluOpType
AX = mybir.AxisListType


class Sched:
    ENG = ("pe", "act", "dve", "pool", "sp")

    def __init__(self, nc, es, n_dma_sems=12, same_engine_sync=True):
        self.nc = nc
        self.same = same_engine_sync
        self.thunks = {e: [] for e in self.ENG}
        self.sem = {e: es.enter_context(nc.semaphore("c_" + e)) for e in self.ENG}
        self.cnt = {e: 0 for e in self.ENG}
        self.seen = {e: {} for e in self.ENG}
        self.dsems = {}
        for q in ("sp", "pool", "act"):
            self.dsems[q] = [
                [es.enter_context(nc.semaphore("d_%s%d" % (q, i))), 0] for i in range(n_dma_sems)
            ]
        self.dnext = {q: 0 for q in self.dsems}
        self.lastw = {}
        self.reads = {}
        self.n_wait = 0

    def _need(self, eng, evs):
        best = {}
        for ev in evs:
            if ev is None:
                continue
            sem, name, val = ev
            if name == "c_" + eng and not self.same:
                continue
            if name == "c_pe" and eng == "pe":
                continue
            if self.seen[eng].get(name, 0) >= val:
                continue
            if name not in best or best[name][1] < val:
                best[name] = (sem, val)
        for name, (sem, val) in best.items():
            self.seen[eng][name] = val
            self.thunks[eng].append(lambda e, sem=sem, val=val: e.wait_ge(sem, val))
            self.n_wait += 1

    def _deps(self, reads, writes):
        evs = []
        for k in reads:
            evs.append(self.lastw.get(k))
        for k in writes:
            evs.append(self.lastw.get(k))
            evs.extend(self.reads.get(k, ()))
        return evs

    def _commit(self, ev, reads, writes):
        for k in reads:
            self.reads.setdefault(k, []).append(ev)
        for k in writes:
            self.lastw[k] = ev
            self.reads[k] = []

    def op(self, eng, fn, reads=(), writes=()):
        self._need(eng, self._deps(reads, writes))
        self.cnt[eng] += 1
        sem = self.sem[eng]
        self.thunks[eng].append(lambda e, fn=fn, sem=sem: fn(e).then_inc(sem, 1))
        ev = (sem, "c_" + eng, self.cnt[eng])
        self._commit(ev, reads, writes)
        return ev

    def dma(self, q, out, in_, reads=(), writes=(), **kw):
        slot = self.dsems[q][self.dnext[q] % len(self.dsems[q])]
        self.dnext[q] += 1
        sem, val = slot
        name = sem.name if hasattr(sem, "name") else str(id(sem))
        name = "d_%s_%d" % (q, (self.dnext[q] - 1) % len(self.dsems[q]))
        evs = self._deps(reads, writes)
        if val > 0:
            evs.append((sem, name, val))
        self._need(q, evs)
        slot[1] = val + 16
        self.thunks[q].append(
            lambda e, out=out, in_=in_, sem=sem, kw=kw: e.dma_start(out=out, in_=in_, **kw).then_inc(sem, 16)
        )
        ev = (sem, name, val + 16)
        self._commit(ev, reads, writes)
        return ev

    def dma_custom(self, q, fn, reads=(), writes=()):
        slot = self.dsems[q][self.dnext[q] % len(self.dsems[q])]
        name = "d_%s_%d" % (q, self.dnext[q] % len(self.dsems[q]))
        self.dnext[q] += 1
        sem, val = slot
        evs = self._deps(reads, writes)
        if val > 0:
            evs.append((sem, name, val))
        self._need(q, evs)
        slot[1] = val + 16
        self.thunks[q].append(lambda e, fn=fn, sem=sem: fn(e).then_inc(sem, 16))
        ev = (sem, name, val + 16)
        self._commit(ev, reads, writes)
        return ev

    def wait_all(self, eng):
        evs = []
        for k, ev in self.lastw.items():
            evs.append(ev)
        for k, l in self.reads.items():
            evs.extend(l)
        self._need(eng, evs)

    def emit(self):
        nc = self.nc
        with nc.Block() as block:
            @block.tensor
            def _(e):
                for t in self.thunks["pe"]:
                    t(e)

            @block.scalar
            def _(e):
                for t in self.thunks["act"]:
                    t(e)

            @block.vector
            def _(e):
                for t in self.thunks["dve"]:
                    t(e)

            @block.gpsimd
            def _(e):
                for t in self.thunks["pool"]:
                    t(e)

            @block.sync
            def _(e):
                for t in self.thunks["sp"]:
                    t(e)
        self.thunks = {e: [] for e in self.ENG}


import contextlib
import math
import numpy as np
import ml_dtypes

D = 2048
CTX = 256
FF = 1536
EPS = 1e-6


def prod(t):
    r = 1
    for a in t:
        r *= a
    return r


class Mem:
    def __init__(self, prog):
        self.prog = prog

    def alloc(self, free, dt, parts=128):
        if isinstance(free, int):
            free = (free,)
        n = prod(free)
        p = self.prog
        p.uid += 1
        t = p.pes.enter_context(p.nc.sbuf_tensor("b%d" % p.uid, [128, n], dt))
        v = t[:parts, :]
        if len(free) == 2:
            v = v.rearrange("p (a b) -> p a b", a=free[0])
        elif len(free) == 3:
            v = v.rearrange("p (a b c) -> p a b c", a=free[0], b=free[1])
        return v


class Cfg:
    def __init__(self, LAT=8192, E=16, DEPTH=4, CF=2):
        self.LAT = LAT
        self.E = E
        self.DEPTH = DEPTH
        self.NTOK = CTX + LAT
        self.cap_l = CF * LAT // E
        self.cap_c = CF * CTX // E
        self.nA = sum(1 for i in range(DEPTH) if i % 3 == 0)
        self.nB = sum(1 for i in range(DEPTH) if i % 3 == 1)
        self.nC = sum(1 for i in range(DEPTH) if i % 3 == 2)


class Prog:
    def __init__(self, cfg):
        self.cfg = cfg
        self.nc = bass.Bass("TRN2", target_bir_lowering=False)
        self.din = {}
        self.uid = 0

    def inp(self, name, shape, dt=F32):
        self.din[name] = self.nc.dram_tensor(name, list(shape), dt, kind="ExternalInput").ap()
        return self.din[name]

    def scratch(self, name, shape, dt):
        return self.nc.dram_tensor(name, list(shape), dt, kind="Internal").ap()

    def barrier(self):
        S = self.S
        evs = []
        for k, ev in S.lastw.items():
            evs.append(ev)
        for k, l in S.reads.items():
            evs.extend(l)
        for e in S.ENG:
            S._need(e, evs)
        S.lastw.clear()
        S.reads.clear()

    def begin_phase(self):
        self.pes = contextlib.ExitStack()
        self.nphase = getattr(self, "nphase", 0) + 1
        self.ps = [self.pes.enter_context(self.nc.psum_tensor("ps%d_%d" % (self.nphase, i), [128, 512], F32)) for i in range(8)]

    def end_phase(self, last=False):
        self.barrier()
        self.S.emit()
        self.pes.close()
        if not last:
            self.begin_phase()

    def act(self, out, in_, func, reads, writes, **kw):
        self.S.op("act", lambda e: e.activation(out=out, in_=in_, func=func, **kw), reads, writes)

    def ts(self, eng, out, in0, s1, s2, op0, op1, reads, writes, **kw):
        if op1 is None:
            self.S.op(eng, lambda e: e.tensor_scalar(out=out, in0=in0, scalar1=s1, scalar2=None, op0=op0, **kw), reads, writes)
        else:
            self.S.op(eng, lambda e: e.tensor_scalar(out=out, in0=in0, scalar1=s1, scalar2=s2, op0=op0, op1=op1, **kw), reads, writes)

    def tt(self, eng, out, in0, in1, op, reads, writes):
        self.S.op(eng, lambda e: e.tensor_tensor(out=out, in0=in0, in1=in1, op=op), reads, writes)

    def stt(self, out, in0, scalar, in1, op0, op1, reads, writes):
        self.S.op("dve", lambda e: e.scalar_tensor_tensor(out=out, in0=in0, scalar=scalar, in1=in1, op0=op0, op1=op1), reads, writes)

    def rsqrt(self, ap, key):
        self.S.op("act", lambda e: e.activation(out=ap, in_=ap, func=AF.Sqrt), [key], [key])
        self.S.op("dve", lambda e: e.reciprocal(out=ap, in_=ap), [key], [key])

    def copy(self, eng, out, in_, reads, writes):
        if eng == "act":
            self.S.op("act", lambda e: e.activation(out=out, in_=in_, func=AF.Copy), reads, writes)
        else:
            self.S.op(eng, lambda e: e.tensor_copy(out=out, in_=in_), reads, writes)

    def memset(self, eng, out, val, writes):
        self.S.op(eng, lambda e: e.memset(out, val), (), writes)

    def mm(self, out, lhsT, rhs, start, stop, reads, writes):
        self.S.op("pe", lambda e: e.matmul(out, lhsT=lhsT, rhs=rhs, start=start, stop=stop), reads, writes)

    def tr(self, out, in_, ident, reads, writes):
        self.S.op("pe", lambda e: e.transpose(out=out, in_=in_, identity=ident), reads, writes)

    def dma(self, q, out, in_, reads, writes, **kw):
        self.S.dma(q, out, in_, reads, writes, **kw)

    def ring(self, name, n, free, dt, parts=128):
        tiles = [self.mem.alloc(free, dt, parts) for _ in range(n)]
        st = {"i": 0}

        def nxt():
            i = st["i"] % n
            st["i"] += 1
            return tiles[i], (name, i)
        return nxt

    def psring(self, name, idxs, bf16=False):
        st = {"i": 0}

        def nxt():
            i = idxs[st["i"] % len(idxs)]
            st["i"] += 1
            t = self.ps[i][:]
            if bf16:
                t = t.bitcast(BF16)
            return t, ("ps", i)
        return nxt

    def load_w(self, dst, key, w_ap, ncols):
        kc = w_ap.shape[0] // 128
        src = w_ap.rearrange("(k p) n -> p k n", p=128)
        step = max(1, kc // 4)
        for k0 in range(0, kc, step):
            k1 = min(kc, k0 + step)
            self.S.dma("pool", dst[:, k0:k1, 0:ncols], src[:, k0:k1, :], (), [key])

    def build(self):
        cfg = self.cfg
        nc = self.nc
        LAT, E, DEPTH, NTOK = cfg.LAT, cfg.E, cfg.DEPTH, cfg.NTOK
        inp = self.inp
        inp("xin", [LAT, D])
        inp("ctxin", [CTX, D])
        inp("ccols", [128, 32])
        inp("mod_w", [DEPTH, D, 6 * D])
        inp("mod_b", [DEPTH, 6 * D])
        inp("norm_mix", [DEPTH, D])
        inp("norm_ffn", [DEPTH, D])
        inp("final_norm", [1, D])
        inp("conv_w_in_r", [cfg.nA, D, 3 * D])
        inp("conv_dw_cols", [cfg.nA, 128, 48])
        inp("conv_w_out", [cfg.nA, D, D])
        inp("router_w", [DEPTH, D, E])
        inp("exp_w_gate", [DEPTH, E, D, FF])
        inp("exp_w_up", [DEPTH, E, D, FF])
        inp("exp_w_down", [DEPTH, E, FF, D])
        inp("ident", [128, 128], BF16)
        if cfg.nB:
            inp("diff_w_qkv", [cfg.nB, D, 3 * D])
            inp("diff_lambda", [cfg.nB, 1, 512])
            inp("diff_subln", [cfg.nB, 256])
            inp("diff_w_out", [cfg.nB, D, D])
            inp("ropeC", [128, NTOK])
            inp("ropeS", [128, NTOK])
            inp("ropeP", [128, 128], BF16)
            self.PM = self.scratch("PM", [NTOK, D], BF16)
        if cfg.nC:
            inp("gla_w_in", [cfg.nC, D, 3 * D])
            inp("gla_gate_w1", [cfg.nC, 2, D, 16])
            inp("gla_gate_w2", [cfg.nC, 2, 16, 1024])
            inp("gla_gate_b", [cfg.nC, 2, 1024])
            inp("gla_onorm", [cfg.nC, 512])
            inp("gla_w_out", [cfg.nC, D, D])
            inp("gmask", [2, 128, 128])
            inp("gtri", [2, 128, 128])
            self.Gm = self.scratch("Gm", [NTOK, D], BF16)
            self.SP = self.scratch("SP", [2, NTOK, 1024], F32)
            self.OF = self.scratch("OF", [NTOK, D], F32)
            if not cfg.nB:
                self.PM = self.scratch("PM", [NTOK, D], BF16)
        self.out = nc.dram_tensor("out", [LAT, D], F32, kind="ExternalOutput").ap()
        self.X = self.scratch("X", [NTOK, D], F32)
        self.modv = self.scratch("modv", [DEPTH, 2, 6 * D], F32)
        self.Hm = self.scratch("Hm", [NTOK, D], BF16)
        self.VT = self.scratch("VT", [D, NTOK], BF16)
        self.BT = self.scratch("BT", [D, NTOK], BF16)
        self.PT = self.scratch("PT", [D, NTOK], BF16)
        self.idx_d = self.scratch("idx_d", [2, E, max(cfg.cap_l, 128)], U32)
        self.gate_d = self.scratch("gate_d", [2, E, max(cfg.cap_l, 128)], F32)
        with contextlib.ExitStack() as es:
            self.S = Sched(nc, es)
            self.mem = Mem(self)
            self.begin_phase()
            self.body()
            self.end_phase(last=True)
        return nc

    def body(self):
        cfg = self.cfg
        self.setup()
        for i in range(cfg.DEPTH):
            kind, j = i % 3, i // 3
            ctx_on = i < cfg.DEPTH - 1
            self.cur_blocks = self.blocks(ctx_on)
            self.phase_mod(i)
            if kind == 0:
                self.phase_conv(i, j, ctx_on)
                self.phase_outproj(i, self.PT, True, self.din["conv_w_out"][j], ctx_on)
            elif kind == 1:
                self.phase_attn(i, j, ctx_on)
                self.phase_outproj(i, self.PM, False, self.din["diff_w_out"][j], ctx_on)
            else:
                self.phase_gla(i, j, ctx_on)
                self.phase_outproj(i, self.PM, False, self.din["gla_w_out"][j], ctx_on)
            self.phase_moe(i, ctx_on)
        self.phase_final()

    def blocks(self, ctx_on):
        b = []
        if ctx_on:
            b.append((0, CTX, 1))
        for r in range(0, self.cfg.LAT, 1024):
            b.append((CTX + r, min(1024, self.cfg.LAT - r), 0))
        return b

    def setup(self):
        self.dma("sp", self.X[0:CTX, :], self.din["ctxin"], (), ["X"])
        LAT = self.cfg.LAT
        for r in range(0, LAT, 1024):
            self.dma("sp", self.X[CTX + r:CTX + r + 1024, :], self.din["xin"][r:r + 1024, :], (), ["X"])
        z = self.mem.alloc(D, BF16)
        self.memset("pool", z, 0.0, ["z"])
        for t in range(CTX // 128):
            self.dma("sp", self.Hm[t * 128:(t + 1) * 128, :], z, ["z"], ["Hm"])
        self.end_phase()

    def phase_mod(self, i):
        m = self.mem
        cc = m.alloc(32, F32)
        sc = m.alloc((16, 2), BF16)
        bias = m.alloc(6 * D, F32, parts=2)
        res = m.alloc(6 * D, F32, parts=2)
        wring = self.ring("modw", 2, (16, 512), BF16)
        pring = self.psring("modp", [0, 1])
        self.dma("sp", cc, self.din["ccols"], (), ["cc"])
        self.act(sc.rearrange("p k v -> p (k v)"), cc, AF.Silu, ["cc"], ["sc"])
        for v in range(2):
            self.dma("sp", bias[v:v + 1, :], self.din["mod_b"][i:i + 1, :], (), ["mbias"])
        for cb in range(6 * D // 512):
            wt, wk = wring()
            self.load_w(wt, wk, self.din["mod_w"][i][:, cb * 512:(cb + 1) * 512], 512)
            pt, pk = pring()
            for k in range(16):
                self.mm(pt[0:2, :], sc[:, k, :], wt[:, k, :], k == 0, k == 15, ["sc", wk], [pk])
            self.tt("dve", res[:, cb * 512:(cb + 1) * 512], pt[0:2, :], bias[:, cb * 512:(cb + 1) * 512], ALU.add,
                    [pk, "mbias"], ["mres"])
        self.dma("sp", self.modv[i], res, ["mres"], ["modv"])
        self.end_phase()

    def load_mod_bc(self, i, which, v, tag):
        m = self.mem
        a_sh, a_sc = (0, 1) if which == "mix" else (3, 4)
        gs = m.alloc(D, F32)
        sh = m.alloc(D, F32)
        tmp = m.alloc(D, F32)
        norm = self.din["norm_mix" if which == "mix" else "norm_ffn"]
        self.dma("sp", gs, self.modv[i, v, a_sc * D:(a_sc + 1) * D].partition_broadcast(128), ["modv"], [tag + "gs"])
        self.dma("sp", tmp, norm[i, :].partition_broadcast(128), (), [tag + "tmp"])
        self.dma("sp", sh, self.modv[i, v, a_sh * D:(a_sh + 1) * D].partition_broadcast(128), ["modv"], [tag + "sh"])
        self.stt(gs, gs, 1.0, tmp, ALU.add, ALU.mult, [tag + "gs", tag + "tmp"], [tag + "gs"])
        return gs, sh, tag + "gs", tag + "sh"

    def load_gate_bc(self, i, which, v, tag):
        a = 2 if which == "mix" else 5
        g = self.mem.alloc(D, F32)
        self.dma("sp", g, self.modv[i, v, a * D:(a + 1) * D].partition_broadcast(128), ["modv"], [tag])
        return g, tag

    def norm_setup(self, i, which, ctx_on):
        m = self.mem
        r = {}
        r["ident"] = m.alloc(128, BF16)
        self.dma("sp", r["ident"], self.din["ident"], (), ["ident"])
        r["mod"] = {0: self.load_mod_bc(i, which, 0, "mL")}
        if ctx_on:
            r["mod"][1] = self.load_mod_bc(i, which, 1, "mC")
        r["xring"] = self.ring("nx", 2, D, F32)
        r["tring"] = self.ring("ntmp", 2, D, F32)
        r["hring"] = self.ring("nh", 2, D, BF16)
        r["ss"] = self.ring("nss", 2, 1, F32)
        r["rs"] = self.ring("nrs", 2, 1, F32)
        r["junk"] = m.alloc(D, BF16)
        r["ptr"] = self.psring("ntr", [6, 7], bf16=True)
        return r

    def norm_block(self, r, blk, hT, hkey, hm_store=False):
        row0, ntok, v = blk
        gs, sh, gsk, shk = r["mod"][v]
        for t in range(ntok // 128):
            xs, xk = r["xring"]()
            self.dma("sp", xs, self.X[row0 + t * 128:row0 + (t + 1) * 128, :], ["X"], [xk])
            ss, sk = r["ss"]()
            rs, rk = r["rs"]()
            self.act(r["junk"], xs, AF.Square, [xk], ["njunk", sk], accum_out=ss)
            self.ts("dve", rs, ss, 1.0 / D, EPS, ALU.mult, ALU.add, [sk], [rk])
            self.rsqrt(rs, rk)
            tmp, tk = r["tring"]()
            self.stt(tmp, xs, rs[:, 0:1], gs, ALU.mult, ALU.mult, [xk, rk, gsk], [tk])
            hb, hk = r["hring"]()
            self.tt("pool", hb, tmp, sh, ALU.add, [tk, shk], [hk])
            if hm_store:
                self.dma("sp", self.Hm[row0 + t * 128:row0 + (t + 1) * 128, :], hb, [hk], ["Hm"])
            self.transpose_tile(r, hb, hk, 128, hT, hkey, t * 128)

    def transpose_tile(self, r, hb, hk, nrow, hT, hkey, col0):
        for g in range(2):
            pt, pk = r["ptr"]()
            for kk in range(8):
                k = g * 8 + kk
                self.tr(pt[:, kk * 128:kk * 128 + nrow], hb[0:nrow, k * 128:(k + 1) * 128], r["ident"][0:nrow, 0:nrow],
                        [hk, "ident"], [pk])
            src = pt.rearrange("p (k t) -> p k t", k=8)[:, :, 0:nrow]
            self.copy("act" if g == 0 else "dve", hT[:, g * 8:(g + 1) * 8, col0:col0 + nrow], src, [pk], [hkey])

    def phase_conv(self, i, j, ctx_on):
        m = self.mem
        r = self.norm_setup(i, "mix", ctx_on)
        hT = m.alloc((16, 1024), BF16)
        wring = self.ring("cw", 2, (16, 384), BF16)
        pring = self.psring("cp", [0, 1, 2, 3, 4, 5])
        ub = self.ring("cub", 2, 512, F32)
        vst = self.ring("cvst", 2, 512, BF16)
        bst = self.ring("cbst", 2, 512, BF16)
        win = self.din["conv_w_in_r"][j]
        for blk in self.cur_blocks:
            row0, ntok, v = blk
            self.norm_block(r, blk, hT, "hT")
            for n in range(16):
                wt, wk = wring()
                self.load_w(wt, wk, win[:, n * 384:(n + 1) * 384], 384)
                for m0 in range(0, ntok, 512):
                    mw = min(512, ntok - m0)
                    pB, kB = pring()
                    pC, kC = pring()
                    pU, kU = pring()
                    for (pp, pk, c0) in ((pB, kB, 0), (pC, kC, 128), (pU, kU, 256)):
                        for k in range(16):
                            self.mm(pp[:, 0:mw], wt[:, k, c0:c0 + 128], hT[:, k, m0:m0 + mw], k == 0, k == 15,
                                    [wk, "hT"], [pk])
                    u, uk = ub()
                    self.copy("act", u[:, 0:mw], pU[:, 0:mw], [kU], [uk])
                    vs, vk = vst()
                    self.tt("dve", vs[:, 0:mw], pC[:, 0:mw], u[:, 0:mw], ALU.mult, [kC, uk], [vk])
                    bs, bk = bst()
                    self.copy("act", bs[:, 0:mw], pB[:, 0:mw], [kB], [bk])
                    self.dma("sp", self.VT[n * 128:(n + 1) * 128, row0 + m0:row0 + m0 + mw], vs[:, 0:mw], [vk], ["VT"])
                    self.dma("sp", self.BT[n * 128:(n + 1) * 128, row0 + m0:row0 + m0 + mw], bs[:, 0:mw], [bk], ["BT"])
        self.end_phase()
        dw = m.alloc(48, F32)
        self.dma("sp", dw, self.din["conv_dw_cols"][j], (), ["dw"])
        PW = 2048
        vring = self.ring("c2v", 2, PW + 2, BF16)
        bring = self.ring("c2b", 2, PW, BF16)
        aring = self.ring("c2a", 2, PW, F32)
        gring = self.ring("c2g", 2, PW, BF16)
        segs = []
        if ctx_on:
            segs.append((0, CTX))
        segs.append((CTX, self.cfg.NTOK))
        for n in range(16):
            for (s0, s1) in segs:
                for t0 in range(s0, s1, PW):
                    w = min(PW, s1 - t0)
                    vp, vk = vring()
                    lo = t0 - 1
                    hi = t0 + w + 1
                    c0 = 0
                    if lo < s0:
                        self.memset("pool", vp[:, 0:1], 0.0, [vk])
                        lo = s0
                        c0 = 1
                    if hi > s1:
                        self.memset("pool", vp[:, w + 1:w + 2], 0.0, [vk])
                        hi = s1
                    self.dma("sp", vp[:, c0:c0 + hi - lo], self.VT[n * 128:(n + 1) * 128, lo:hi], ["VT"], [vk])
                    bp, bk = bring()
                    self.dma("sp", bp[:, 0:w], self.BT[n * 128:(n + 1) * 128, t0:t0 + w], ["BT"], [bk])
                    ac, ak = aring()
                    self.ts("dve", ac[:, 0:w], vp[:, 1:w + 1], dw[:, 16 + n:17 + n], None, ALU.mult, None, [vk, "dw"], [ak])
                    self.stt(ac[:, 0:w], vp[:, 0:w], dw[:, n:n + 1], ac[:, 0:w], ALU.mult, ALU.add, [vk, "dw", ak], [ak])
                    self.stt(ac[:, 0:w], vp[:, 2:w + 2], dw[:, 32 + n:33 + n], ac[:, 0:w], ALU.mult, ALU.add, [vk, "dw", ak], [ak])
                    gp, gk = gring()
                    self.tt("pool", gp[:, 0:w], ac[:, 0:w], bp[:, 0:w], ALU.mult, [ak, bk], [gk])
                    self.dma("sp", self.PT[n * 128:(n + 1) * 128, t0:t0 + w], gp[:, 0:w], [gk], ["PT"])
        self.end_phase()

    def phase_outproj(self, i, src, src_fm, w_ap, ctx_on):
        m = self.mem
        w = m.alloc((16, D), BF16)
        self.load_w(w, "ow", w_ap, D)
        gate = {0: self.load_gate_bc(i, "mix", 0, "ogL")}
        if ctx_on:
            gate[1] = self.load_gate_bc(i, "mix", 1, "ogC")
        aT = m.alloc((16, 1024), BF16)
        xring = self.ring("ox", 2, D, F32)
        tring = self.ring("ot", 2, 512, F32)
        pring = self.psring("op", [0, 1, 2, 3])
        if not src_fm:
            ident = m.alloc(128, BF16)
            self.dma("sp", ident, self.din["ident"], (), ["ident"])
            r = {"ident": ident, "ptr": self.psring("otr", [6, 7], bf16=True)}
            hring = self.ring("oh", 2, D, BF16)
        for blk in self.cur_blocks:
            row0, ntok, v = blk
            g, gk = gate[v]
            if src_fm:
                srcv = src.rearrange("(k p) t -> p k t", p=128)
                for k0 in range(0, 16, 4):
                    self.dma("sp", aT[:, k0:k0 + 4, 0:ntok], srcv[:, k0:k0 + 4, row0:row0 + ntok], ["PT"], ["aT"])
            else:
                for t in range(ntok // 128):
                    hb, hk = hring()
                    self.dma("sp", hb, src[row0 + t * 128:row0 + (t + 1) * 128, :], ["PM"], [hk])
                    self.transpose_tile(r, hb, hk, 128, aT, "aT", t * 128)
            for t in range(ntok // 128):
                xs, xk = xring()
                self.dma("sp", xs, self.X[row0 + t * 128:row0 + (t + 1) * 128, :], ["X"], [xk])
                for cb in range(4):
                    pt, pk = pring()
                    for k in range(16):
                        self.mm(pt, aT[:, k, t * 128:(t + 1) * 128], w[:, k, cb * 512:(cb + 1) * 512], k == 0, k == 15,
                                ["aT", "ow"], [pk])
                    tmp, tk = tring()
                    self.tt("dve", tmp, pt, g[:, cb * 512:(cb + 1) * 512], ALU.mult, [pk, gk], [tk])
                    self.tt("pool", xs[:, cb * 512:(cb + 1) * 512], xs[:, cb * 512:(cb + 1) * 512], tmp, ALU.add, [xk, tk], [xk])
                self.dma("sp", self.X[row0 + t * 128:row0 + (t + 1) * 128, :], xs, [xk], ["X"])
        self.end_phase()


    def phase_attn(self, i, j, ctx_on):
        cfg = self.cfg
        m = self.mem
        NTOK = cfg.NTOK
        NT = NTOK // 128
        QT, KT, Vm = self.VT, self.BT, self.Hm
        wqkv = self.din["diff_w_qkv"][j]
        r = self.norm_setup(i, "mix", True)
        hT = m.alloc((16, 1024), BF16)
        Pm = m.alloc(128, BF16)
        self.dma("sp", Pm, self.din["ropeP"], (), ["Pm"])
        cring = self.ring("rc", 2, 512, F32)
        sring = self.ring("rs", 2, 512, F32)
        wring = self.ring("aw", 2, (16, 512), BF16)
        xbr = self.ring("axb", 2, 512, BF16)
        t1r = self.ring("at1", 2, 512, F32)
        t2r = self.ring("at2", 2, 512, F32)
        ror = self.ring("aro", 2, 512, BF16)
        vsr = self.ring("avs", 2, 512, BF16)
        pa = self.psring("apa", [0, 1])
        pp = self.psring("app", [2, 3])
        pv = self.psring("apv", [4, 5])
        for blk in self.blocks(True):
            row0, ntok, v = blk
            self.norm_block(r, blk, hT, "hT")
            for wb in range(8):
                wt, wk = wring()
                self.load_w(wt, wk, wqkv[:, wb * 512:(wb + 1) * 512], 512)
                for m0 in range(0, ntok, 512):
                    mw = min(512, ntok - m0)
                    ct, ck = cring()
                    st, sk = sring()
                    self.dma("sp", ct[:, 0:mw], self.din["ropeC"][:, row0 + m0:row0 + m0 + mw], (), [ck])
                    self.dma("sp", st[:, 0:mw], self.din["ropeS"][:, row0 + m0:row0 + m0 + mw], (), [sk])
                    for c4 in range(4):
                        c = wb * 4 + c4
                        a_, ak = pa()
                        for k in range(16):
                            self.mm(a_[:, 0:mw], wt[:, k, c4 * 128:(c4 + 1) * 128], hT[:, k, m0:m0 + mw], k == 0, k == 15, [wk, "hT"], [ak])
                        xb, xk = xbr()
                        self.copy("act", xb[:, 0:mw], a_[:, 0:mw], [ak], [xk])
                        p_, pk = pp()
                        self.mm(p_[:, 0:mw], Pm, xb[:, 0:mw], True, True, ["Pm", xk], [pk])
                        t1, t1k = t1r()
                        self.tt("pool", t1[:, 0:mw], xb[:, 0:mw], ct[:, 0:mw], ALU.mult, [xk, ck], [t1k])
                        t2, t2k = t2r()
                        self.tt("dve", t2[:, 0:mw], p_[:, 0:mw], st[:, 0:mw], ALU.mult, [pk, sk], [t2k])
                        ro, rk = ror()
                        self.tt("dve", ro[:, 0:mw], t1[:, 0:mw], t2[:, 0:mw], ALU.add, [t1k, t2k], [rk])
                        dst = QT if c < 16 else KT
                        self.dma("sp", dst[(c % 16) * 128:(c % 16 + 1) * 128, row0 + m0:row0 + m0 + mw], ro[:, 0:mw], [rk], ["QK"])
            for vb in range(4):
                wt, wk = wring()
                self.load_w(wt, wk, wqkv[:, 4096 + vb * 512:4096 + (vb + 1) * 512], 512)
                for t in range(ntok // 128):
                    v_, vk = pv()
                    for k in range(16):
                        self.mm(v_, hT[:, k, t * 128:(t + 1) * 128], wt[:, k, :], k == 0, k == 15, ["hT", wk], [vk])
                    vs, vsk = vsr()
                    self.copy("act", vs, v_, [vk], [vsk])
                    self.dma("sp", Vm[row0 + t * 128:row0 + (t + 1) * 128, vb * 512:(vb + 1) * 512], vs, [vsk], ["Vm"])
        self.end_phase()
        lambda_init = 0.8 - 0.6 * math.exp(-0.3 * i)
        lp = m.alloc(512, F32, parts=1)
        self.dma("sp", lp, self.din["diff_lambda"][j], (), ["lp"])
        pr = m.alloc(256, F32, parts=1)
        sm = m.alloc(2, F32, parts=1)
        lam1 = m.alloc(1, F32, parts=1)
        self.tt("dve", pr[:, 0:128], lp[:, 0:128], lp[:, 128:256], ALU.mult, ["lp"], ["pr"])
        self.tt("dve", pr[:, 128:256], lp[:, 256:384], lp[:, 384:512], ALU.mult, ["lp"], ["pr"])
        self.S.op("dve", lambda e: e.reduce_sum(out=sm[:, 0:1], in_=pr[:, 0:128], axis=AX.X), ["pr"], ["sm"])
        self.S.op("dve", lambda e: e.reduce_sum(out=sm[:, 1:2], in_=pr[:, 128:256], axis=AX.X), ["pr"], ["sm"])
        self.act(sm, sm, AF.Exp, ["sm"], ["sm"])
        self.tt("dve", lam1, sm[:, 0:1], sm[:, 1:2], ALU.subtract, ["sm"], ["lam1"])
        self.ts("dve", lam1, lam1, -1.0, -lambda_init, ALU.mult, ALU.add, ["lam1"], ["lam1"])
        ones1 = m.alloc(128, F32, parts=1)
        self.memset("pool", ones1, 1.0, ["ones1"])
        neglam = m.alloc(1, F32)
        pl, plk = pa()
        self.mm(pl[:, 0:1], ones1, lam1, True, True, ["ones1", "lam1"], [plk])
        self.copy("dve", neglam, pl[:, 0:1], [plk], ["neglam"])
        sub = m.alloc(256, F32)
        self.dma("sp", sub, self.din["diff_subln"][j, :].partition_broadcast(128), (), ["sub"])
        self.ts("dve", sub, sub, 1.0 - lambda_init, None, ALU.mult, None, ["sub"], ["sub"])
        kring = self.ring("kth", 2, (2, NTOK), BF16)
        vring = self.ring("vh", 2, (NT, 257), BF16)
        for _ in range(2):
            vh, vhk = vring()
            self.memset("pool", vh[:, :, 256:257], 1.0, [vhk])
        qring = self.ring("qt", 2, (2, 256), BF16)
        ptr = self.ring("pT", 4, 512, BF16)
        psc = self.psring("psc", [4, 5, 6, 7])
        tar = self.ring("tA", 2, 256, F32)
        orr = self.ring("ao", 2, 256, F32)
        osr = self.ring("aos", 2, 256, BF16)
        smr = self.ring("asm", 2, 4, F32)
        junk = m.alloc(256, BF16)
        scale = 1.0 / math.sqrt(128.0)
        qblocks = []
        if ctx_on:
            qblocks.append((0, 0, 2))
        for q0 in range(CTX, NTOK, 256):
            qblocks.append((q0, 0, NT))
        for h in range(8):
            kth, kk = kring()
            vh, vhk = vring()
            for t in range(2):
                self.dma("sp", kth[:, t, :], KT[(2 * h + t) * 128:(2 * h + t + 1) * 128, :], ["QK"], [kk])
            vsrc = Vm[:, h * 256:(h + 1) * 256].rearrange("(kt p) c -> p kt c", p=128)
            for k0 in range(0, NT, 8):
                k1 = min(NT, k0 + 8)
                self.dma("sp", vh[:, k0:k1, 0:256], vsrc[:, k0:k1, :], ["Vm"], [vhk])
            for (q0, kt0, kt1) in qblocks:
                qt, qk = qring()
                for t in range(2):
                    self.dma("sp", qt[:, t, :], QT[(2 * h + t) * 128:(2 * h + t + 1) * 128, q0:q0 + 256], ["QK"], [qk])
                items = [(t, kt) for t in range(2) for kt in range(kt0, kt1, 2)]
                pend = []

                def issue_pv(it):
                    t, kt, pT, pTk = it
                    for j in range(2):
                        for qi in range(2):
                            b = t * 2 + qi
                            self.mm(self.ps[b][:, 0:257], pT[:, j * 256 + qi * 128:j * 256 + (qi + 1) * 128], vh[:, kt + j, :],
                                    (kt + j) == kt0, (kt + j) == kt1 - 1, [pTk, vhk], [("ps", b)])

                for (t, kt) in items:
                    s_, sk_ = psc()
                    for j in range(2):
                        self.mm(s_[:, j * 256:(j + 1) * 256], kth[:, t, (kt + j) * 128:(kt + j + 1) * 128], qt[:, t, :], True, True,
                                [kk, qk], [sk_])
                    pT, pTk = ptr()
                    self.act(pT, s_, AF.Exp, [sk_], [pTk], scale=scale)
                    pend.append((t, kt, pT, pTk))
                    if len(pend) > 1:
                        issue_pv(pend.pop(0))
                while pend:
                    issue_pv(pend.pop(0))
                for qi in range(2):
                    O0 = self.ps[qi]
                    O1 = self.ps[2 + qi]
                    k0_, k1_ = ("ps", qi), ("ps", 2 + qi)
                    s4, s4k = smr()
                    self.S.op("dve", lambda e, s4=s4, O0=O0: e.reciprocal(out=s4[:, 0:1], in_=O0[:, 256:257]), [k0_], [s4k])
                    self.S.op("dve", lambda e, s4=s4, O1=O1: e.reciprocal(out=s4[:, 1:2], in_=O1[:, 256:257]), [k1_], [s4k])
                    self.tt("dve", s4[:, 1:2], s4[:, 1:2], neglam, ALU.mult, [s4k, "neglam"], [s4k])
                    tA, tAk = tar()
                    self.ts("dve", tA, O0[:, 0:256], s4[:, 0:1], None, ALU.mult, None, [k0_, s4k], [tAk])
                    o, ok = orr()
                    self.stt(o, O1[:, 0:256], s4[:, 1:2], tA, ALU.mult, ALU.add, [k1_, s4k, tAk], [ok])
                    self.act(junk, o, AF.Square, [ok], ["ajunk", s4k], accum_out=s4[:, 2:3])
                    self.ts("dve", s4[:, 2:3], s4[:, 2:3], 1.0 / 256, 1e-5, ALU.mult, ALU.add, [s4k], [s4k])
                    self.rsqrt(s4[:, 2:3], s4k)
                    os_, osk = osr()
                    self.stt(os_, o, s4[:, 2:3], sub, ALU.mult, ALU.mult, [ok, s4k, "sub"], [osk])
                    self.dma("sp", self.PM[q0 + qi * 128:q0 + (qi + 1) * 128, h * 256:(h + 1) * 256], os_, [osk], ["PM"])
        self.end_phase()


    def phase_gla(self, i, j, ctx_on):
        cfg = self.cfg
        m = self.mem
        NTOK = cfg.NTOK
        NT = NTOK // 128
        QT, KT, Vm, Gm, SP, OF = self.VT, self.BT, self.Hm, self.Gm, self.SP, self.OF
        win = self.din["gla_w_in"][j]
        r = self.norm_setup(i, "mix", True)
        hT = m.alloc((16, 1024), BF16)
        wring = self.ring("gw", 2, (16, 512), BF16)
        w1 = [m.alloc((16, 16), BF16) for _ in range(2)]
        w2 = [m.alloc(1024, BF16, parts=16) for _ in range(2)]
        bb = [m.alloc(1024, F32) for _ in range(2)]
        one = m.alloc(1, F32)
        self.memset("pool", one, 1.0, ["one"])
        for dr in range(2):
            self.load_w(w1[dr], "gw1", self.din["gla_gate_w1"][j, dr], 16)
            self.S.dma("pool", w2[dr], self.din["gla_gate_w2"][j, dr], (), ["gw2"])
            self.dma("sp", bb[dr], self.din["gla_gate_b"][j, dr, :].partition_broadcast(128), (), ["gbb"])
        str_ = self.ring("gst", 2, 512, BF16)
        t1r = self.ring("gt1", 2, 1024, BF16, parts=16)
        zr = self.ring("gz", 2, 512, F32)
        spr = self.ring("gsp", 2, 512, F32)
        pa = self.psring("gpa", [0, 1, 2, 3])
        pz = self.psring("gpz", [4, 5])
        for blk in self.blocks(True):
            row0, ntok, v = blk
            self.norm_block(r, blk, hT, "hT")
            for wb in range(4):
                wt, wk = wring()
                self.load_w(wt, wk, win[:, wb * 512:(wb + 1) * 512], 512)
                for m0 in range(0, ntok, 512):
                    mw = min(512, ntok - m0)
                    for c4 in range(4):
                        c = wb * 4 + c4
                        a_, ak = pa()
                        for k in range(16):
                            self.mm(a_[:, 0:mw], wt[:, k, c4 * 128:(c4 + 1) * 128], hT[:, k, m0:m0 + mw], k == 0, k == 15, [wk, "hT"], [ak])
                        st, sk = str_()
                        if c < 8:
                            self.act(st[:, 0:mw], a_[:, 0:mw], AF.Copy, [ak], [sk], scale=1.0 / 16.0)
                        else:
                            self.copy("dve", st[:, 0:mw], a_[:, 0:mw], [ak], [sk])
                        dst = QT if c < 8 else KT
                        self.dma("sp", dst[(c % 8) * 128:(c % 8 + 1) * 128, row0 + m0:row0 + m0 + mw], st[:, 0:mw], [sk], ["QK"])
            for vb in range(8):
                wt, wk = wring()
                self.load_w(wt, wk, win[:, 2048 + vb * 512:2048 + (vb + 1) * 512], 512)
                for t in range(ntok // 128):
                    a_, ak = pa()
                    for k in range(16):
                        self.mm(a_, hT[:, k, t * 128:(t + 1) * 128], wt[:, k, :], k == 0, k == 15, ["hT", wk], [ak])
                    st, sk = str_()
                    self.copy("act" if t % 2 else "dve", st, a_, [ak], [sk])
                    dst = Vm if vb < 4 else Gm
                    self.dma("sp", dst[row0 + t * 128:row0 + (t + 1) * 128, (vb % 4) * 512:(vb % 4 + 1) * 512], st, [sk], ["VG"])
            for dr in range(2):
                t1, t1k = t1r()
                for m0 in range(0, ntok, 512):
                    mw = min(512, ntok - m0)
                    a_, ak = pz()
                    for k in range(16):
                        self.mm(a_[0:16, 0:mw], w1[dr][:, k, :], hT[:, k, m0:m0 + mw], k == 0, k == 15, ["gw1", "hT"], [ak])
                    self.copy("dve", t1[:, m0:m0 + mw], a_[0:16, 0:mw], [ak], [t1k])
                for t in range(ntok // 128):
                    for cb in range(2):
                        a_, ak = pa()
                        self.mm(a_, t1[:, t * 128:(t + 1) * 128], w2[dr][:, cb * 512:(cb + 1) * 512], True, True, [t1k, "gw2"], [ak])
                        z, zk = zr()
                        self.tt("dve", z, a_, bb[dr][:, cb * 512:(cb + 1) * 512], ALU.add, [ak, "gbb"], [zk])
                        self.act(z, z, AF.Exp, [zk], [zk], scale=-1.0)
                        sp, spk = spr()
                        self.act(sp, z, AF.Ln, [zk, "one"], [spk], bias=one[:, 0:1])
                        self.dma("sp", SP[dr, row0 + t * 128:row0 + (t + 1) * 128, cb * 512:(cb + 1) * 512], sp, [spk], ["SPd"])
        self.end_phase()
        ident = m.alloc(128, BF16)
        self.dma("sp", ident, self.din["ident"], (), ["ident"])
        onb = m.alloc(512, F32)
        self.dma("sp", onb, self.din["gla_onorm"][j, :].partition_broadcast(128), (), ["onb"])
        mask = [m.alloc(128, F32) for _ in range(2)]
        tri = [m.alloc(128, F32) for _ in range(2)]
        for dr in range(2):
            self.dma("sp", mask[dr], self.din["gmask"][dr], (), ["gmask"])
            self.dma("sp", tri[dr], self.din["gtri"][dr], (), ["gtri"])
        Sf = m.alloc((8, 512), F32)
        Sb = m.alloc((8, 512), BF16)
        qr = self.ring("sq", 2, (8, 128), BF16)
        kr = self.ring("sk", 2, (8, 128), BF16)
        vr = self.ring("sv", 2, D, BF16)
        gr = self.ring("sg", 2, D, BF16)
        spr2 = self.ring("ssp", 2, 1024, F32)
        ofr = self.ring("sof", 2, D, F32)
        pmr = self.ring("spm", 2, D, BF16)
        ebr = self.ring("seb", 4, 128, F32)
        enr = self.ring("sen", 4, 128, F32)
        qtr = self.ring("sqt", 4, (2, 128), BF16)
        ktr = self.ring("skt", 4, (2, 128), BF16)
        khr = self.ring("skh", 4, 128, BF16)
        khm = self.ring("skhm", 2, 256, BF16)
        atr = self.ring("sat", 2, 128, BF16)
        ostr = self.ring("sos", 2, 512, F32)
        onr = self.ring("son", 2, 512, F32)
        sgr = self.ring("ssg", 2, 512, F32)
        s4r = self.ring("ss4", 2, 2, F32)
        junk = m.alloc(512, BF16)
        pb = self.psring("spb", [0, 1])
        ptr_ = self.psring("sptr", [2], bf16=True)
        pA = self.psring("spA", [3])
        pO = self.psring("spO", [4, 5])
        pS = self.psring("spS", [6, 7])
        QTv = QT.rearrange("(c p) t -> p c t", p=128)
        KTv = KT.rearrange("(c p) t -> p c t", p=128)
        for dr in range(2):
            self.memset("pool", Sf, 0.0, ["Sf"])
            self.memset("pool", Sb, 0.0, ["Sb"])
            if dr == 0:
                order = list(range(NT))
            else:
                order = [1, 0] + list(range(NT - 1, 1, -1))
            endc = 127 if dr == 0 else 0
            for tt_ in order:
                rows = slice(tt_ * 128, (tt_ + 1) * 128)
                q_, qk = qr()
                k_, kk = kr()
                v_, vk = vr()
                sp_, spk = spr2()
                self.dma("sp", q_, QTv[:, 0:8, rows], ["QK"], [qk])
                self.dma("sp", k_, KTv[:, 0:8, rows], ["QK"], [kk])
                self.dma("sp", v_, Vm[rows, :], ["VG"], [vk])
                self.dma("sp", sp_, SP[dr, rows, :], ["SPd"], [spk])
                if dr == 1:
                    g_, gk = gr()
                    of_, ofk = ofr()
                    self.dma("sp", g_, Gm[rows, :], ["VG"], [gk])
                    self.dma("sp", of_, OF[rows, :], ["OFd"], [ofk])
                    pm_, pmk = pmr()
                else:
                    of_, ofk = ofr()
                for h in range(4):
                    qt, qtk = qtr()
                    kt, ktk = ktr()
                    kh, khk = khm()
                    ebs = []
                    for dc in range(2):
                        c = h * 2 + dc
                        b_, bk = pb()
                        self.mm(b_[:, 0:128], sp_[:, c * 128:(c + 1) * 128], tri[dr], True, True, [spk, "gtri"], [bk])
                        eb, ebk = ebr()
                        en, enk = enr()
                        self.act(eb, b_[:, 0:128], AF.Exp, [bk], [ebk])
                        self.act(en, b_[:, 0:128], AF.Exp, [bk], [enk], scale=-1.0)
                        self.tt("dve", qt[:, dc, :], q_[:, c, :], eb, ALU.mult, [qk, ebk], [qtk])
                        self.tt("pool", kt[:, dc, :], k_[:, c, :], en, ALU.mult, [kk, enk], [ktk])
                        khT, khTk = khr()
                        self.ts("dve", khT, kt[:, dc, :], eb[:, endc:endc + 1], None, ALU.mult, None, [ktk, ebk], [khTk])
                        p_, pk = ptr_()
                        self.tr(p_[:, 0:128], khT, ident, [khTk, "ident"], [pk])
                        self.copy("act", kh[:, dc * 128:(dc + 1) * 128], p_[:, 0:128], [pk], [khk])
                        ebs.append((eb, ebk))
                    a_, ak = pA()
                    for dc in range(2):
                        self.mm(a_[:, 0:128], kt[:, dc, :], qt[:, dc, :], dc == 0, dc == 1, [ktk, qtk], [ak])
                    at, atk = atr()
                    self.tt("dve", at, a_[:, 0:128], mask[dr], ALU.mult, [ak, "gmask"], [atk])
                    o_, ok = pO()
                    self.mm(o_, at, v_[:, h * 512:(h + 1) * 512], True, False, [atk, vk], [ok])
                    for dc in range(2):
                        self.mm(o_, qt[:, dc, :], Sb[:, h * 2 + dc, :], False, dc == 1, [qtk, "Sb"], [ok])
                    for dc in range(2):
                        c = h * 2 + dc
                        s_, sk = pS()
                        self.mm(s_, kh[:, dc * 128:(dc + 1) * 128], v_[:, h * 512:(h + 1) * 512], True, True, [khk, vk], [sk])
                        eb, ebk = ebs[dc]
                        self.stt(Sf[:, c, :], Sf[:, c, :], eb[:, endc:endc + 1], s_, ALU.mult, ALU.add, ["Sf", ebk, sk, ok], ["Sf"])
                        self.copy("act", Sb[:, c, :], Sf[:, c, :], ["Sf"], ["Sb"])
                    if dr == 0:
                        self.copy("act", of_[:, h * 512:(h + 1) * 512], o_, [ok], [ofk])
                    else:
                        os_, osk = ostr()
                        self.tt("dve", os_, o_, of_[:, h * 512:(h + 1) * 512], ALU.add, [ok, ofk], [osk])
                        s4, s4k = s4r()
                        self.act(junk, os_, AF.Square, [osk], ["gjunk", s4k], accum_out=s4[:, 0:1])
                        self.ts("dve", s4[:, 0:1], s4[:, 0:1], 1.0 / 512, EPS, ALU.mult, ALU.add, [s4k], [s4k])
                        self.rsqrt(s4[:, 0:1], s4k)
                        on, onk = onr()
                        self.stt(on, os_, s4[:, 0:1], onb, ALU.mult, ALU.mult, [osk, s4k, "onb"], [onk])
                        sg, sgk = sgr()
                        self.act(sg, g_[:, h * 512:(h + 1) * 512], AF.Silu, [gk], [sgk])
                        self.tt("pool", pm_[:, h * 512:(h + 1) * 512], on, sg, ALU.mult, [onk, sgk], [pmk])
                if dr == 0:
                    self.dma("sp", OF[rows, :], of_, [ofk], ["OFd"])
                else:
                    self.dma("sp", self.PM[rows, :], pm_, [pmk], ["PM"])
        self.end_phase()

    def phase_moe(self, i, ctx_on):
        cfg = self.cfg
        m = self.mem
        E = cfg.E
        NTOK = cfg.NTOK
        LAT = cfg.LAT
        aff = m.alloc(NTOK, F32, parts=E)
        r = self.norm_setup(i, "ffn", ctx_on)
        hT = m.alloc((16, 1024), BF16)
        rw = m.alloc((16, E), BF16)
        self.load_w(rw, "rw", self.din["router_w"][i], E)
        ones = m.alloc(E, BF16, parts=E)
        self.memset("pool", ones, 1.0, ["ones"])
        ex = self.ring("mex", 2, 512, BF16, parts=E)
        exf = self.ring("mexf", 2, 512, F32, parts=E)
        rc = self.ring("mrc", 2, 512, F32, parts=E)
        pring = self.psring("mp", [0, 1])
        pring2 = self.psring("mp2", [2, 3])
        for blk in self.cur_blocks:
            row0, ntok, v = blk
            self.norm_block(r, blk, hT, "hT", hm_store=True)
            for m0 in range(0, ntok, 512):
                mw = min(512, ntok - m0)
                pt, pk = pring()
                for k in range(16):
                    self.mm(pt[0:E, 0:mw], rw[:, k, :], hT[:, k, m0:m0 + mw], k == 0, k == 15, ["rw", "hT"], [pk])
                ef, efk = exf()
                self.act(ef[:, 0:mw], pt[0:E, 0:mw], AF.Exp, [pk], [efk])
                e_, ek = ex()
                self.copy("dve", e_[:, 0:mw], ef[:, 0:mw], [efk], [ek])
                lo, lk = ex()
                self.tt("dve", lo[:, 0:mw], ef[:, 0:mw], e_[:, 0:mw], ALU.subtract, [efk, ek], [lk])
                p2, pk2 = pring2()
                self.mm(p2[0:E, 0:mw], ones, e_[:, 0:mw], True, False, ["ones", ek], [pk2])
                self.mm(p2[0:E, 0:mw], ones, lo[:, 0:mw], False, True, ["ones", lk], [pk2])
                rr, rk = rc()
                self.S.op("dve", lambda e, rr=rr, p2=p2, mw=mw: e.reciprocal(out=rr[:, 0:mw], in_=p2[0:E, 0:mw]), [pk2], [rk])
                self.tt("dve", aff[:, row0 + m0:row0 + m0 + mw], ef[:, 0:mw], rr[:, 0:mw], ALU.mult, [efk, rk], ["aff"])
        self.barrier()
        segs = [(0, CTX, LAT, cfg.cap_l)]
        if ctx_on:
            segs.append((1, 0, CTX, cfg.cap_c))
        res = {}
        for (sid, c0, n, cap) in segs:
            vals = m.alloc(cap, F32, parts=E)
            idx = m.alloc(cap, U32, parts=E)
            src = aff[:, c0:c0 + n]
            vk, ik = "tv%d" % sid, "ti%d" % sid
            for rr_ in range(cap // 8):
                sl = slice(rr_ * 8, rr_ * 8 + 8)
                self.S.op("dve", lambda e, vals=vals, sl=sl, src=src: e.max(out=vals[:, sl], in_=src), ["aff"], [vk])
                self.S.op("dve", lambda e, vals=vals, idx=idx, sl=sl, src=src: e.max_index(out=idx[:, sl], in_max=vals[:, sl], in_values=src),
                          ["aff", vk], [ik])
                self.S.op("dve", lambda e, vals=vals, sl=sl, src=src: e.match_replace(out=src, in_to_replace=vals[:, sl], in_values=src, imm_value=-1.0),
                          [vk, ik], ["aff"])
            self.dma("sp", self.gate_d[sid, :, 0:cap], vals, [vk], ["gate_d"])
            self.dma("sp", self.idx_d[sid, :, 0:cap], idx, [ik], ["idx_d"])
            res[sid] = (c0, n, cap)
        self.end_phase()
        res2 = {}
        for sid in sorted(res):
            c0, n, cap = res[sid]
            gs = min(128, cap)
            ng = cap // gs
            idxT = m.alloc((E, ng), U32)
            gateT = m.alloc((E, ng), F32)
            for e_ in range(E):
                self.dma("sp", idxT[0:gs, e_, :], self.idx_d[sid, e_, 0:cap].rearrange("(g p) -> p g", p=gs), (), ["idxT%d" % sid],
                         allow_slow_non_contiguous=True)
                self.dma("sp", gateT[0:gs, e_, :], self.gate_d[sid, e_, 0:cap].rearrange("(g p) -> p g", p=gs), (), ["gateT%d" % sid],
                         allow_slow_non_contiguous=True)
            res2[sid] = (idxT, gateT, gs, ng, c0, n)
        res = res2
        groups = []
        slot = 0
        for sid in sorted(res):
            idxT, gateT, gs, ng, c0, n = res[sid]
            for g in range(ng):
                groups.append((sid, g, gs, slot))
                slot += gs
        NS = slot
        ident = m.alloc(128, BF16)
        self.dma("sp", ident, self.din["ident"], (), ["ident"])
        r = {"ident": ident, "ptr": self.psring("mtr", [6, 7], bf16=True)}
        g2 = {0: self.load_gate_bc(i, "ffn", 0, "g2L")}
        if ctx_on:
            g2[1] = self.load_gate_bc(i, "ffn", 1, "g2C")
        XT = m.alloc((16, NS), BF16)
        zT = m.alloc((12, NS), BF16)
        FB = 256
        wgr = self.ring("wg", 2, (16, FB), BF16)
        wur = self.ring("wu", 2, (16, FB), BF16)
        wd = m.alloc((12, D), BF16)
        xgr = self.ring("xg", 2, D, BF16)
        ysr = self.ring("ys", 2, D, F32)
        sar = self.ring("sa", 2, 512, F32)
        pA = self.psring("pA", [0, 1])
        pU = self.psring("pU", [2, 3])
        pY = self.psring("pY", [4, 5])
        mtiles = []
        nl = cfg.cap_l
        for s0 in range(0, nl, 512):
            mtiles.append((s0, min(512, nl - s0)))
        if ctx_on:
            mtiles.append((nl, NS - nl))
        for e in range(E):
            for (sid, g, gs, s0) in groups:
                idxT, gateT, _, _, c0, n = res[sid]
                xg, xk = xgr()
                srcrows = self.Hm
                ia = idxT[0:gs, e, g:g + 1]
                self.S.dma_custom("pool", lambda en, xg=xg, gs=gs, srcrows=srcrows, ia=ia, c0=c0: en.indirect_dma_start(
                    out=xg[0:gs, :], out_offset=None, in_=srcrows, in_offset=bass.IndirectOffsetOnAxis(ap=ia, axis=0),
                    element_offset=c0 * D),
                    ["Hm", "idxT%d" % sid], [xk])
                self.transpose_tile(r, xg, xk, gs, XT, "XT", s0)
            wdv = self.din["exp_w_down"][i, e].rearrange("(c p) n -> p c n", p=128)
            for c0_ in range(0, 12, 3):
                self.S.dma("pool", wd[:, c0_:c0_ + 3, :], wdv[:, c0_:c0_ + 3, :], (), ["wd"])
            for fb in range(FF // FB):
                wg, wgk = wgr()
                wu, wuk = wur()
                self.load_w(wg, wgk, self.din["exp_w_gate"][i, e][:, fb * FB:(fb + 1) * FB], FB)
                self.load_w(wu, wuk, self.din["exp_w_up"][i, e][:, fb * FB:(fb + 1) * FB], FB)
                for c in range(FB // 128):
                    fc = fb * (FB // 128) + c
                    for (s0, w_) in mtiles:
                        a_, ak = pA()
                        u_, uk = pU()
                        for k in range(16):
                            self.mm(a_[:, 0:w_], wg[:, k, c * 128:(c + 1) * 128], XT[:, k, s0:s0 + w_], k == 0, k == 15, [wgk, "XT"], [ak])
                        for k in range(16):
                            self.mm(u_[:, 0:w_], wu[:, k, c * 128:(c + 1) * 128], XT[:, k, s0:s0 + w_], k == 0, k == 15, [wuk, "XT"], [uk])
                        sa, sk = sar()
                        self.act(sa[:, 0:w_], a_[:, 0:w_], AF.Silu, [ak], [sk])
                        self.tt("dve", zT[:, fc, s0:s0 + w_], sa[:, 0:w_], u_[:, 0:w_], ALU.mult, [sk, uk], ["zT"])
            for (sid, g, gs, s0) in groups:
                idxT, gateT, _, _, c0, n = res[sid]
                gbc, gbk = g2[sid]
                ys, yk = ysr()
                for db in range(4):
                    y_, ypk = pY()
                    for c in range(12):
                        self.mm(y_[0:gs, :], zT[:, c, s0:s0 + gs], wd[:, c, db * 512:(db + 1) * 512], c == 0, c == 11, ["zT", "wd"], [ypk])
                    self.stt(ys[0:gs, db * 512:(db + 1) * 512], y_[0:gs, :], gateT[0:gs, e, g:g + 1], gbc[0:gs, db * 512:(db + 1) * 512],
                             ALU.mult, ALU.mult, [ypk, "gateT%d" % sid, gbk], [yk])
                dst = self.X
                ia = idxT[0:gs, e, g:g + 1]
                self.S.dma_custom("pool", lambda en, ys=ys, gs=gs, dst=dst, ia=ia, c0=c0: en.indirect_dma_start(
                    out=dst, out_offset=bass.IndirectOffsetOnAxis(ap=ia, axis=0), in_=ys[0:gs, :], in_offset=None, compute_op=ALU.add,
                    element_offset=c0 * D),
                    [yk, "idxT%d" % sid], ["X"])
        self.end_phase()

    def phase_final(self):
        m = self.mem
        g = m.alloc(D, F32)
        self.dma("sp", g, self.din["final_norm"][0, :].partition_broadcast(128), (), ["fg"])
        xring = self.ring("fx", 2, D, F32)
        oring = self.ring("fo", 2, D, F32)
        ss = self.ring("fss", 2, 1, F32)
        junk = m.alloc(D, BF16)
        for t in range(self.cfg.LAT // 128):
            xs, xk = xring()
            self.dma("sp", xs, self.X[CTX + t * 128:CTX + (t + 1) * 128, :], ["X"], [xk])
            s, sk = ss()
            self.act(junk, xs, AF.Square, [xk], ["fjunk", sk], accum_out=s)
            self.ts("dve", s, s, 1.0 / D, EPS, ALU.mult, ALU.add, [sk], [sk])
            self.rsqrt(s, sk)
            o, ok = oring()
            self.stt(o, xs, s[:, 0:1], g, ALU.mult, ALU.mult, [xk, sk, "fg"], [ok])
            self.dma("sp", self.out[t * 128:(t + 1) * 128, :], o, [ok], ["out"])


def prep_inputs(inp, cfg):
    f32 = np.float32
    d = {}
    d["xin"] = np.ascontiguousarray(inp["x"][0], dtype=f32)
    d["ctxin"] = np.ascontiguousarray(inp["ctx"][0], dtype=f32)
    cl = np.asarray(inp["c"][0], f32).reshape(16, 128).T
    cc = np.asarray(inp["c_ctx"], f32).reshape(16, 128).T
    d["ccols"] = np.ascontiguousarray(np.stack([cl, cc], axis=2).reshape(128, 32))
    for k in ("mod_w", "mod_b", "norm_mix", "norm_ffn", "router_w", "exp_w_gate", "exp_w_up", "exp_w_down", "conv_w_out"):
        d[k] = np.ascontiguousarray(inp[k], dtype=f32)
    d["final_norm"] = np.asarray(inp["final_norm"], f32).reshape(1, D)
    w = np.asarray(inp["conv_w_in"], f32)
    nA = w.shape[0]
    parts = [w[:, :, s * D:(s + 1) * D].reshape(nA, D, 16, 1, 128) for s in range(3)]
    d["conv_w_in_r"] = np.ascontiguousarray(np.concatenate(parts, axis=3).reshape(nA, D, 3 * D))
    dw = np.asarray(inp["conv_w_dw"], f32).reshape(nA, 3, 16, 128)
    d["conv_dw_cols"] = np.ascontiguousarray(dw.transpose(0, 3, 1, 2).reshape(nA, 128, 48))
    d["ident"] = np.eye(128, dtype=f32).astype(ml_dtypes.bfloat16)
    if cfg.nB:
        d["diff_w_qkv"] = np.ascontiguousarray(inp["diff_w_qkv"], dtype=f32)
        d["diff_lambda"] = np.asarray(inp["diff_lambda"], f32).reshape(cfg.nB, 1, 512)
        d["diff_subln"] = np.asarray(inp["diff_subln"], f32)
        d["diff_w_out"] = np.ascontiguousarray(inp["diff_w_out"], dtype=f32)
        n = np.arange(cfg.LAT)
        pos = [n // 64, n % 64]
        inv = 10000.0 ** (-np.arange(32, dtype=np.float64) / 32)
        C = np.ones((128, cfg.NTOK), np.float64)
        Sn = np.zeros((128, cfg.NTOK), np.float64)
        P = np.zeros((128, 128), np.float64)
        for dd in range(128):
            sct, within = dd // 64, dd % 64
            ang = pos[sct].astype(np.float64) * inv[within % 32]
            ang = (pos[sct].astype(np.float32) * inv[within % 32].astype(np.float32)).astype(np.float64)
            C[dd, CTX:] = np.cos(ang)
            Sn[dd, CTX:] = np.sin(ang)
            if within < 32:
                P[dd + 32, dd] = -1.0
            else:
                P[dd - 32, dd] = 1.0
        d["ropeC"] = C.astype(f32)
        d["ropeS"] = Sn.astype(f32)
        d["ropeP"] = P.astype(f32).astype(ml_dtypes.bfloat16)
    if cfg.nC:
        for k in ("gla_w_in", "gla_gate_w1", "gla_gate_w2", "gla_gate_b", "gla_onorm", "gla_w_out"):
            d[k] = np.ascontiguousarray(inp[k], dtype=f32)
        jj, ii = np.meshgrid(np.arange(128), np.arange(128), indexing="ij")
        mk = np.stack([(jj <= ii), (jj >= ii)]).astype(f32)
        d["gmask"] = mk
        d["gtri"] = (mk * (-1.0 / 16.0)).astype(f32)
    return d


def kernel(**inputs):
    cfg = Cfg()
    prog = Prog(cfg)
    nc = prog.build()
    d = prep_inputs(inputs, cfg)
    d = {k: v for k, v in d.items() if k in prog.din}
    res = run_bass_kernel_spmd(nc, [d], core_ids=[0])
    out = np.asarray(res.results[0]["out"], dtype=np.float32)
    return out.reshape(1, cfg.LAT, D)
```

```python
import numpy as np
import concourse.bass as bass
import concourse.mybir as mybir
from concourse.bass_utils import run_bass_kernel_spmd

F32 = mybir.dt.float32
BF16 = mybir.dt.bfloat16
I32 = mybir.dt.int32
U32 = mybir.dt.uint32
AF = mybir.ActivationFunctionType
ALU = mybir.AluOpType
AX = mybir.AxisListType


class Sched:
    ENG = ("pe", "act", "dve", "pool", "sp")

    def __init__(self, nc, es, n_dma_sems=12, same_engine_sync=True):
        self.nc = nc
        self.same = same_engine_sync
        self.thunks = {e: [] for e in self.ENG}
        self.sem = {e: es.enter_context(nc.semaphore("c_" + e)) for e in self.ENG}
        self.cnt = {e: 0 for e in self.ENG}
        self.seen = {e: {} for e in self.ENG}
        self.dsems = {}
        for q in ("sp", "pool", "act"):
            self.dsems[q] = [
                [es.enter_context(nc.semaphore("d_%s%d" % (q, i))), 0] for i in range(n_dma_sems)
            ]
        self.dnext = {q: 0 for q in self.dsems}
        self.lastw = {}
        self.reads = {}
        self.n_wait = 0

    def _need(self, eng, evs):
        best = {}
        for ev in evs:
            if ev is None:
                continue
            sem, name, val = ev
            if name == "c_" + eng and not self.same:
                continue
            if name == "c_pe" and eng == "pe":
                continue
            if self.seen[eng].get(name, 0) >= val:
                continue
            if name not in best or best[name][1] < val:
                best[name] = (sem, val)
        for name, (sem, val) in best.items():
            self.seen[eng][name] = val
            self.thunks[eng].append(lambda e, sem=sem, val=val: e.wait_ge(sem, val))
            self.n_wait += 1

    def _deps(self, reads, writes):
        evs = []
        for k in reads:
            evs.append(self.lastw.get(k))
        for k in writes:
            evs.append(self.lastw.get(k))
            evs.extend(self.reads.get(k, ()))
        return evs

    def _commit(self, ev, reads, writes):
        for k in reads:
            self.reads.setdefault(k, []).append(ev)
        for k in writes:
            self.lastw[k] = ev
            self.reads[k] = []

    def op(self, eng, fn, reads=(), writes=()):
        self._need(eng, self._deps(reads, writes))
        self.cnt[eng] += 1
        sem = self.sem[eng]
        self.thunks[eng].append(lambda e, fn=fn, sem=sem: fn(e).then_inc(sem, 1))
        ev = (sem, "c_" + eng, self.cnt[eng])
        self._commit(ev, reads, writes)
        return ev

    def dma(self, q, out, in_, reads=(), writes=(), **kw):
        slot = self.dsems[q][self.dnext[q] % len(self.dsems[q])]
        self.dnext[q] += 1
        sem, val = slot
        name = sem.name if hasattr(sem, "name") else str(id(sem))
        name = "d_%s_%d" % (q, (self.dnext[q] - 1) % len(self.dsems[q]))
        evs = self._deps(reads, writes)
        if val > 0:
            evs.append((sem, name, val))
        self._need(q, evs)
        slot[1] = val + 16
        self.thunks[q].append(
            lambda e, out=out, in_=in_, sem=sem, kw=kw: e.dma_start(out=out, in_=in_, **kw).then_inc(sem, 16)
        )
        ev = (sem, name, val + 16)
        self._commit(ev, reads, writes)
        return ev

    def dma_custom(self, q, fn, reads=(), writes=()):
        slot = self.dsems[q][self.dnext[q] % len(self.dsems[q])]
        name = "d_%s_%d" % (q, self.dnext[q] % len(self.dsems[q]))
        self.dnext[q] += 1
        sem, val = slot
        evs = self._deps(reads, writes)
        if val > 0:
            evs.append((sem, name, val))
        self._need(q, evs)
        slot[1] = val + 16
        self.thunks[q].append(lambda e, fn=fn, sem=sem: fn(e).then_inc(sem, 16))
        ev = (sem, name, val + 16)
        self._commit(ev, reads, writes)
        return ev

    def wait_all(self, eng):
        evs = []
        for k, ev in self.lastw.items():
            evs.append(ev)
        for k, l in self.reads.items():
            evs.extend(l)
        self._need(eng, evs)

    def emit(self):
        nc = self.nc
        with nc.Block() as block:
            @block.tensor
            def _(e):
                for t in self.thunks["pe"]:
                    t(e)

            @block.scalar
            def _(e):
                for t in self.thunks["act"]:
                    t(e)

            @block.vector
            def _(e):
                for t in self.thunks["dve"]:
                    t(e)

            @block.gpsimd
            def _(e):
                for t in self.thunks["pool"]:
                    t(e)

            @block.sync
            def _(e):
                for t in self.thunks["sp"]:
                    t(e)
        self.thunks = {e: [] for e in self.ENG}


import contextlib
import math
import numpy as np
import ml_dtypes

D = 2048
CTX = 256
FF = 1536
EPS = 1e-6


def prod(t):
    r = 1
    for a in t:
        r *= a
    return r


class Mem:
    def __init__(self, prog):
        self.prog = prog

    def alloc(self, free, dt, parts=128):
        if isinstance(free, int):
            free = (free,)
        n = prod(free)
        p = self.prog
        p.uid += 1
        t = p.pes.enter_context(p.nc.sbuf_tensor("b%d" % p.uid, [128, n], dt))
        v = t[:parts, :]
        if len(free) == 2:
            v = v.rearrange("p (a b) -> p a b", a=free[0])
        elif len(free) == 3:
            v = v.rearrange("p (a b c) -> p a b c", a=free[0], b=free[1])
        return v


class Cfg:
    def __init__(self, LAT=8192, E=16, DEPTH=4, CF=2):
        self.LAT = LAT
        self.E = E
        self.DEPTH = DEPTH
        self.NTOK = CTX + LAT
        self.cap_l = CF * LAT // E
        self.cap_c = CF * CTX // E
        self.nA = sum(1 for i in range(DEPTH) if i % 3 == 0)
        self.nB = sum(1 for i in range(DEPTH) if i % 3 == 1)
        self.nC = sum(1 for i in range(DEPTH) if i % 3 == 2)


class Prog:
    def __init__(self, cfg):
        self.cfg = cfg
        self.nc = bass.Bass("TRN2", target_bir_lowering=False)
        self.din = {}
        self.uid = 0

    def inp(self, name, shape, dt=F32):
        self.din[name] = self.nc.dram_tensor(name, list(shape), dt, kind="ExternalInput").ap()
        return self.din[name]

    def scratch(self, name, shape, dt):
        return self.nc.dram_tensor(name, list(shape), dt, kind="Internal").ap()

    def barrier(self):
        S = self.S
        evs = []
        for k, ev in S.lastw.items():
            evs.append(ev)
        for k, l in S.reads.items():
            evs.extend(l)
        for e in S.ENG:
            S._need(e, evs)
        S.lastw.clear()
        S.reads.clear()

    def begin_phase(self):
        self.pes = contextlib.ExitStack()
        self.nphase = getattr(self, "nphase", 0) + 1
        self.ps = [self.pes.enter_context(self.nc.psum_tensor("ps%d_%d" % (self.nphase, i), [128, 512], F32)) for i in range(8)]

    def end_phase(self, last=False):
        self.barrier()
        self.S.emit()
        self.pes.close()
        if not last:
            self.begin_phase()

    def act(self, out, in_, func, reads, writes, **kw):
        self.S.op("act", lambda e: e.activation(out=out, in_=in_, func=func, **kw), reads, writes)

    def ts(self, eng, out, in0, s1, s2, op0, op1, reads, writes, **kw):
        if op1 is None:
            self.S.op(eng, lambda e: e.tensor_scalar(out=out, in0=in0, scalar1=s1, scalar2=None, op0=op0, **kw), reads, writes)
        else:
            self.S.op(eng, lambda e: e.tensor_scalar(out=out, in0=in0, scalar1=s1, scalar2=s2, op0=op0, op1=op1, **kw), reads, writes)

    def tt(self, eng, out, in0, in1, op, reads, writes):
        self.S.op(eng, lambda e: e.tensor_tensor(out=out, in0=in0, in1=in1, op=op), reads, writes)

    def stt(self, out, in0, scalar, in1, op0, op1, reads, writes):
        self.S.op("dve", lambda e: e.scalar_tensor_tensor(out=out, in0=in0, scalar=scalar, in1=in1, op0=op0, op1=op1), reads, writes)

    def rsqrt(self, ap, key):
        self.S.op("act", lambda e: e.activation(out=ap, in_=ap, func=AF.Sqrt), [key], [key])
        self.S.op("dve", lambda e: e.reciprocal(out=ap, in_=ap), [key], [key])

    def copy(self, eng, out, in_, reads, writes):
        if eng == "act":
            self.S.op("act", lambda e: e.activation(out=out, in_=in_, func=AF.Copy), reads, writes)
        else:
            self.S.op(eng, lambda e: e.tensor_copy(out=out, in_=in_), reads, writes)

    def memset(self, eng, out, val, writes):
        self.S.op(eng, lambda e: e.memset(out, val), (), writes)

    def mm(self, out, lhsT, rhs, start, stop, reads, writes):
        self.S.op("pe", lambda e: e.matmul(out, lhsT=lhsT, rhs=rhs, start=start, stop=stop), reads, writes)

    def tr(self, out, in_, ident, reads, writes):
        self.S.op("pe", lambda e: e.transpose(out=out, in_=in_, identity=ident), reads, writes)

    def dma(self, q, out, in_, reads, writes, **kw):
        self.S.dma(q, out, in_, reads, writes, **kw)

    def ring(self, name, n, free, dt, parts=128):
        tiles = [self.mem.alloc(free, dt, parts) for _ in range(n)]
        st = {"i": 0}

        def nxt():
            i = st["i"] % n
            st["i"] += 1
            return tiles[i], (name, i)
        return nxt

    def psring(self, name, idxs, bf16=False):
        st = {"i": 0}

        def nxt():
            i = idxs[st["i"] % len(idxs)]
            st["i"] += 1
            t = self.ps[i][:]
            if bf16:
                t = t.bitcast(BF16)
            return t, ("ps", i)
        return nxt

    def load_w(self, dst, key, w_ap, ncols):
        kc = w_ap.shape[0] // 128
        src = w_ap.rearrange("(k p) n -> p k n", p=128)
        step = max(1, kc // 4)
        for k0 in range(0, kc, step):
            k1 = min(kc, k0 + step)
            self.S.dma("pool", dst[:, k0:k1, 0:ncols], src[:, k0:k1, :], (), [key])

    def build(self):
        cfg = self.cfg
        nc = self.nc
        LAT, E, DEPTH, NTOK = cfg.LAT, cfg.E, cfg.DEPTH, cfg.NTOK
        inp = self.inp
        inp("xin", [LAT, D])
        inp("ctxin", [CTX, D])
        inp("ccols", [128, 32])
        inp("mod_w", [DEPTH, D, 6 * D])
        inp("mod_b", [DEPTH, 6 * D])
        inp("norm_mix", [DEPTH, D])
        inp("norm_ffn", [DEPTH, D])
        inp("final_norm", [1, D])
        inp("conv_w_in_r", [cfg.nA, D, 3 * D])
        inp("conv_dw_cols", [cfg.nA, 128, 48])
        inp("conv_w_out", [cfg.nA, D, D])
        inp("router_w", [DEPTH, D, E])
        inp("exp_w_gate", [DEPTH, E, D, FF])
        inp("exp_w_up", [DEPTH, E, D, FF])
        inp("exp_w_down", [DEPTH, E, FF, D])
        inp("ident", [128, 128], BF16)
        if cfg.nB:
            inp("diff_w_qkv", [cfg.nB, D, 3 * D])
            inp("diff_lambda", [cfg.nB, 1, 512])
            inp("diff_subln", [cfg.nB, 256])
            inp("diff_w_out", [cfg.nB, D, D])
            inp("ropeC", [128, NTOK])
            inp("ropeS", [128, NTOK])
            inp("ropeP", [128, 128], BF16)
            self.PM = self.scratch("PM", [NTOK, D], BF16)
        if cfg.nC:
            inp("gla_w_in", [cfg.nC, D, 3 * D])
            inp("gla_gate_w1", [cfg.nC, 2, D, 16])
            inp("gla_gate_w2", [cfg.nC, 2, 16, 1024])
            inp("gla_gate_b", [cfg.nC, 2, 1024])
            inp("gla_onorm", [cfg.nC, 512])
            inp("gla_w_out", [cfg.nC, D, D])
            inp("gmask", [2, 128, 128])
            inp("gtri", [2, 128, 128])
            self.Gm = self.scratch("Gm", [NTOK, D], BF16)
            self.SP = self.scratch("SP", [2, NTOK, 1024], F32)
            self.OF = self.scratch("OF", [NTOK, D], F32)
            if not cfg.nB:
                self.PM = self.scratch("PM", [NTOK, D], BF16)
        self.out = nc.dram_tensor("out", [LAT, D], F32, kind="ExternalOutput").ap()
        self.X = self.scratch("X", [NTOK, D], F32)
        self.modv = self.scratch("modv", [DEPTH, 2, 6 * D], F32)
        self.Hm = self.scratch("Hm", [NTOK, D], BF16)
        self.VT = self.scratch("VT", [D, NTOK], BF16)
        self.BT = self.scratch("BT", [D, NTOK], BF16)
        self.PT = self.scratch("PT", [D, NTOK], BF16)
        self.idx_d = self.scratch("idx_d", [2, E, max(cfg.cap_l, 128)], U32)
        self.gate_d = self.scratch("gate_d", [2, E, max(cfg.cap_l, 128)], F32)
        with contextlib.ExitStack() as es:
            self.S = Sched(nc, es)
            self.mem = Mem(self)
            self.begin_phase()
            self.body()
            self.end_phase(last=True)
        return nc

    def body(self):
        cfg = self.cfg
        self.setup()
        for i in range(cfg.DEPTH):
            kind, j = i % 3, i // 3
            ctx_on = i < cfg.DEPTH - 1
            self.cur_blocks = self.blocks(ctx_on)
            self.phase_mod(i)
            if kind == 0:
                self.phase_conv(i, j, ctx_on)
                self.phase_outproj(i, self.PT, True, self.din["conv_w_out"][j], ctx_on)
            elif kind == 1:
                self.phase_attn(i, j, ctx_on)
                self.phase_outproj(i, self.PM, False, self.din["diff_w_out"][j], ctx_on)
            else:
                self.phase_gla(i, j, ctx_on)
                self.phase_outproj(i, self.PM, False, self.din["gla_w_out"][j], ctx_on)
            self.phase_moe(i, ctx_on)
        self.phase_final()

    def blocks(self, ctx_on):
        b = []
        if ctx_on:
            b.append((0, CTX, 1))
        for r in range(0, self.cfg.LAT, 1024):
            b.append((CTX + r, min(1024, self.cfg.LAT - r), 0))
        return b

    def setup(self):
        self.dma("sp", self.X[0:CTX, :], self.din["ctxin"], (), ["X"])
        LAT = self.cfg.LAT
        for r in range(0, LAT, 1024):
            self.dma("sp", self.X[CTX + r:CTX + r + 1024, :], self.din["xin"][r:r + 1024, :], (), ["X"])
        z = self.mem.alloc(D, BF16)
        self.memset("pool", z, 0.0, ["z"])
        for t in range(CTX // 128):
            self.dma("sp", self.Hm[t * 128:(t + 1) * 128, :], z, ["z"], ["Hm"])
        self.end_phase()

    def phase_mod(self, i):
        m = self.mem
        cc = m.alloc(32, F32)
        sc = m.alloc((16, 2), BF16)
        bias = m.alloc(6 * D, F32, parts=2)
        res = m.alloc(6 * D, F32, parts=2)
        wring = self.ring("modw", 2, (16, 512), BF16)
        pring = self.psring("modp", [0, 1])
        self.dma("sp", cc, self.din["ccols"], (), ["cc"])
        self.act(sc.rearrange("p k v -> p (k v)"), cc, AF.Silu, ["cc"], ["sc"])
        for v in range(2):
            self.dma("sp", bias[v:v + 1, :], self.din["mod_b"][i:i + 1, :], (), ["mbias"])
        for cb in range(6 * D // 512):
            wt, wk = wring()
            self.load_w(wt, wk, self.din["mod_w"][i][:, cb * 512:(cb + 1) * 512], 512)
            pt, pk = pring()
            for k in range(16):
                self.mm(pt[0:2, :], sc[:, k, :], wt[:, k, :], k == 0, k == 15, ["sc", wk], [pk])
            self.tt("dve", res[:, cb * 512:(cb + 1) * 512], pt[0:2, :], bias[:, cb * 512:(cb + 1) * 512], ALU.add,
                    [pk, "mbias"], ["mres"])
        self.dma("sp", self.modv[i], res, ["mres"], ["modv"])
        self.end_phase()

    def load_mod_bc(self, i, which, v, tag):
        m = self.mem
        a_sh, a_sc = (0, 1) if which == "mix" else (3, 4)
        gs = m.alloc(D, F32)
        sh = m.alloc(D, F32)
        tmp = m.alloc(D, F32)
        norm = self.din["norm_mix" if which == "mix" else "norm_ffn"]
        self.dma("sp", gs, self.modv[i, v, a_sc * D:(a_sc + 1) * D].partition_broadcast(128), ["modv"], [tag + "gs"])
        self.dma("sp", tmp, norm[i, :].partition_broadcast(128), (), [tag + "tmp"])
        self.dma("sp", sh, self.modv[i, v, a_sh * D:(a_sh + 1) * D].partition_broadcast(128), ["modv"], [tag + "sh"])
        self.stt(gs, gs, 1.0, tmp, ALU.add, ALU.mult, [tag + "gs", tag + "tmp"], [tag + "gs"])
        return gs, sh, tag + "gs", tag + "sh"

    def load_gate_bc(self, i, which, v, tag):
        a = 2 if which == "mix" else 5
        g = self.mem.alloc(D, F32)
        self.dma("sp", g, self.modv[i, v, a * D:(a + 1) * D].partition_broadcast(128), ["modv"], [tag])
        return g, tag

    def norm_setup(self, i, which, ctx_on):
        m = self.mem
        r = {}
        r["ident"] = m.alloc(128, BF16)
        self.dma("sp", r["ident"], self.din["ident"], (), ["ident"])
        r["mod"] = {0: self.load_mod_bc(i, which, 0, "mL")}
        if ctx_on:
            r["mod"][1] = self.load_mod_bc(i, which, 1, "mC")
        r["xring"] = self.ring("nx", 2, D, F32)
        r["tring"] = self.ring("ntmp", 2, D, F32)
        r["hring"] = self.ring("nh", 2, D, BF16)
        r["ss"] = self.ring("nss", 2, 1, F32)
        r["rs"] = self.ring("nrs", 2, 1, F32)
        r["junk"] = m.alloc(D, BF16)
        r["ptr"] = self.psring("ntr", [6, 7], bf16=True)
        return r

    def norm_block(self, r, blk, hT, hkey, hm_store=False):
        row0, ntok, v = blk
        gs, sh, gsk, shk = r["mod"][v]
        for t in range(ntok // 128):
            xs, xk = r["xring"]()
            self.dma("sp", xs, self.X[row0 + t * 128:row0 + (t + 1) * 128, :], ["X"], [xk])
            ss, sk = r["ss"]()
            rs, rk = r["rs"]()
            self.act(r["junk"], xs, AF.Square, [xk], ["njunk", sk], accum_out=ss)
            self.ts("dve", rs, ss, 1.0 / D, EPS, ALU.mult, ALU.add, [sk], [rk])
            self.rsqrt(rs, rk)
            tmp, tk = r["tring"]()
            self.stt(tmp, xs, rs[:, 0:1], gs, ALU.mult, ALU.mult, [xk, rk, gsk], [tk])
            hb, hk = r["hring"]()
            self.tt("pool", hb, tmp, sh, ALU.add, [tk, shk], [hk])
            if hm_store:
                self.dma("sp", self.Hm[row0 + t * 128:row0 + (t + 1) * 128, :], hb, [hk], ["Hm"])
            self.transpose_tile(r, hb, hk, 128, hT, hkey, t * 128)

    def transpose_tile(self, r, hb, hk, nrow, hT, hkey, col0):
        for g in range(2):
            pt, pk = r["ptr"]()
            for kk in range(8):
                k = g * 8 + kk
                self.tr(pt[:, kk * 128:kk * 128 + nrow], hb[0:nrow, k * 128:(k + 1) * 128], r["ident"][0:nrow, 0:nrow],
                        [hk, "ident"], [pk])
            src = pt.rearrange("p (k t) -> p k t", k=8)[:, :, 0:nrow]
            self.copy("act" if g == 0 else "dve", hT[:, g * 8:(g + 1) * 8, col0:col0 + nrow], src, [pk], [hkey])

    def phase_conv(self, i, j, ctx_on):
        m = self.mem
        r = self.norm_setup(i, "mix", ctx_on)
        hT = m.alloc((16, 1024), BF16)
        wring = self.ring("cw", 2, (16, 384), BF16)
        pring = self.psring("cp", [0, 1, 2, 3, 4, 5])
        ub = self.ring("cub", 2, 512, F32)
        vst = self.ring("cvst", 2, 512, BF16)
        bst = self.ring("cbst", 2, 512, BF16)
        win = self.din["conv_w_in_r"][j]
        for blk in self.cur_blocks:
            row0, ntok, v = blk
            self.norm_block(r, blk, hT, "hT")
            for n in range(16):
                wt, wk = wring()
                self.load_w(wt, wk, win[:, n * 384:(n + 1) * 384], 384)
                for m0 in range(0, ntok, 512):
                    mw = min(512, ntok - m0)
                    pB, kB = pring()
                    pC, kC = pring()
                    pU, kU = pring()
                    for (pp, pk, c0) in ((pB, kB, 0), (pC, kC, 128), (pU, kU, 256)):
                        for k in range(16):
                            self.mm(pp[:, 0:mw], wt[:, k, c0:c0 + 128], hT[:, k, m0:m0 + mw], k == 0, k == 15,
                                    [wk, "hT"], [pk])
                    u, uk = ub()
                    self.copy("act", u[:, 0:mw], pU[:, 0:mw], [kU], [uk])
                    vs, vk = vst()
                    self.tt("dve", vs[:, 0:mw], pC[:, 0:mw], u[:, 0:mw], ALU.mult, [kC, uk], [vk])
                    bs, bk = bst()
                    self.copy("act", bs[:, 0:mw], pB[:, 0:mw], [kB], [bk])
                    self.dma("sp", self.VT[n * 128:(n + 1) * 128, row0 + m0:row0 + m0 + mw], vs[:, 0:mw], [vk], ["VT"])
                    self.dma("sp", self.BT[n * 128:(n + 1) * 128, row0 + m0:row0 + m0 + mw], bs[:, 0:mw], [bk], ["BT"])
        self.end_phase()
        dw = m.alloc(48, F32)
        self.dma("sp", dw, self.din["conv_dw_cols"][j], (), ["dw"])
        PW = 2048
        vring = self.ring("c2v", 2, PW + 2, BF16)
        bring = self.ring("c2b", 2, PW, BF16)
        aring = self.ring("c2a", 2, PW, F32)
        gring = self.ring("c2g", 2, PW, BF16)
        segs = []
        if ctx_on:
            segs.append((0, CTX))
        segs.append((CTX, self.cfg.NTOK))
        for n in range(16):
            for (s0, s1) in segs:
                for t0 in range(s0, s1, PW):
                    w = min(PW, s1 - t0)
                    vp, vk = vring()
                    lo = t0 - 1
                    hi = t0 + w + 1
                    c0 = 0
                    if lo < s0:
                        self.memset("pool", vp[:, 0:1], 0.0, [vk])
                        lo = s0
                        c0 = 1
                    if hi > s1:
                        self.memset("pool", vp[:, w + 1:w + 2], 0.0, [vk])
                        hi = s1
                    self.dma("sp", vp[:, c0:c0 + hi - lo], self.VT[n * 128:(n + 1) * 128, lo:hi], ["VT"], [vk])
                    bp, bk = bring()
                    self.dma("sp", bp[:, 0:w], self.BT[n * 128:(n + 1) * 128, t0:t0 + w], ["BT"], [bk])
                    ac, ak = aring()
                    self.ts("dve", ac[:, 0:w], vp[:, 1:w + 1], dw[:, 16 + n:17 + n], None, ALU.mult, None, [vk, "dw"], [ak])
                    self.stt(ac[:, 0:w], vp[:, 0:w], dw[:, n:n + 1], ac[:, 0:w], ALU.mult, ALU.add, [vk, "dw", ak], [ak])
                    self.stt(ac[:, 0:w], vp[:, 2:w + 2], dw[:, 32 + n:33 + n], ac[:, 0:w], ALU.mult, ALU.add, [vk, "dw", ak], [ak])
                    gp, gk = gring()
                    self.tt("pool", gp[:, 0:w], ac[:, 0:w], bp[:, 0:w], ALU.mult, [ak, bk], [gk])
                    self.dma("sp", self.PT[n * 128:(n + 1) * 128, t0:t0 + w], gp[:, 0:w], [gk], ["PT"])
        self.end_phase()

    def phase_outproj(self, i, src, src_fm, w_ap, ctx_on):
        m = self.mem
        w = m.alloc((16, D), BF16)
        self.load_w(w, "ow", w_ap, D)
        gate = {0: self.load_gate_bc(i, "mix", 0, "ogL")}
        if ctx_on:
            gate[1] = self.load_gate_bc(i, "mix", 1, "ogC")
        aT = m.alloc((16, 1024), BF16)
        xring = self.ring("ox", 2, D, F32)
        tring = self.ring("ot", 2, 512, F32)
        pring = self.psring("op", [0, 1, 2, 3])
        if not src_fm:
            ident = m.alloc(128, BF16)
            self.dma("sp", ident, self.din["ident"], (), ["ident"])
            r = {"ident": ident, "ptr": self.psring("otr", [6, 7], bf16=True)}
            hring = self.ring("oh", 2, D, BF16)
        for blk in self.cur_blocks:
            row0, ntok, v = blk
            g, gk = gate[v]
            if src_fm:
                srcv = src.rearrange("(k p) t -> p k t", p=128)
                for k0 in range(0, 16, 4):
                    self.dma("sp", aT[:, k0:k0 + 4, 0:ntok], srcv[:, k0:k0 + 4, row0:row0 + ntok], ["PT"], ["aT"])
            else:
                for t in range(ntok // 128):
                    hb, hk = hring()
                    self.dma("sp", hb, src[row0 + t * 128:row0 + (t + 1) * 128, :], ["PM"], [hk])
                    self.transpose_tile(r, hb, hk, 128, aT, "aT", t * 128)
            for t in range(ntok // 128):
                xs, xk = xring()
                self.dma("sp", xs, self.X[row0 + t * 128:row0 + (t + 1) * 128, :], ["X"], [xk])
                for cb in range(4):
                    pt, pk = pring()
                    for k in range(16):
                        self.mm(pt, aT[:, k, t * 128:(t + 1) * 128], w[:, k, cb * 512:(cb + 1) * 512], k == 0, k == 15,
                                ["aT", "ow"], [pk])
                    tmp, tk = tring()
                    self.tt("dve", tmp, pt, g[:, cb * 512:(cb + 1) * 512], ALU.mult, [pk, gk], [tk])
                    self.tt("pool", xs[:, cb * 512:(cb + 1) * 512], xs[:, cb * 512:(cb + 1) * 512], tmp, ALU.add, [xk, tk], [xk])
                self.dma("sp", self.X[row0 + t * 128:row0 + (t + 1) * 128, :], xs, [xk], ["X"])
        self.end_phase()


    def phase_attn(self, i, j, ctx_on):
        cfg = self.cfg
        m = self.mem
        NTOK = cfg.NTOK
        NT = NTOK // 128
        QT, KT, Vm = self.VT, self.BT, self.Hm
        wqkv = self.din["diff_w_qkv"][j]
        r = self.norm_setup(i, "mix", True)
        hT = m.alloc((16, 1024), BF16)
        Pm = m.alloc(128, BF16)
        self.dma("sp", Pm, self.din["ropeP"], (), ["Pm"])
        cring = self.ring("rc", 2, 512, F32)
        sring = self.ring("rs", 2, 512, F32)
        wring = self.ring("aw", 2, (16, 512), BF16)
        xbr = self.ring("axb", 2, 512, BF16)
        t1r = self.ring("at1", 2, 512, F32)
        t2r = self.ring("at2", 2, 512, F32)
        ror = self.ring("aro", 2, 512, BF16)
        vsr = self.ring("avs", 2, 512, BF16)
        pa = self.psring("apa", [0, 1])
        pp = self.psring("app", [2, 3])
        pv = self.psring("apv", [4, 5])
        for blk in self.blocks(True):
            row0, ntok, v = blk
            self.norm_block(r, blk, hT, "hT")
            for wb in range(8):
                wt, wk = wring()
                self.load_w(wt, wk, wqkv[:, wb * 512:(wb + 1) * 512], 512)
                for m0 in range(0, ntok, 512):
                    mw = min(512, ntok - m0)
                    ct, ck = cring()
                    st, sk = sring()
                    self.dma("sp", ct[:, 0:mw], self.din["ropeC"][:, row0 + m0:row0 + m0 + mw], (), [ck])
                    self.dma("sp", st[:, 0:mw], self.din["ropeS"][:, row0 + m0:row0 + m0 + mw], (), [sk])
                    for c4 in range(4):
                        c = wb * 4 + c4
                        a_, ak = pa()
                        for k in range(16):
                            self.mm(a_[:, 0:mw], wt[:, k, c4 * 128:(c4 + 1) * 128], hT[:, k, m0:m0 + mw], k == 0, k == 15, [wk, "hT"], [ak])
                        xb, xk = xbr()
                        self.copy("act", xb[:, 0:mw], a_[:, 0:mw], [ak], [xk])
                        p_, pk = pp()
                        self.mm(p_[:, 0:mw], Pm, xb[:, 0:mw], True, True, ["Pm", xk], [pk])
                        t1, t1k = t1r()
                        self.tt("pool", t1[:, 0:mw], xb[:, 0:mw], ct[:, 0:mw], ALU.mult, [xk, ck], [t1k])
                        t2, t2k = t2r()
                        self.tt("dve", t2[:, 0:mw], p_[:, 0:mw], st[:, 0:mw], ALU.mult, [pk, sk], [t2k])
                        ro, rk = ror()
                        self.tt("dve", ro[:, 0:mw], t1[:, 0:mw], t2[:, 0:mw], ALU.add, [t1k, t2k], [rk])
                        dst = QT if c < 16 else KT
                        self.dma("sp", dst[(c % 16) * 128:(c % 16 + 1) * 128, row0 + m0:row0 + m0 + mw], ro[:, 0:mw], [rk], ["QK"])
            for vb in range(4):
                wt, wk = wring()
                self.load_w(wt, wk, wqkv[:, 4096 + vb * 512:4096 + (vb + 1) * 512], 512)
                for t in range(ntok // 128):
                    v_, vk = pv()
                    for k in range(16):
                        self.mm(v_, hT[:, k, t * 128:(t + 1) * 128], wt[:, k, :], k == 0, k == 15, ["hT", wk], [vk])
                    vs, vsk = vsr()
                    self.copy("act", vs, v_, [vk], [vsk])
                    self.dma("sp", Vm[row0 + t * 128:row0 + (t + 1) * 128, vb * 512:(vb + 1) * 512], vs, [vsk], ["Vm"])
        self.end_phase()
        lambda_init = 0.8 - 0.6 * math.exp(-0.3 * i)
        lp = m.alloc(512, F32, parts=1)
        self.dma("sp", lp, self.din["diff_lambda"][j], (), ["lp"])
        pr = m.alloc(256, F32, parts=1)
        sm = m.alloc(2, F32, parts=1)
        lam1 = m.alloc(1, F32, parts=1)
        self.tt("dve", pr[:, 0:128], lp[:, 0:128], lp[:, 128:256], ALU.mult, ["lp"], ["pr"])
        self.tt("dve", pr[:, 128:256], lp[:, 256:384], lp[:, 384:512], ALU.mult, ["lp"], ["pr"])
        self.S.op("dve", lambda e: e.reduce_sum(out=sm[:, 0:1], in_=pr[:, 0:128], axis=AX.X), ["pr"], ["sm"])
        self.S.op("dve", lambda e: e.reduce_sum(out=sm[:, 1:2], in_=pr[:, 128:256], axis=AX.X), ["pr"], ["sm"])
        self.act(sm, sm, AF.Exp, ["sm"], ["sm"])
        self.tt("dve", lam1, sm[:, 0:1], sm[:, 1:2], ALU.subtract, ["sm"], ["lam1"])
        self.ts("dve", lam1, lam1, -1.0, -lambda_init, ALU.mult, ALU.add, ["lam1"], ["lam1"])
        ones1 = m.alloc(128, F32, parts=1)
        self.memset("pool", ones1, 1.0, ["ones1"])
        neglam = m.alloc(1, F32)
        pl, plk = pa()
        self.mm(pl[:, 0:1], ones1, lam1, True, True, ["ones1", "lam1"], [plk])
        self.copy("dve", neglam, pl[:, 0:1], [plk], ["neglam"])
        sub = m.alloc(256, F32)
        self.dma("sp", sub, self.din["diff_subln"][j, :].partition_broadcast(128), (), ["sub"])
        self.ts("dve", sub, sub, 1.0 - lambda_init, None, ALU.mult, None, ["sub"], ["sub"])
        kring = self.ring("kth", 2, (2, NTOK), BF16)
        vring = self.ring("vh", 2, (NT, 257), BF16)
        for _ in range(2):
            vh, vhk = vring()
            self.memset("pool", vh[:, :, 256:257], 1.0, [vhk])
        qring = self.ring("qt", 2, (2, 256), BF16)
        ptr = self.ring("pT", 4, 512, BF16)
        psc = self.psring("psc", [4, 5, 6, 7])
        tar = self.ring("tA", 2, 256, F32)
        orr = self.ring("ao", 2, 256, F32)
        osr = self.ring("aos", 2, 256, BF16)
        smr = self.ring("asm", 2, 4, F32)
        junk = m.alloc(256, BF16)
        scale = 1.0 / math.sqrt(128.0)
        qblocks = []
        if ctx_on:
            qblocks.append((0, 0, 2))
        for q0 in range(CTX, NTOK, 256):
            qblocks.append((q0, 0, NT))
        for h in range(8):
            kth, kk = kring()
            vh, vhk = vring()
            for t in range(2):
                self.dma("sp", kth[:, t, :], KT[(2 * h + t) * 128:(2 * h + t + 1) * 128, :], ["QK"], [kk])
            vsrc = Vm[:, h * 256:(h + 1) * 256].rearrange("(kt p) c -> p kt c", p=128)
            for k0 in range(0, NT, 8):
                k1 = min(NT, k0 + 8)
                self.dma("sp", vh[:, k0:k1, 0:256], vsrc[:, k0:k1, :], ["Vm"], [vhk])
            for (q0, kt0, kt1) in qblocks:
                qt, qk = qring()
                for t in range(2):
                    self.dma("sp", qt[:, t, :], QT[(2 * h + t) * 128:(2 * h + t + 1) * 128, q0:q0 + 256], ["QK"], [qk])
                items = [(t, kt) for t in range(2) for kt in range(kt0, kt1, 2)]
                pend = []

                def issue_pv(it):
                    t, kt, pT, pTk = it
                    for j in range(2):
                        for qi in range(2):
                            b = t * 2 + qi
                            self.mm(self.ps[b][:, 0:257], pT[:, j * 256 + qi * 128:j * 256 + (qi + 1) * 128], vh[:, kt + j, :],
                                    (kt + j) == kt0, (kt + j) == kt1 - 1, [pTk, vhk], [("ps", b)])

                for (t, kt) in items:
                    s_, sk_ = psc()
                    for j in range(2):
                        self.mm(s_[:, j * 256:(j + 1) * 256], kth[:, t, (kt + j) * 128:(kt + j + 1) * 128], qt[:, t, :], True, True,
                                [kk, qk], [sk_])
                    pT, pTk = ptr()
                    self.act(pT, s_, AF.Exp, [sk_], [pTk], scale=scale)
                    pend.append((t, kt, pT, pTk))
                    if len(pend) > 1:
                        issue_pv(pend.pop(0))
                while pend:
                    issue_pv(pend.pop(0))
                for qi in range(2):
                    O0 = self.ps[qi]
                    O1 = self.ps[2 + qi]
                    k0_, k1_ = ("ps", qi), ("ps", 2 + qi)
                    s4, s4k = smr()
                    self.S.op("dve", lambda e, s4=s4, O0=O0: e.reciprocal(out=s4[:, 0:1], in_=O0[:, 256:257]), [k0_], [s4k])
                    self.S.op("dve", lambda e, s4=s4, O1=O1: e.reciprocal(out=s4[:, 1:2], in_=O1[:, 256:257]), [k1_], [s4k])
                    self.tt("dve", s4[:, 1:2], s4[:, 1:2], neglam, ALU.mult, [s4k, "neglam"], [s4k])
                    tA, tAk = tar()
                    self.ts("dve", tA, O0[:, 0:256], s4[:, 0:1], None, ALU.mult, None, [k0_, s4k], [tAk])
                    o, ok = orr()
                    self.stt(o, O1[:, 0:256], s4[:, 1:2], tA, ALU.mult, ALU.add, [k1_, s4k, tAk], [ok])
                    self.act(junk, o, AF.Square, [ok], ["ajunk", s4k], accum_out=s4[:, 2:3])
                    self.ts("dve", s4[:, 2:3], s4[:, 2:3], 1.0 / 256, 1e-5, ALU.mult, ALU.add, [s4k], [s4k])
                    self.rsqrt(s4[:, 2:3], s4k)
                    os_, osk = osr()
                    self.stt(os_, o, s4[:, 2:3], sub, ALU.mult, ALU.mult, [ok, s4k, "sub"], [osk])
                    self.dma("sp", self.PM[q0 + qi * 128:q0 + (qi + 1) * 128, h * 256:(h + 1) * 256], os_, [osk], ["PM"])
        self.end_phase()


    def phase_gla(self, i, j, ctx_on):
        cfg = self.cfg
        m = self.mem
        NTOK = cfg.NTOK
        NT = NTOK // 128
        QT, KT, Vm, Gm, SP, OF = self.VT, self.BT, self.Hm, self.Gm, self.SP, self.OF
        win = self.din["gla_w_in"][j]
        r = self.norm_setup(i, "mix", True)
        hT = m.alloc((16, 1024), BF16)
        wring = self.ring("gw", 2, (16, 512), BF16)
        w1 = [m.alloc((16, 16), BF16) for _ in range(2)]
        w2 = [m.alloc(1024, BF16, parts=16) for _ in range(2)]
        bb = [m.alloc(1024, F32) for _ in range(2)]
        one = m.alloc(1, F32)
        self.memset("pool", one, 1.0, ["one"])
        for dr in range(2):
            self.load_w(w1[dr], "gw1", self.din["gla_gate_w1"][j, dr], 16)
            self.S.dma("pool", w2[dr], self.din["gla_gate_w2"][j, dr], (), ["gw2"])
            self.dma("sp", bb[dr], self.din["gla_gate_b"][j, dr, :].partition_broadcast(128), (), ["gbb"])
        str_ = self.ring("gst", 2, 512, BF16)
        t1r = self.ring("gt1", 2, 1024, BF16, parts=16)
        zr = self.ring("gz", 2, 512, F32)
        spr = self.ring("gsp", 2, 512, F32)
        pa = self.psring("gpa", [0, 1, 2, 3])
        pz = self.psring("gpz", [4, 5])
        for blk in self.blocks(True):
            row0, ntok, v = blk
            self.norm_block(r, blk, hT, "hT")
            for wb in range(4):
                wt, wk = wring()
                self.load_w(wt, wk, win[:, wb * 512:(wb + 1) * 512], 512)
                for m0 in range(0, ntok, 512):
                    mw = min(512, ntok - m0)
                    for c4 in range(4):
                        c = wb * 4 + c4
                        a_, ak = pa()
                        for k in range(16):
                            self.mm(a_[:, 0:mw], wt[:, k, c4 * 128:(c4 + 1) * 128], hT[:, k, m0:m0 + mw], k == 0, k == 15, [wk, "hT"], [ak])
                        st, sk = str_()
                        if c < 8:
                            self.act(st[:, 0:mw], a_[:, 0:mw], AF.Copy, [ak], [sk], scale=1.0 / 16.0)
                        else:
                            self.copy("dve", st[:, 0:mw], a_[:, 0:mw], [ak], [sk])
                        dst = QT if c < 8 else KT
                        self.dma("sp", dst[(c % 8) * 128:(c % 8 + 1) * 128, row0 + m0:row0 + m0 + mw], st[:, 0:mw], [sk], ["QK"])
            for vb in range(8):
                wt, wk = wring()
                self.load_w(wt, wk, win[:, 2048 + vb * 512:2048 + (vb + 1) * 512], 512)
                for t in range(ntok // 128):
                    a_, ak = pa()
                    for k in range(16):
                        self.mm(a_, hT[:, k, t * 128:(t + 1) * 128], wt[:, k, :], k == 0, k == 15, ["hT", wk], [ak])
                    st, sk = str_()
                    self.copy("act" if t % 2 else "dve", st, a_, [ak], [sk])
                    dst = Vm if vb < 4 else Gm
                    self.dma("sp", dst[row0 + t * 128:row0 + (t + 1) * 128, (vb % 4) * 512:(vb % 4 + 1) * 512], st, [sk], ["VG"])
            for dr in range(2):
                t1, t1k = t1r()
                for m0 in range(0, ntok, 512):
                    mw = min(512, ntok - m0)
                    a_, ak = pz()
                    for k in range(16):
                        self.mm(a_[0:16, 0:mw], w1[dr][:, k, :], hT[:, k, m0:m0 + mw], k == 0, k == 15, ["gw1", "hT"], [ak])
                    self.copy("dve", t1[:, m0:m0 + mw], a_[0:16, 0:mw], [ak], [t1k])
                for t in range(ntok // 128):
                    for cb in range(2):
                        a_, ak = pa()
                        self.mm(a_, t1[:, t * 128:(t + 1) * 128], w2[dr][:, cb * 512:(cb + 1) * 512], True, True, [t1k, "gw2"], [ak])
                        z, zk = zr()
                        self.tt("dve", z, a_, bb[dr][:, cb * 512:(cb + 1) * 512], ALU.add, [ak, "gbb"], [zk])
                        self.act(z, z, AF.Exp, [zk], [zk], scale=-1.0)
                        sp, spk = spr()
                        self.act(sp, z, AF.Ln, [zk, "one"], [spk], bias=one[:, 0:1])
                        self.dma("sp", SP[dr, row0 + t * 128:row0 + (t + 1) * 128, cb * 512:(cb + 1) * 512], sp, [spk], ["SPd"])
        self.end_phase()
        ident = m.alloc(128, BF16)
        self.dma("sp", ident, self.din["ident"], (), ["ident"])
        onb = m.alloc(512, F32)
        self.dma("sp", onb, self.din["gla_onorm"][j, :].partition_broadcast(128), (), ["onb"])
        mask = [m.alloc(128, F32) for _ in range(2)]
        tri = [m.alloc(128, F32) for _ in range(2)]
        for dr in range(2):
            self.dma("sp", mask[dr], self.din["gmask"][dr], (), ["gmask"])
            self.dma("sp", tri[dr], self.din["gtri"][dr], (), ["gtri"])
        Sf = m.alloc((8, 512), F32)
        Sb = m.alloc((8, 512), BF16)
        qr = self.ring("sq", 2, (8, 128), BF16)
        kr = self.ring("sk", 2, (8, 128), BF16)
        vr = self.ring("sv", 2, D, BF16)
        gr = self.ring("sg", 2, D, BF16)
        spr2 = self.ring("ssp", 2, 1024, F32)
        ofr = self.ring("sof", 2, D, F32)
        pmr = self.ring("spm", 2, D, BF16)
        ebr = self.ring("seb", 4, 128, F32)
        enr = self.ring("sen", 4, 128, F32)
        qtr = self.ring("sqt", 4, (2, 128), BF16)
        ktr = self.ring("skt", 4, (2, 128), BF16)
        khr = self.ring("skh", 4, 128, BF16)
        khm = self.ring("skhm", 2, 256, BF16)
        atr = self.ring("sat", 2, 128, BF16)
        ostr = self.ring("sos", 2, 512, F32)
        onr = self.ring("son", 2, 512, F32)
        sgr = self.ring("ssg", 2, 512, F32)
        s4r = self.ring("ss4", 2, 2, F32)
        junk = m.alloc(512, BF16)
        pb = self.psring("spb", [0, 1])
        ptr_ = self.psring("sptr", [2], bf16=True)
        pA = self.psring("spA", [3])
        pO = self.psring("spO", [4, 5])
        pS = self.psring("spS", [6, 7])
        QTv = QT.rearrange("(c p) t -> p c t", p=128)
        KTv = KT.rearrange("(c p) t -> p c t", p=128)
        for dr in range(2):
            self.memset("pool", Sf, 0.0, ["Sf"])
            self.memset("pool", Sb, 0.0, ["Sb"])
            if dr == 0:
                order = list(range(NT))
            else:
                order = [1, 0] + list(range(NT - 1, 1, -1))
            endc = 127 if dr == 0 else 0
            for tt_ in order:
                rows = slice(tt_ * 128, (tt_ + 1) * 128)
                q_, qk = qr()
                k_, kk = kr()
                v_, vk = vr()
                sp_, spk = spr2()
                self.dma("sp", q_, QTv[:, 0:8, rows], ["QK"], [qk])
                self.dma("sp", k_, KTv[:, 0:8, rows], ["QK"], [kk])
                self.dma("sp", v_, Vm[rows, :], ["VG"], [vk])
                self.dma("sp", sp_, SP[dr, rows, :], ["SPd"], [spk])
                if dr == 1:
                    g_, gk = gr()
                    of_, ofk = ofr()
                    self.dma("sp", g_, Gm[rows, :], ["VG"], [gk])
                    self.dma("sp", of_, OF[rows, :], ["OFd"], [ofk])
                    pm_, pmk = pmr()
                else:
                    of_, ofk = ofr()
                for h in range(4):
                    qt, qtk = qtr()
                    kt, ktk = ktr()
                    kh, khk = khm()
                    ebs = []
                    for dc in range(2):
                        c = h * 2 + dc
                        b_, bk = pb()
                        self.mm(b_[:, 0:128], sp_[:, c * 128:(c + 1) * 128], tri[dr], True, True, [spk, "gtri"], [bk])
                        eb, ebk = ebr()
                        en, enk = enr()
                        self.act(eb, b_[:, 0:128], AF.Exp, [bk], [ebk])
                        self.act(en, b_[:, 0:128], AF.Exp, [bk], [enk], scale=-1.0)
                        self.tt("dve", qt[:, dc, :], q_[:, c, :], eb, ALU.mult, [qk, ebk], [qtk])
                        self.tt("pool", kt[:, dc, :], k_[:, c, :], en, ALU.mult, [kk, enk], [ktk])
                        khT, khTk = khr()
                        self.ts("dve", khT, kt[:, dc, :], eb[:, endc:endc + 1], None, ALU.mult, None, [ktk, ebk], [khTk])
                        p_, pk = ptr_()
                        self.tr(p_[:, 0:128], khT, ident, [khTk, "ident"], [pk])
                        self.copy("act", kh[:, dc * 128:(dc + 1) * 128], p_[:, 0:128], [pk], [khk])
                        ebs.append((eb, ebk))
                    a_, ak = pA()
                    for dc in range(2):
                        self.mm(a_[:, 0:128], kt[:, dc, :], qt[:, dc, :], dc == 0, dc == 1, [ktk, qtk], [ak])
                    at, atk = atr()
                    self.tt("dve", at, a_[:, 0:128], mask[dr], ALU.mult, [ak, "gmask"], [atk])
                    o_, ok = pO()
                    self.mm(o_, at, v_[:, h * 512:(h + 1) * 512], True, False, [atk, vk], [ok])
                    for dc in range(2):
                        self.mm(o_, qt[:, dc, :], Sb[:, h * 2 + dc, :], False, dc == 1, [qtk, "Sb"], [ok])
                    for dc in range(2):
                        c = h * 2 + dc
                        s_, sk = pS()
                        self.mm(s_, kh[:, dc * 128:(dc + 1) * 128], v_[:, h * 512:(h + 1) * 512], True, True, [khk, vk], [sk])
                        eb, ebk = ebs[dc]
                        self.stt(Sf[:, c, :], Sf[:, c, :], eb[:, endc:endc + 1], s_, ALU.mult, ALU.add, ["Sf", ebk, sk, ok], ["Sf"])
                        self.copy("act", Sb[:, c, :], Sf[:, c, :], ["Sf"], ["Sb"])
                    if dr == 0:
                        self.copy("act", of_[:, h * 512:(h + 1) * 512], o_, [ok], [ofk])
                    else:
                        os_, osk = ostr()
                        self.tt("dve", os_, o_, of_[:, h * 512:(h + 1) * 512], ALU.add, [ok, ofk], [osk])
                        s4, s4k = s4r()
                        self.act(junk, os_, AF.Square, [osk], ["gjunk", s4k], accum_out=s4[:, 0:1])
                        self.ts("dve", s4[:, 0:1], s4[:, 0:1], 1.0 / 512, EPS, ALU.mult, ALU.add, [s4k], [s4k])
                        self.rsqrt(s4[:, 0:1], s4k)
                        on, onk = onr()
                        self.stt(on, os_, s4[:, 0:1], onb, ALU.mult, ALU.mult, [osk, s4k, "onb"], [onk])
                        sg, sgk = sgr()
                        self.act(sg, g_[:, h * 512:(h + 1) * 512], AF.Silu, [gk], [sgk])
                        self.tt("pool", pm_[:, h * 512:(h + 1) * 512], on, sg, ALU.mult, [onk, sgk], [pmk])
                if dr == 0:
                    self.dma("sp", OF[rows, :], of_, [ofk], ["OFd"])
                else:
                    self.dma("sp", self.PM[rows, :], pm_, [pmk], ["PM"])
        self.end_phase()

    def phase_moe(self, i, ctx_on):
        cfg = self.cfg
        m = self.mem
        E = cfg.E
        NTOK = cfg.NTOK
        LAT = cfg.LAT
        aff = m.alloc(NTOK, F32, parts=E)
        r = self.norm_setup(i, "ffn", ctx_on)
        hT = m.alloc((16, 1024), BF16)
        rw = m.alloc((16, E), BF16)
        self.load_w(rw, "rw", self.din["router_w"][i], E)
        ones = m.alloc(E, BF16, parts=E)
        self.memset("pool", ones, 1.0, ["ones"])
        ex = self.ring("mex", 2, 512, BF16, parts=E)
        exf = self.ring("mexf", 2, 512, F32, parts=E)
        rc = self.ring("mrc", 2, 512, F32, parts=E)
        pring = self.psring("mp", [0, 1])
        pring2 = self.psring("mp2", [2, 3])
        for blk in self.cur_blocks:
            row0, ntok, v = blk
            self.norm_block(r, blk, hT, "hT", hm_store=True)
            for m0 in range(0, ntok, 512):
                mw = min(512, ntok - m0)
                pt, pk = pring()
                for k in range(16):
                    self.mm(pt[0:E, 0:mw], rw[:, k, :], hT[:, k, m0:m0 + mw], k == 0, k == 15, ["rw", "hT"], [pk])
                ef, efk = exf()
                self.act(ef[:, 0:mw], pt[0:E, 0:mw], AF.Exp, [pk], [efk])
                e_, ek = ex()
                self.copy("dve", e_[:, 0:mw], ef[:, 0:mw], [efk], [ek])
                lo, lk = ex()
                self.tt("dve", lo[:, 0:mw], ef[:, 0:mw], e_[:, 0:mw], ALU.subtract, [efk, ek], [lk])
                p2, pk2 = pring2()
                self.mm(p2[0:E, 0:mw], ones, e_[:, 0:mw], True, False, ["ones", ek], [pk2])
                self.mm(p2[0:E, 0:mw], ones, lo[:, 0:mw], False, True, ["ones", lk], [pk2])
                rr, rk = rc()
                self.S.op("dve", lambda e, rr=rr, p2=p2, mw=mw: e.reciprocal(out=rr[:, 0:mw], in_=p2[0:E, 0:mw]), [pk2], [rk])
                self.tt("dve", aff[:, row0 + m0:row0 + m0 + mw], ef[:, 0:mw], rr[:, 0:mw], ALU.mult, [efk, rk], ["aff"])
        self.barrier()
        segs = [(0, CTX, LAT, cfg.cap_l)]
        if ctx_on:
            segs.append((1, 0, CTX, cfg.cap_c))
        res = {}
        for (sid, c0, n, cap) in segs:
            vals = m.alloc(cap, F32, parts=E)
            idx = m.alloc(cap, U32, parts=E)
            src = aff[:, c0:c0 + n]
            vk, ik = "tv%d" % sid, "ti%d" % sid
            for rr_ in range(cap // 8):
                sl = slice(rr_ * 8, rr_ * 8 + 8)
                self.S.op("dve", lambda e, vals=vals, sl=sl, src=src: e.max(out=vals[:, sl], in_=src), ["aff"], [vk])
                self.S.op("dve", lambda e, vals=vals, idx=idx, sl=sl, src=src: e.max_index(out=idx[:, sl], in_max=vals[:, sl], in_values=src),
                          ["aff", vk], [ik])
                self.S.op("dve", lambda e, vals=vals, sl=sl, src=src: e.match_replace(out=src, in_to_replace=vals[:, sl], in_values=src, imm_value=-1.0),
                          [vk, ik], ["aff"])
            self.dma("sp", self.gate_d[sid, :, 0:cap], vals, [vk], ["gate_d"])
            self.dma("sp", self.idx_d[sid, :, 0:cap], idx, [ik], ["idx_d"])
            res[sid] = (c0, n, cap)
        self.end_phase()
        res2 = {}
        for sid in sorted(res):
            c0, n, cap = res[sid]
            gs = min(128, cap)
            ng = cap // gs
            idxT = m.alloc((E, ng), U32)
            gateT = m.alloc((E, ng), F32)
            for e_ in range(E):
                self.dma("sp", idxT[0:gs, e_, :], self.idx_d[sid, e_, 0:cap].rearrange("(g p) -> p g", p=gs), (), ["idxT%d" % sid],
                         allow_slow_non_contiguous=True)
                self.dma("sp", gateT[0:gs, e_, :], self.gate_d[sid, e_, 0:cap].rearrange("(g p) -> p g", p=gs), (), ["gateT%d" % sid],
                         allow_slow_non_contiguous=True)
            res2[sid] = (idxT, gateT, gs, ng, c0, n)
        res = res2
        groups = []
        slot = 0
        for sid in sorted(res):
            idxT, gateT, gs, ng, c0, n = res[sid]
            for g in range(ng):
                groups.append((sid, g, gs, slot))
                slot += gs
        NS = slot
        ident = m.alloc(128, BF16)
        self.dma("sp", ident, self.din["ident"], (), ["ident"])
        r = {"ident": ident, "ptr": self.psring("mtr", [6, 7], bf16=True)}
        g2 = {0: self.load_gate_bc(i, "ffn", 0, "g2L")}
        if ctx_on:
            g2[1] = self.load_gate_bc(i, "ffn", 1, "g2C")
        XT = m.alloc((16, NS), BF16)
        zT = m.alloc((12, NS), BF16)
        FB = 256
        wgr = self.ring("wg", 2, (16, FB), BF16)
        wur = self.ring("wu", 2, (16, FB), BF16)
        wd = m.alloc((12, D), BF16)
        xgr = self.ring("xg", 2, D, BF16)
        ysr = self.ring("ys", 2, D, F32)
        sar = self.ring("sa", 2, 512, F32)
        pA = self.psring("pA", [0, 1])
        pU = self.psring("pU", [2, 3])
        pY = self.psring("pY", [4, 5])
        mtiles = []
        nl = cfg.cap_l
        for s0 in range(0, nl, 512):
            mtiles.append((s0, min(512, nl - s0)))
        if ctx_on:
            mtiles.append((nl, NS - nl))
        pre = None
        for e in range(E):
            for (sid, g, gs, s0) in groups:
                idxT, gateT, _, _, c0, n = res[sid]
                xg, xk = xgr()
                srcrows = self.Hm
                ia = idxT[0:gs, e, g:g + 1]
                self.S.dma_custom("pool", lambda en, xg=xg, gs=gs, srcrows=srcrows, ia=ia, c0=c0: en.indirect_dma_start(
                    out=xg[0:gs, :], out_offset=None, in_=srcrows, in_offset=bass.IndirectOffsetOnAxis(ap=ia, axis=0),
                    element_offset=c0 * D),
                    ["Hm", "idxT%d" % sid], [xk])
                self.transpose_tile(r, xg, xk, gs, XT, "XT", s0)
            wdv = self.din["exp_w_down"][i, e].rearrange("(c p) n -> p c n", p=128)
            for fb in range(FF // FB):
                if fb == 0 and pre is not None:
                    wg, wgk, wu, wuk = pre
                    pre = None
                else:
                    wg, wgk = wgr()
                    wu, wuk = wur()
                    self.load_w(wg, wgk, self.din["exp_w_gate"][i, e][:, fb * FB:(fb + 1) * FB], FB)
                    self.load_w(wu, wuk, self.din["exp_w_up"][i, e][:, fb * FB:(fb + 1) * FB], FB)
                if fb == 1:
                    for c0_ in range(0, 12, 3):
                        self.S.dma("pool", wd[:, c0_:c0_ + 3, :], wdv[:, c0_:c0_ + 3, :], (), ["wd"])
                for c in range(FB // 128):
                    fc = fb * (FB // 128) + c
                    for (s0, w_) in mtiles:
                        a_, ak = pA()
                        u_, uk = pU()
                        for k in range(16):
                            self.mm(a_[:, 0:w_], wg[:, k, c * 128:(c + 1) * 128], XT[:, k, s0:s0 + w_], k == 0, k == 15, [wgk, "XT"], [ak])
                        for k in range(16):
                            self.mm(u_[:, 0:w_], wu[:, k, c * 128:(c + 1) * 128], XT[:, k, s0:s0 + w_], k == 0, k == 15, [wuk, "XT"], [uk])
                        sa, sk = sar()
                        self.act(sa[:, 0:w_], a_[:, 0:w_], AF.Silu, [ak], [sk])
                        self.tt("dve", zT[:, fc, s0:s0 + w_], sa[:, 0:w_], u_[:, 0:w_], ALU.mult, [sk, uk], ["zT"])
            if e + 1 < E:
                wg, wgk = wgr()
                wu, wuk = wur()
                self.load_w(wg, wgk, self.din["exp_w_gate"][i, e + 1][:, 0:FB], FB)
                self.load_w(wu, wuk, self.din["exp_w_up"][i, e + 1][:, 0:FB], FB)
                pre = (wg, wgk, wu, wuk)
            for (sid, g, gs, s0) in groups:
                idxT, gateT, _, _, c0, n = res[sid]
                gbc, gbk = g2[sid]
                ys, yk = ysr()
                for db in range(4):
                    y_, ypk = pY()
                    for c in range(12):
                        self.mm(y_[0:gs, :], zT[:, c, s0:s0 + gs], wd[:, c, db * 512:(db + 1) * 512], c == 0, c == 11, ["zT", "wd"], [ypk])
                    self.stt(ys[0:gs, db * 512:(db + 1) * 512], y_[0:gs, :], gateT[0:gs, e, g:g + 1], gbc[0:gs, db * 512:(db + 1) * 512],
                             ALU.mult, ALU.mult, [ypk, "gateT%d" % sid, gbk], [yk])
                dst = self.X
                ia = idxT[0:gs, e, g:g + 1]
                self.S.dma_custom("pool", lambda en, ys=ys, gs=gs, dst=dst, ia=ia, c0=c0: en.indirect_dma_start(
                    out=dst, out_offset=bass.IndirectOffsetOnAxis(ap=ia, axis=0), in_=ys[0:gs, :], in_offset=None, compute_op=ALU.add,
                    element_offset=c0 * D),
                    [yk, "idxT%d" % sid], ["X"])
        self.end_phase()

    def phase_final(self):
        m = self.mem
        g = m.alloc(D, F32)
        self.dma("sp", g, self.din["final_norm"][0, :].partition_broadcast(128), (), ["fg"])
        xring = self.ring("fx", 2, D, F32)
        oring = self.ring("fo", 2, D, F32)
        ss = self.ring("fss", 2, 1, F32)
        junk = m.alloc(D, BF16)
        for t in range(self.cfg.LAT // 128):
            xs, xk = xring()
            self.dma("sp", xs, self.X[CTX + t * 128:CTX + (t + 1) * 128, :], ["X"], [xk])
            s, sk = ss()
            self.act(junk, xs, AF.Square, [xk], ["fjunk", sk], accum_out=s)
            self.ts("dve", s, s, 1.0 / D, EPS, ALU.mult, ALU.add, [sk], [sk])
            self.rsqrt(s, sk)
            o, ok = oring()
            self.stt(o, xs, s[:, 0:1], g, ALU.mult, ALU.mult, [xk, sk, "fg"], [ok])
            self.dma("sp", self.out[t * 128:(t + 1) * 128, :], o, [ok], ["out"])


def prep_inputs(inp, cfg):
    f32 = np.float32
    d = {}
    d["xin"] = np.ascontiguousarray(inp["x"][0], dtype=f32)
    d["ctxin"] = np.ascontiguousarray(inp["ctx"][0], dtype=f32)
    cl = np.asarray(inp["c"][0], f32).reshape(16, 128).T
    cc = np.asarray(inp["c_ctx"], f32).reshape(16, 128).T
    d["ccols"] = np.ascontiguousarray(np.stack([cl, cc], axis=2).reshape(128, 32))
    for k in ("mod_w", "mod_b", "norm_mix", "norm_ffn", "router_w", "exp_w_gate", "exp_w_up", "exp_w_down", "conv_w_out"):
        d[k] = np.ascontiguousarray(inp[k], dtype=f32)
    d["final_norm"] = np.asarray(inp["final_norm"], f32).reshape(1, D)
    w = np.asarray(inp["conv_w_in"], f32)
    nA = w.shape[0]
    parts = [w[:, :, s * D:(s + 1) * D].reshape(nA, D, 16, 1, 128) for s in range(3)]
    d["conv_w_in_r"] = np.ascontiguousarray(np.concatenate(parts, axis=3).reshape(nA, D, 3 * D))
    dw = np.asarray(inp["conv_w_dw"], f32).reshape(nA, 3, 16, 128)
    d["conv_dw_cols"] = np.ascontiguousarray(dw.transpose(0, 3, 1, 2).reshape(nA, 128, 48))
    d["ident"] = np.eye(128, dtype=f32).astype(ml_dtypes.bfloat16)
    if cfg.nB:
        d["diff_w_qkv"] = np.ascontiguousarray(inp["diff_w_qkv"], dtype=f32)
        d["diff_lambda"] = np.asarray(inp["diff_lambda"], f32).reshape(cfg.nB, 1, 512)
        d["diff_subln"] = np.asarray(inp["diff_subln"], f32)
        d["diff_w_out"] = np.ascontiguousarray(inp["diff_w_out"], dtype=f32)
        n = np.arange(cfg.LAT)
        pos = [n // 64, n % 64]
        inv = 10000.0 ** (-np.arange(32, dtype=np.float64) / 32)
        C = np.ones((128, cfg.NTOK), np.float64)
        Sn = np.zeros((128, cfg.NTOK), np.float64)
        P = np.zeros((128, 128), np.float64)
        for dd in range(128):
            sct, within = dd // 64, dd % 64
            ang = pos[sct].astype(np.float64) * inv[within % 32]
            ang = (pos[sct].astype(np.float32) * inv[within % 32].astype(np.float32)).astype(np.float64)
            C[dd, CTX:] = np.cos(ang)
            Sn[dd, CTX:] = np.sin(ang)
            if within < 32:
                P[dd + 32, dd] = -1.0
            else:
                P[dd - 32, dd] = 1.0
        d["ropeC"] = C.astype(f32)
        d["ropeS"] = Sn.astype(f32)
        d["ropeP"] = P.astype(f32).astype(ml_dtypes.bfloat16)
    if cfg.nC:
        for k in ("gla_w_in", "gla_gate_w1", "gla_gate_w2", "gla_gate_b", "gla_onorm", "gla_w_out"):
            d[k] = np.ascontiguousarray(inp[k], dtype=f32)
        jj, ii = np.meshgrid(np.arange(128), np.arange(128), indexing="ij")
        mk = np.stack([(jj <= ii), (jj >= ii)]).astype(f32)
        d["gmask"] = mk
        d["gtri"] = (mk * (-1.0 / 16.0)).astype(f32)
    return d


def kernel(**inputs):
    cfg = Cfg()
    prog = Prog(cfg)
    nc = prog.build()
    d = prep_inputs(inputs, cfg)
    d = {k: v for k, v in d.items() if k in prog.din}
    res = run_bass_kernel_spmd(nc, [d], core_ids=[0])
    out = np.asarray(res.results[0]["out"], dtype=np.float32)
    return out.reshape(1, cfg.LAT, D)
```

```python
import numpy as np
import concourse.bass as bass
import concourse.mybir as mybir
from concourse.bass_utils import run_bass_kernel_spmd

F32 = mybir.dt.float32
BF16 = mybir.dt.bfloat16
I32 = mybir.dt.int32
U32 = mybir.dt.uint32
AF = mybir.ActivationFunctionType
ALU = mybir.AluOpType
AX = mybir.AxisListType


class Sched:
    ENG = ("pe", "act", "dve", "pool", "sp")

    def __init__(self, nc, es, n_dma_sems=12, same_engine_sync=True):
        self.nc = nc
        self.same = same_engine_sync
        self.thunks = {e: [] for e in self.ENG}
        self.sem = {e: es.enter_context(nc.semaphore("c_" + e)) for e in self.ENG}
        self.cnt = {e: 0 for e in self.ENG}
        self.seen = {e: {} for e in self.ENG}
        self.dsems = {}
        for q in ("sp", "pool", "act"):
            self.dsems[q] = [
                [es.enter_context(nc.semaphore("d_%s%d" % (q, i))), 0] for i in range(n_dma_sems)
            ]
        self.dnext = {q: 0 for q in self.dsems}
        self.lastw = {}
        self.reads = {}
        self.n_wait = 0

    def _need(self, eng, evs):
        best = {}
        for ev in evs:
            if ev is None:
                continue
            sem, name, val = ev
            if name == "c_" + eng and not self.same:
                continue
            if name == "c_pe" and eng == "pe":
                continue
            if self.seen[eng].get(name, 0) >= val:
                continue
            if name not in best or best[name][1] < val:
                best[name] = (sem, val)
        for name, (sem, val) in best.items():
            self.seen[eng][name] = val
            self.thunks[eng].append(lambda e, sem=sem, val=val: e.wait_ge(sem, val))
            self.n_wait += 1

    def _deps(self, reads, writes):
        evs = []
        for k in reads:
            evs.append(self.lastw.get(k))
        for k in writes:
            evs.append(self.lastw.get(k))
            evs.extend(self.reads.get(k, ()))
        return evs

    def _commit(self, ev, reads, writes):
        for k in reads:
            self.reads.setdefault(k, []).append(ev)
        for k in writes:
            self.lastw[k] = ev
            self.reads[k] = []

    def op(self, eng, fn, reads=(), writes=()):
        self._need(eng, self._deps(reads, writes))
        self.cnt[eng] += 1
        sem = self.sem[eng]
        self.thunks[eng].append(lambda e, fn=fn, sem=sem: fn(e).then_inc(sem, 1))
        ev = (sem, "c_" + eng, self.cnt[eng])
        self._commit(ev, reads, writes)
        return ev

    def dma(self, q, out, in_, reads=(), writes=(), **kw):
        slot = self.dsems[q][self.dnext[q] % len(self.dsems[q])]
        self.dnext[q] += 1
        sem, val = slot
        name = sem.name if hasattr(sem, "name") else str(id(sem))
        name = "d_%s_%d" % (q, (self.dnext[q] - 1) % len(self.dsems[q]))
        evs = self._deps(reads, writes)
        if val > 0:
            evs.append((sem, name, val))
        self._need(q, evs)
        slot[1] = val + 16
        self.thunks[q].append(
            lambda e, out=out, in_=in_, sem=sem, kw=kw: e.dma_start(out=out, in_=in_, **kw).then_inc(sem, 16)
        )
        ev = (sem, name, val + 16)
        self._commit(ev, reads, writes)
        return ev

    def dma_custom(self, q, fn, reads=(), writes=()):
        slot = self.dsems[q][self.dnext[q] % len(self.dsems[q])]
        name = "d_%s_%d" % (q, self.dnext[q] % len(self.dsems[q]))
        self.dnext[q] += 1
        sem, val = slot
        evs = self._deps(reads, writes)
        if val > 0:
            evs.append((sem, name, val))
        self._need(q, evs)
        slot[1] = val + 16
        self.thunks[q].append(lambda e, fn=fn, sem=sem: fn(e).then_inc(sem, 16))
        ev = (sem, name, val + 16)
        self._commit(ev, reads, writes)
        return ev

    def wait_all(self, eng):
        evs = []
        for k, ev in self.lastw.items():
            evs.append(ev)
        for k, l in self.reads.items():
            evs.extend(l)
        self._need(eng, evs)

    def emit(self):
        nc = self.nc
        with nc.Block() as block:
            @block.tensor
            def _(e):
                for t in self.thunks["pe"]:
                    t(e)

            @block.scalar
            def _(e):
                for t in self.thunks["act"]:
                    t(e)

            @block.vector
            def _(e):
                for t in self.thunks["dve"]:
                    t(e)

            @block.gpsimd
            def _(e):
                for t in self.thunks["pool"]:
                    t(e)

            @block.sync
            def _(e):
                for t in self.thunks["sp"]:
                    t(e)
        self.thunks = {e: [] for e in self.ENG}


import contextlib
import math
import numpy as np
import ml_dtypes

D = 2048
CTX = 256
FF = 1536
EPS = 1e-6


def prod(t):
    r = 1
    for a in t:
        r *= a
    return r


class Mem:
    def __init__(self, prog):
        self.prog = prog

    def alloc(self, free, dt, parts=128):
        if isinstance(free, int):
            free = (free,)
        n = prod(free)
        p = self.prog
        p.uid += 1
        t = p.pes.enter_context(p.nc.sbuf_tensor("b%d" % p.uid, [128, n], dt))
        v = t[:parts, :]
        if len(free) == 2:
            v = v.rearrange("p (a b) -> p a b", a=free[0])
        elif len(free) == 3:
            v = v.rearrange("p (a b c) -> p a b c", a=free[0], b=free[1])
        return v


class Cfg:
    def __init__(self, LAT=8192, E=16, DEPTH=4, CF=2):
        self.LAT = LAT
        self.E = E
        self.DEPTH = DEPTH
        self.NTOK = CTX + LAT
        self.cap_l = CF * LAT // E
        self.cap_c = CF * CTX // E
        self.nA = sum(1 for i in range(DEPTH) if i % 3 == 0)
        self.nB = sum(1 for i in range(DEPTH) if i % 3 == 1)
        self.nC = sum(1 for i in range(DEPTH) if i % 3 == 2)


class Prog:
    def __init__(self, cfg):
        self.cfg = cfg
        self.nc = bass.Bass("TRN2", target_bir_lowering=False)
        self.din = {}
        self.uid = 0

    def inp(self, name, shape, dt=F32):
        self.din[name] = self.nc.dram_tensor(name, list(shape), dt, kind="ExternalInput").ap()
        return self.din[name]

    def scratch(self, name, shape, dt):
        return self.nc.dram_tensor(name, list(shape), dt, kind="Internal").ap()

    def barrier(self):
        S = self.S
        evs = []
        for k, ev in S.lastw.items():
            evs.append(ev)
        for k, l in S.reads.items():
            evs.extend(l)
        for e in S.ENG:
            S._need(e, evs)
        S.lastw.clear()
        S.reads.clear()

    def begin_phase(self):
        self.pes = contextlib.ExitStack()
        self.nphase = getattr(self, "nphase", 0) + 1
        self.ps = [self.pes.enter_context(self.nc.psum_tensor("ps%d_%d" % (self.nphase, i), [128, 512], F32)) for i in range(8)]

    def end_phase(self, last=False):
        self.barrier()
        self.S.emit()
        self.pes.close()
        if not last:
            self.begin_phase()

    def act(self, out, in_, func, reads, writes, **kw):
        self.S.op("act", lambda e: e.activation(out=out, in_=in_, func=func, **kw), reads, writes)

    def ts(self, eng, out, in0, s1, s2, op0, op1, reads, writes, **kw):
        if op1 is None:
            self.S.op(eng, lambda e: e.tensor_scalar(out=out, in0=in0, scalar1=s1, scalar2=None, op0=op0, **kw), reads, writes)
        else:
            self.S.op(eng, lambda e: e.tensor_scalar(out=out, in0=in0, scalar1=s1, scalar2=s2, op0=op0, op1=op1, **kw), reads, writes)

    def tt(self, eng, out, in0, in1, op, reads, writes):
        self.S.op(eng, lambda e: e.tensor_tensor(out=out, in0=in0, in1=in1, op=op), reads, writes)

    def stt(self, out, in0, scalar, in1, op0, op1, reads, writes):
        self.S.op("dve", lambda e: e.scalar_tensor_tensor(out=out, in0=in0, scalar=scalar, in1=in1, op0=op0, op1=op1), reads, writes)

    def rsqrt(self, ap, key):
        self.S.op("act", lambda e: e.activation(out=ap, in_=ap, func=AF.Sqrt), [key], [key])
        self.S.op("dve", lambda e: e.reciprocal(out=ap, in_=ap), [key], [key])

    def copy(self, eng, out, in_, reads, writes):
        if eng == "act":
            self.S.op("act", lambda e: e.activation(out=out, in_=in_, func=AF.Copy), reads, writes)
        else:
            self.S.op(eng, lambda e: e.tensor_copy(out=out, in_=in_), reads, writes)

    def memset(self, eng, out, val, writes):
        self.S.op(eng, lambda e: e.memset(out, val), (), writes)

    def mm(self, out, lhsT, rhs, start, stop, reads, writes):
        self.S.op("pe", lambda e: e.matmul(out, lhsT=lhsT, rhs=rhs, start=start, stop=stop), reads, writes)

    def tr(self, out, in_, ident, reads, writes):
        self.S.op("pe", lambda e: e.transpose(out=out, in_=in_, identity=ident), reads, writes)

    def dma(self, q, out, in_, reads, writes, **kw):
        self.S.dma(q, out, in_, reads, writes, **kw)

    def ring(self, name, n, free, dt, parts=128):
        tiles = [self.mem.alloc(free, dt, parts) for _ in range(n)]
        st = {"i": 0}

        def nxt():
            i = st["i"] % n
            st["i"] += 1
            return tiles[i], (name, i)
        return nxt

    def psring(self, name, idxs, bf16=False):
        st = {"i": 0}

        def nxt():
            i = idxs[st["i"] % len(idxs)]
            st["i"] += 1
            t = self.ps[i][:]
            if bf16:
                t = t.bitcast(BF16)
            return t, ("ps", i)
        return nxt

    def load_w(self, dst, key, w_ap, ncols):
        kc = w_ap.shape[0] // 128
        src = w_ap.rearrange("(k p) n -> p k n", p=128)
        step = max(1, kc // 4)
        for k0 in range(0, kc, step):
            k1 = min(kc, k0 + step)
            self.S.dma("pool", dst[:, k0:k1, 0:ncols], src[:, k0:k1, :], (), [key])

    def build(self):
        cfg = self.cfg
        nc = self.nc
        LAT, E, DEPTH, NTOK = cfg.LAT, cfg.E, cfg.DEPTH, cfg.NTOK
        inp = self.inp
        inp("xin", [LAT, D])
        inp("ctxin", [CTX, D])
        inp("ccols", [128, 32])
        inp("mod_w", [DEPTH, D, 6 * D])
        inp("mod_b", [DEPTH, 6 * D])
        inp("norm_mix", [DEPTH, D])
        inp("norm_ffn", [DEPTH, D])
        inp("final_norm", [1, D])
        inp("conv_w_in_r", [cfg.nA, D, 3 * D])
        inp("conv_dw_cols", [cfg.nA, 128, 48])
        inp("conv_w_out", [cfg.nA, D, D])
        inp("router_w", [DEPTH, D, E])
        inp("exp_w_gate", [DEPTH, E, D, FF])
        inp("exp_w_up", [DEPTH, E, D, FF])
        inp("exp_w_down", [DEPTH, E, FF, D])
        inp("ident", [128, 128], BF16)
        if cfg.nB:
            inp("diff_w_qkv", [cfg.nB, D, 3 * D])
            inp("diff_lambda", [cfg.nB, 1, 512])
            inp("diff_subln", [cfg.nB, 256])
            inp("diff_w_out", [cfg.nB, D, D])
            inp("ropeC", [128, NTOK])
            inp("ropeS", [128, NTOK])
            inp("ropeP", [128, 128], BF16)
            self.PM = self.scratch("PM", [NTOK, D], BF16)
        if cfg.nC:
            inp("gla_w_in", [cfg.nC, D, 3 * D])
            inp("gla_gate_w1", [cfg.nC, 2, D, 16])
            inp("gla_gate_w2", [cfg.nC, 2, 16, 1024])
            inp("gla_gate_b", [cfg.nC, 2, 1024])
            inp("gla_onorm", [cfg.nC, 512])
            inp("gla_w_out", [cfg.nC, D, D])
            inp("gmask", [2, 128, 128])
            inp("gtri", [2, 128, 128])
            self.Gm = self.scratch("Gm", [NTOK, D], BF16)
            self.SP = self.scratch("SP", [2, NTOK, 1024], F32)
            self.OF = self.scratch("OF", [NTOK, D], F32)
            if not cfg.nB:
                self.PM = self.scratch("PM", [NTOK, D], BF16)
        self.out = nc.dram_tensor("out", [LAT, D], F32, kind="ExternalOutput").ap()
        self.X = self.scratch("X", [NTOK, D], F32)
        self.modv = self.scratch("modv", [DEPTH, 2, 6 * D], F32)
        self.Hm = self.scratch("Hm", [NTOK, D], BF16)
        self.VT = self.scratch("VT", [D, NTOK], BF16)
        self.BT = self.scratch("BT", [D, NTOK], BF16)
        self.PT = self.scratch("PT", [D, NTOK], BF16)
        self.idx_d = self.scratch("idx_d", [2, E, max(cfg.cap_l, 128)], U32)
        self.gate_d = self.scratch("gate_d", [2, E, max(cfg.cap_l, 128)], F32)
        with contextlib.ExitStack() as es:
            self.S = Sched(nc, es)
            self.mem = Mem(self)
            self.begin_phase()
            self.body()
            self.end_phase(last=True)
        return nc

    def body(self):
        cfg = self.cfg
        self.setup()
        for i in range(cfg.DEPTH):
            kind, j = i % 3, i // 3
            ctx_on = i < cfg.DEPTH - 1
            self.cur_blocks = self.blocks(ctx_on)
            self.phase_mod(i)
            if kind == 0:
                self.phase_conv(i, j, ctx_on)
                self.phase_outproj(i, self.PT, True, self.din["conv_w_out"][j], ctx_on)
            elif kind == 1:
                self.phase_attn(i, j, ctx_on)
                self.phase_outproj(i, self.PM, False, self.din["diff_w_out"][j], ctx_on)
            else:
                self.phase_gla(i, j, ctx_on)
                self.phase_outproj(i, self.PM, False, self.din["gla_w_out"][j], ctx_on)
            self.phase_moe(i, ctx_on)
        self.phase_final()

    def blocks(self, ctx_on):
        b = []
        if ctx_on:
            b.append((0, CTX, 1))
        for r in range(0, self.cfg.LAT, 1024):
            b.append((CTX + r, min(1024, self.cfg.LAT - r), 0))
        return b

    def setup(self):
        self.dma("sp", self.X[0:CTX, :], self.din["ctxin"], (), ["X"])
        LAT = self.cfg.LAT
        for r in range(0, LAT, 1024):
            self.dma("sp", self.X[CTX + r:CTX + r + 1024, :], self.din["xin"][r:r + 1024, :], (), ["X"])
        z = self.mem.alloc(D, BF16)
        self.memset("pool", z, 0.0, ["z"])
        for t in range(CTX // 128):
            self.dma("sp", self.Hm[t * 128:(t + 1) * 128, :], z, ["z"], ["Hm"])
        self.end_phase()

    def phase_mod(self, i):
        m = self.mem
        cc = m.alloc(32, F32)
        sc = m.alloc((16, 2), BF16)
        bias = m.alloc(6 * D, F32, parts=2)
        res = m.alloc(6 * D, F32, parts=2)
        wring = self.ring("modw", 2, (16, 512), BF16)
        pring = self.psring("modp", [0, 1])
        self.dma("sp", cc, self.din["ccols"], (), ["cc"])
        self.act(sc.rearrange("p k v -> p (k v)"), cc, AF.Silu, ["cc"], ["sc"])
        for v in range(2):
            self.dma("sp", bias[v:v + 1, :], self.din["mod_b"][i:i + 1, :], (), ["mbias"])
        for cb in range(6 * D // 512):
            wt, wk = wring()
            self.load_w(wt, wk, self.din["mod_w"][i][:, cb * 512:(cb + 1) * 512], 512)
            pt, pk = pring()
            for k in range(16):
                self.mm(pt[0:2, :], sc[:, k, :], wt[:, k, :], k == 0, k == 15, ["sc", wk], [pk])
            self.tt("dve", res[:, cb * 512:(cb + 1) * 512], pt[0:2, :], bias[:, cb * 512:(cb + 1) * 512], ALU.add,
                    [pk, "mbias"], ["mres"])
        self.dma("sp", self.modv[i], res, ["mres"], ["modv"])
        self.end_phase()

    def load_mod_bc(self, i, which, v, tag):
        m = self.mem
        a_sh, a_sc = (0, 1) if which == "mix" else (3, 4)
        gs = m.alloc(D, F32)
        sh = m.alloc(D, F32)
        tmp = m.alloc(D, F32)
        norm = self.din["norm_mix" if which == "mix" else "norm_ffn"]
        self.dma("sp", gs, self.modv[i, v, a_sc * D:(a_sc + 1) * D].partition_broadcast(128), ["modv"], [tag + "gs"])
        self.dma("sp", tmp, norm[i, :].partition_broadcast(128), (), [tag + "tmp"])
        self.dma("sp", sh, self.modv[i, v, a_sh * D:(a_sh + 1) * D].partition_broadcast(128), ["modv"], [tag + "sh"])
        self.stt(gs, gs, 1.0, tmp, ALU.add, ALU.mult, [tag + "gs", tag + "tmp"], [tag + "gs"])
        return gs, sh, tag + "gs", tag + "sh"

    def load_gate_bc(self, i, which, v, tag):
        a = 2 if which == "mix" else 5
        g = self.mem.alloc(D, F32)
        self.dma("sp", g, self.modv[i, v, a * D:(a + 1) * D].partition_broadcast(128), ["modv"], [tag])
        return g, tag

    def norm_setup(self, i, which, ctx_on):
        m = self.mem
        r = {}
        r["ident"] = m.alloc(128, BF16)
        self.dma("sp", r["ident"], self.din["ident"], (), ["ident"])
        r["mod"] = {0: self.load_mod_bc(i, which, 0, "mL")}
        if ctx_on:
            r["mod"][1] = self.load_mod_bc(i, which, 1, "mC")
        r["xring"] = self.ring("nx", 2, D, F32)
        r["tring"] = self.ring("ntmp", 2, D, F32)
        r["hring"] = self.ring("nh", 2, D, BF16)
        r["ss"] = self.ring("nss", 2, 1, F32)
        r["rs"] = self.ring("nrs", 2, 1, F32)
        r["junk"] = m.alloc(D, BF16)
        r["ptr"] = self.psring("ntr", [6, 7], bf16=True)
        return r

    def norm_block(self, r, blk, hT, hkey, hm_store=False):
        row0, ntok, v = blk
        gs, sh, gsk, shk = r["mod"][v]
        nt_ = ntok // 128

        def ld(t):
            xs_, xk_ = r["xring"]()
            self.dma("sp", xs_, self.X[row0 + t * 128:row0 + (t + 1) * 128, :], ["X"], [xk_])
            return xs_, xk_
        nxt = ld(0)
        for t in range(nt_):
            xs, xk = nxt
            if t + 1 < nt_:
                nxt = ld(t + 1)
            ss, sk = r["ss"]()
            rs, rk = r["rs"]()
            self.act(r["junk"], xs, AF.Square, [xk], ["njunk", sk], accum_out=ss)
            self.ts("dve", rs, ss, 1.0 / D, EPS, ALU.mult, ALU.add, [sk], [rk])
            self.rsqrt(rs, rk)
            tmp, tk = r["tring"]()
            self.stt(tmp, xs, rs[:, 0:1], gs, ALU.mult, ALU.mult, [xk, rk, gsk], [tk])
            hb, hk = r["hring"]()
            self.tt("pool", hb, tmp, sh, ALU.add, [tk, shk], [hk])
            if hm_store:
                self.dma("sp", self.Hm[row0 + t * 128:row0 + (t + 1) * 128, :], hb, [hk], ["Hm"])
            self.transpose_tile(r, hb, hk, 128, hT, hkey, t * 128)

    def transpose_tile(self, r, hb, hk, nrow, hT, hkey, col0):
        for g in range(2):
            pt, pk = r["ptr"]()
            for kk in range(8):
                k = g * 8 + kk
                self.tr(pt[:, kk * 128:kk * 128 + nrow], hb[0:nrow, k * 128:(k + 1) * 128], r["ident"][0:nrow, 0:nrow],
                        [hk, "ident"], [pk])
            src = pt.rearrange("p (k t) -> p k t", k=8)[:, :, 0:nrow]
            self.copy("act" if g == 0 else "dve", hT[:, g * 8:(g + 1) * 8, col0:col0 + nrow], src, [pk], [hkey])

    def phase_conv(self, i, j, ctx_on):
        m = self.mem
        r = self.norm_setup(i, "mix", ctx_on)
        hT = m.alloc((16, 1024), BF16)
        wring = self.ring("cw", 2, (16, 384), BF16)
        pring = self.psring("cp", [0, 1, 2, 3, 4, 5])
        ub = self.ring("cub", 2, 512, F32)
        vst = self.ring("cvst", 2, 512, BF16)
        bst = self.ring("cbst", 2, 512, BF16)
        win = self.din["conv_w_in_r"][j]
        for blk in self.cur_blocks:
            row0, ntok, v = blk
            self.norm_block(r, blk, hT, "hT")
            for n in range(16):
                wt, wk = wring()
                self.load_w(wt, wk, win[:, n * 384:(n + 1) * 384], 384)
                for m0 in range(0, ntok, 512):
                    mw = min(512, ntok - m0)
                    pB, kB = pring()
                    pC, kC = pring()
                    pU, kU = pring()
                    for (pp, pk, c0) in ((pB, kB, 0), (pC, kC, 128), (pU, kU, 256)):
                        for k in range(16):
                            self.mm(pp[:, 0:mw], wt[:, k, c0:c0 + 128], hT[:, k, m0:m0 + mw], k == 0, k == 15,
                                    [wk, "hT"], [pk])
                    u, uk = ub()
                    self.copy("act", u[:, 0:mw], pU[:, 0:mw], [kU], [uk])
                    vs, vk = vst()
                    self.tt("dve", vs[:, 0:mw], pC[:, 0:mw], u[:, 0:mw], ALU.mult, [kC, uk], [vk])
                    bs, bk = bst()
                    self.copy("act", bs[:, 0:mw], pB[:, 0:mw], [kB], [bk])
                    self.dma("sp", self.VT[n * 128:(n + 1) * 128, row0 + m0:row0 + m0 + mw], vs[:, 0:mw], [vk], ["VT"])
                    self.dma("sp", self.BT[n * 128:(n + 1) * 128, row0 + m0:row0 + m0 + mw], bs[:, 0:mw], [bk], ["BT"])
        self.end_phase()
        dw = m.alloc(48, F32)
        self.dma("sp", dw, self.din["conv_dw_cols"][j], (), ["dw"])
        PW = 2048
        vring = self.ring("c2v", 2, PW + 2, BF16)
        bring = self.ring("c2b", 2, PW, BF16)
        aring = self.ring("c2a", 2, PW, F32)
        gring = self.ring("c2g", 2, PW, BF16)
        segs = []
        if ctx_on:
            segs.append((0, CTX))
        segs.append((CTX, self.cfg.NTOK))
        for n in range(16):
            for (s0, s1) in segs:
                for t0 in range(s0, s1, PW):
                    w = min(PW, s1 - t0)
                    vp, vk = vring()
                    lo = t0 - 1
                    hi = t0 + w + 1
                    c0 = 0
                    if lo < s0:
                        self.memset("pool", vp[:, 0:1], 0.0, [vk])
                        lo = s0
                        c0 = 1
                    if hi > s1:
                        self.memset("pool", vp[:, w + 1:w + 2], 0.0, [vk])
                        hi = s1
                    self.dma("sp", vp[:, c0:c0 + hi - lo], self.VT[n * 128:(n + 1) * 128, lo:hi], ["VT"], [vk])
                    bp, bk = bring()
                    self.dma("sp", bp[:, 0:w], self.BT[n * 128:(n + 1) * 128, t0:t0 + w], ["BT"], [bk])
                    ac, ak = aring()
                    self.ts("dve", ac[:, 0:w], vp[:, 1:w + 1], dw[:, 16 + n:17 + n], None, ALU.mult, None, [vk, "dw"], [ak])
                    self.stt(ac[:, 0:w], vp[:, 0:w], dw[:, n:n + 1], ac[:, 0:w], ALU.mult, ALU.add, [vk, "dw", ak], [ak])
                    self.stt(ac[:, 0:w], vp[:, 2:w + 2], dw[:, 32 + n:33 + n], ac[:, 0:w], ALU.mult, ALU.add, [vk, "dw", ak], [ak])
                    gp, gk = gring()
                    self.tt("pool", gp[:, 0:w], ac[:, 0:w], bp[:, 0:w], ALU.mult, [ak, bk], [gk])
                    self.dma("sp", self.PT[n * 128:(n + 1) * 128, t0:t0 + w], gp[:, 0:w], [gk], ["PT"])
        self.end_phase()

    def phase_outproj(self, i, src, src_fm, w_ap, ctx_on):
        m = self.mem
        w = m.alloc((16, D), BF16)
        self.load_w(w, "ow", w_ap, D)
        gate = {0: self.load_gate_bc(i, "mix", 0, "ogL")}
        if ctx_on:
            gate[1] = self.load_gate_bc(i, "mix", 1, "ogC")
        aT = m.alloc((16, 1024), BF16)
        xring = self.ring("ox", 2, D, F32)
        tring = self.ring("ot", 2, 512, F32)
        pring = self.psring("op", [0, 1, 2, 3])
        if not src_fm:
            ident = m.alloc(128, BF16)
            self.dma("sp", ident, self.din["ident"], (), ["ident"])
            r = {"ident": ident, "ptr": self.psring("otr", [6, 7], bf16=True)}
            hring = self.ring("oh", 2, D, BF16)
        for blk in self.cur_blocks:
            row0, ntok, v = blk
            g, gk = gate[v]
            if src_fm:
                srcv = src.rearrange("(k p) t -> p k t", p=128)
                for k0 in range(0, 16, 4):
                    self.dma("sp", aT[:, k0:k0 + 4, 0:ntok], srcv[:, k0:k0 + 4, row0:row0 + ntok], ["PT"], ["aT"])
            else:
                for t in range(ntok // 128):
                    hb, hk = hring()
                    self.dma("sp", hb, src[row0 + t * 128:row0 + (t + 1) * 128, :], ["PM"], [hk])
                    self.transpose_tile(r, hb, hk, 128, aT, "aT", t * 128)
            nt_ = ntok // 128

            def ldx(t, row0=row0):
                xs_, xk_ = xring()
                self.dma("sp", xs_, self.X[row0 + t * 128:row0 + (t + 1) * 128, :], ["X"], [xk_])
                return xs_, xk_
            nxt = ldx(0)
            for t in range(nt_):
                xs, xk = nxt
                if t + 1 < nt_:
                    nxt = ldx(t + 1)
                for cb in range(4):
                    pt, pk = pring()
                    for k in range(16):
                        self.mm(pt, aT[:, k, t * 128:(t + 1) * 128], w[:, k, cb * 512:(cb + 1) * 512], k == 0, k == 15,
                                ["aT", "ow"], [pk])
                    tmp, tk = tring()
                    self.tt("dve", tmp, pt, g[:, cb * 512:(cb + 1) * 512], ALU.mult, [pk, gk], [tk])
                    self.tt("pool", xs[:, cb * 512:(cb + 1) * 512], xs[:, cb * 512:(cb + 1) * 512], tmp, ALU.add, [xk, tk], [xk])
                self.dma("sp", self.X[row0 + t * 128:row0 + (t + 1) * 128, :], xs, [xk], ["X"])
        self.end_phase()


    def phase_attn(self, i, j, ctx_on):
        cfg = self.cfg
        m = self.mem
        NTOK = cfg.NTOK
        NT = NTOK // 128
        QT, KT, Vm = self.VT, self.BT, self.Hm
        wqkv = self.din["diff_w_qkv"][j]
        r = self.norm_setup(i, "mix", True)
        hT = m.alloc((16, 1024), BF16)
        Pm = m.alloc(128, BF16)
        self.dma("sp", Pm, self.din["ropeP"], (), ["Pm"])
        cring = self.ring("rc", 2, 512, F32)
        sring = self.ring("rs", 2, 512, F32)
        wring = self.ring("aw", 2, (16, 512), BF16)
        xbr = self.ring("axb", 2, 512, BF16)
        t1r = self.ring("at1", 2, 512, F32)
        t2r = self.ring("at2", 2, 512, F32)
        ror = self.ring("aro", 2, 512, BF16)
        vsr = self.ring("avs", 2, 512, BF16)
        pa = self.psring("apa", [0, 1])
        pp = self.psring("app", [2, 3])
        pv = self.psring("apv", [4, 5])
        for blk in self.blocks(True):
            row0, ntok, v = blk
            self.norm_block(r, blk, hT, "hT")
            for wb in range(8):
                wt, wk = wring()
                self.load_w(wt, wk, wqkv[:, wb * 512:(wb + 1) * 512], 512)
                for m0 in range(0, ntok, 512):
                    mw = min(512, ntok - m0)
                    ct, ck = cring()
                    st, sk = sring()
                    self.dma("sp", ct[:, 0:mw], self.din["ropeC"][:, row0 + m0:row0 + m0 + mw], (), [ck])
                    self.dma("sp", st[:, 0:mw], self.din["ropeS"][:, row0 + m0:row0 + m0 + mw], (), [sk])
                    for c4 in range(4):
                        c = wb * 4 + c4
                        a_, ak = pa()
                        for k in range(16):
                            self.mm(a_[:, 0:mw], wt[:, k, c4 * 128:(c4 + 1) * 128], hT[:, k, m0:m0 + mw], k == 0, k == 15, [wk, "hT"], [ak])
                        xb, xk = xbr()
                        self.copy("act", xb[:, 0:mw], a_[:, 0:mw], [ak], [xk])
                        p_, pk = pp()
                        self.mm(p_[:, 0:mw], Pm, xb[:, 0:mw], True, True, ["Pm", xk], [pk])
                        t1, t1k = t1r()
                        self.tt("pool", t1[:, 0:mw], xb[:, 0:mw], ct[:, 0:mw], ALU.mult, [xk, ck], [t1k])
                        t2, t2k = t2r()
                        self.tt("dve", t2[:, 0:mw], p_[:, 0:mw], st[:, 0:mw], ALU.mult, [pk, sk], [t2k])
                        ro, rk = ror()
                        self.tt("dve", ro[:, 0:mw], t1[:, 0:mw], t2[:, 0:mw], ALU.add, [t1k, t2k], [rk])
                        dst = QT if c < 16 else KT
                        self.dma("sp", dst[(c % 16) * 128:(c % 16 + 1) * 128, row0 + m0:row0 + m0 + mw], ro[:, 0:mw], [rk], ["QK"])
            for vb in range(4):
                wt, wk = wring()
                self.load_w(wt, wk, wqkv[:, 4096 + vb * 512:4096 + (vb + 1) * 512], 512)
                for t in range(ntok // 128):
                    v_, vk = pv()
                    for k in range(16):
                        self.mm(v_, hT[:, k, t * 128:(t + 1) * 128], wt[:, k, :], k == 0, k == 15, ["hT", wk], [vk])
                    vs, vsk = vsr()
                    self.copy("act", vs, v_, [vk], [vsk])
                    self.dma("sp", Vm[row0 + t * 128:row0 + (t + 1) * 128, vb * 512:(vb + 1) * 512], vs, [vsk], ["Vm"])
        self.end_phase()
        lambda_init = 0.8 - 0.6 * math.exp(-0.3 * i)
        lp = m.alloc(512, F32, parts=1)
        self.dma("sp", lp, self.din["diff_lambda"][j], (), ["lp"])
        pr = m.alloc(256, F32, parts=1)
        sm = m.alloc(2, F32, parts=1)
        lam1 = m.alloc(1, F32, parts=1)
        self.tt("dve", pr[:, 0:128], lp[:, 0:128], lp[:, 128:256], ALU.mult, ["lp"], ["pr"])
        self.tt("dve", pr[:, 128:256], lp[:, 256:384], lp[:, 384:512], ALU.mult, ["lp"], ["pr"])
        self.S.op("dve", lambda e: e.reduce_sum(out=sm[:, 0:1], in_=pr[:, 0:128], axis=AX.X), ["pr"], ["sm"])
        self.S.op("dve", lambda e: e.reduce_sum(out=sm[:, 1:2], in_=pr[:, 128:256], axis=AX.X), ["pr"], ["sm"])
        self.act(sm, sm, AF.Exp, ["sm"], ["sm"])
        self.tt("dve", lam1, sm[:, 0:1], sm[:, 1:2], ALU.subtract, ["sm"], ["lam1"])
        self.ts("dve", lam1, lam1, -1.0, -lambda_init, ALU.mult, ALU.add, ["lam1"], ["lam1"])
        ones1 = m.alloc(128, F32, parts=1)
        self.memset("pool", ones1, 1.0, ["ones1"])
        neglam = m.alloc(1, F32)
        pl, plk = pa()
        self.mm(pl[:, 0:1], ones1, lam1, True, True, ["ones1", "lam1"], [plk])
        self.copy("dve", neglam, pl[:, 0:1], [plk], ["neglam"])
        sub = m.alloc(256, F32)
        self.dma("sp", sub, self.din["diff_subln"][j, :].partition_broadcast(128), (), ["sub"])
        self.ts("dve", sub, sub, 1.0 - lambda_init, None, ALU.mult, None, ["sub"], ["sub"])
        kring = self.ring("kth", 2, (2, NTOK), BF16)
        vring = self.ring("vh", 2, (NT, 257), BF16)
        for _ in range(2):
            vh, vhk = vring()
            self.memset("pool", vh[:, :, 256:257], 1.0, [vhk])
        qring = self.ring("qt", 2, (2, 256), BF16)
        ptr = self.ring("pT", 4, 512, BF16)
        psc = self.psring("psc", [4, 5, 6, 7])
        tar = self.ring("tA", 2, 256, F32)
        orr = self.ring("ao", 2, 256, F32)
        osr = self.ring("aos", 2, 256, BF16)
        smr = self.ring("asm", 2, 4, F32)
        junk = m.alloc(256, BF16)
        scale = 1.0 / math.sqrt(128.0)
        qblocks = []
        if ctx_on:
            qblocks.append((0, 0, 2))
        for q0 in range(CTX, NTOK, 256):
            qblocks.append((q0, 0, NT))
        for h in range(8):
            kth, kk = kring()
            vh, vhk = vring()
            for t in range(2):
                self.dma("sp", kth[:, t, :], KT[(2 * h + t) * 128:(2 * h + t + 1) * 128, :], ["QK"], [kk])
            vsrc = Vm[:, h * 256:(h + 1) * 256].rearrange("(kt p) c -> p kt c", p=128)
            for k0 in range(0, NT, 8):
                k1 = min(NT, k0 + 8)
                self.dma("sp", vh[:, k0:k1, 0:256], vsrc[:, k0:k1, :], ["Vm"], [vhk])
            def ldq(q0, h=h):
                qt_, qk_ = qring()
                for t in range(2):
                    self.dma("sp", qt_[:, t, :], QT[(2 * h + t) * 128:(2 * h + t + 1) * 128, q0:q0 + 256], ["QK"], [qk_])
                return qt_, qk_
            nq = ldq(qblocks[0][0])
            for qbi, (q0, kt0, kt1) in enumerate(qblocks):
                qt, qk = nq
                if qbi + 1 < len(qblocks):
                    nq = ldq(qblocks[qbi + 1][0])
                items = [(t, kt) for t in range(2) for kt in range(kt0, kt1, 2)]
                pend = []

                def issue_pv(it):
                    t, kt, pT, pTk = it
                    for j in range(2):
                        for qi in range(2):
                            b = t * 2 + qi
                            self.mm(self.ps[b][:, 0:257], pT[:, j * 256 + qi * 128:j * 256 + (qi + 1) * 128], vh[:, kt + j, :],
                                    (kt + j) == kt0, (kt + j) == kt1 - 1, [pTk, vhk], [("ps", b)])

                for (t, kt) in items:
                    s_, sk_ = psc()
                    for j in range(2):
                        self.mm(s_[:, j * 256:(j + 1) * 256], kth[:, t, (kt + j) * 128:(kt + j + 1) * 128], qt[:, t, :], True, True,
                                [kk, qk], [sk_])
                    pT, pTk = ptr()
                    self.act(pT, s_, AF.Exp, [sk_], [pTk], scale=scale)
                    pend.append((t, kt, pT, pTk))
                    if len(pend) > 1:
                        issue_pv(pend.pop(0))
                while pend:
                    issue_pv(pend.pop(0))
                for qi in range(2):
                    O0 = self.ps[qi]
                    O1 = self.ps[2 + qi]
                    k0_, k1_ = ("ps", qi), ("ps", 2 + qi)
                    s4, s4k = smr()
                    self.S.op("dve", lambda e, s4=s4, O0=O0: e.reciprocal(out=s4[:, 0:1], in_=O0[:, 256:257]), [k0_], [s4k])
                    self.S.op("dve", lambda e, s4=s4, O1=O1: e.reciprocal(out=s4[:, 1:2], in_=O1[:, 256:257]), [k1_], [s4k])
                    self.tt("dve", s4[:, 1:2], s4[:, 1:2], neglam, ALU.mult, [s4k, "neglam"], [s4k])
                    tA, tAk = tar()
                    self.ts("dve", tA, O0[:, 0:256], s4[:, 0:1], None, ALU.mult, None, [k0_, s4k], [tAk])
                    o, ok = orr()
                    self.stt(o, O1[:, 0:256], s4[:, 1:2], tA, ALU.mult, ALU.add, [k1_, s4k, tAk], [ok])
                    self.act(junk, o, AF.Square, [ok], ["ajunk", s4k], accum_out=s4[:, 2:3])
                    self.ts("dve", s4[:, 2:3], s4[:, 2:3], 1.0 / 256, 1e-5, ALU.mult, ALU.add, [s4k], [s4k])
                    self.rsqrt(s4[:, 2:3], s4k)
                    os_, osk = osr()
                    self.stt(os_, o, s4[:, 2:3], sub, ALU.mult, ALU.mult, [ok, s4k, "sub"], [osk])
                    self.dma("sp", self.PM[q0 + qi * 128:q0 + (qi + 1) * 128, h * 256:(h + 1) * 256], os_, [osk], ["PM"])
        self.end_phase()


    def phase_gla(self, i, j, ctx_on):
        cfg = self.cfg
        m = self.mem
        NTOK = cfg.NTOK
        NT = NTOK // 128
        QT, KT, Vm, Gm, SP, OF = self.VT, self.BT, self.Hm, self.Gm, self.SP, self.OF
        win = self.din["gla_w_in"][j]
        r = self.norm_setup(i, "mix", True)
        hT = m.alloc((16, 1024), BF16)
        wring = self.ring("gw", 2, (16, 512), BF16)
        w1 = [m.alloc((16, 16), BF16) for _ in range(2)]
        w2 = [m.alloc(1024, BF16, parts=16) for _ in range(2)]
        bb = [m.alloc(1024, F32) for _ in range(2)]
        one = m.alloc(1, F32)
        self.memset("pool", one, 1.0, ["one"])
        for dr in range(2):
            self.load_w(w1[dr], "gw1", self.din["gla_gate_w1"][j, dr], 16)
            self.S.dma("pool", w2[dr], self.din["gla_gate_w2"][j, dr], (), ["gw2"])
            self.dma("sp", bb[dr], self.din["gla_gate_b"][j, dr, :].partition_broadcast(128), (), ["gbb"])
        str_ = self.ring("gst", 2, 512, BF16)
        t1r = self.ring("gt1", 2, 1024, BF16, parts=16)
        zr = self.ring("gz", 2, 512, F32)
        spr = self.ring("gsp", 2, 512, F32)
        pa = self.psring("gpa", [0, 1, 2, 3])
        pz = self.psring("gpz", [4, 5])
        for blk in self.blocks(True):
            row0, ntok, v = blk
            self.norm_block(r, blk, hT, "hT")
            for wb in range(4):
                wt, wk = wring()
                self.load_w(wt, wk, win[:, wb * 512:(wb + 1) * 512], 512)
                for m0 in range(0, ntok, 512):
                    mw = min(512, ntok - m0)
                    for c4 in range(4):
                        c = wb * 4 + c4
                        a_, ak = pa()
                        for k in range(16):
                            self.mm(a_[:, 0:mw], wt[:, k, c4 * 128:(c4 + 1) * 128], hT[:, k, m0:m0 + mw], k == 0, k == 15, [wk, "hT"], [ak])
                        st, sk = str_()
                        if c < 8:
                            self.act(st[:, 0:mw], a_[:, 0:mw], AF.Copy, [ak], [sk], scale=1.0 / 16.0)
                        else:
                            self.copy("dve", st[:, 0:mw], a_[:, 0:mw], [ak], [sk])
                        dst = QT if c < 8 else KT
                        self.dma("sp", dst[(c % 8) * 128:(c % 8 + 1) * 128, row0 + m0:row0 + m0 + mw], st[:, 0:mw], [sk], ["QK"])
            for vb in range(8):
                wt, wk = wring()
                self.load_w(wt, wk, win[:, 2048 + vb * 512:2048 + (vb + 1) * 512], 512)
                for t in range(ntok // 128):
                    a_, ak = pa()
                    for k in range(16):
                        self.mm(a_, hT[:, k, t * 128:(t + 1) * 128], wt[:, k, :], k == 0, k == 15, ["hT", wk], [ak])
                    st, sk = str_()
                    self.copy("act" if t % 2 else "dve", st, a_, [ak], [sk])
                    dst = Vm if vb < 4 else Gm
                    self.dma("sp", dst[row0 + t * 128:row0 + (t + 1) * 128, (vb % 4) * 512:(vb % 4 + 1) * 512], st, [sk], ["VG"])
            for dr in range(2):
                t1, t1k = t1r()
                for m0 in range(0, ntok, 512):
                    mw = min(512, ntok - m0)
                    a_, ak = pz()
                    for k in range(16):
                        self.mm(a_[0:16, 0:mw], w1[dr][:, k, :], hT[:, k, m0:m0 + mw], k == 0, k == 15, ["gw1", "hT"], [ak])
                    self.copy("dve", t1[:, m0:m0 + mw], a_[0:16, 0:mw], [ak], [t1k])
                for t in range(ntok // 128):
                    for cb in range(2):
                        a_, ak = pa()
                        self.mm(a_, t1[:, t * 128:(t + 1) * 128], w2[dr][:, cb * 512:(cb + 1) * 512], True, True, [t1k, "gw2"], [ak])
                        z, zk = zr()
                        self.tt("dve", z, a_, bb[dr][:, cb * 512:(cb + 1) * 512], ALU.add, [ak, "gbb"], [zk])
                        self.act(z, z, AF.Exp, [zk], [zk], scale=-1.0)
                        sp, spk = spr()
                        self.act(sp, z, AF.Ln, [zk, "one"], [spk], bias=one[:, 0:1])
                        self.dma("sp", SP[dr, row0 + t * 128:row0 + (t + 1) * 128, cb * 512:(cb + 1) * 512], sp, [spk], ["SPd"])
        self.end_phase()
        ident = m.alloc(128, BF16)
        self.dma("sp", ident, self.din["ident"], (), ["ident"])
        onb = m.alloc(512, F32)
        self.dma("sp", onb, self.din["gla_onorm"][j, :].partition_broadcast(128), (), ["onb"])
        mask = [m.alloc(128, F32) for _ in range(2)]
        tri = [m.alloc(128, F32) for _ in range(2)]
        for dr in range(2):
            self.dma("sp", mask[dr], self.din["gmask"][dr], (), ["gmask"])
            self.dma("sp", tri[dr], self.din["gtri"][dr], (), ["gtri"])
        Sf = m.alloc((8, 512), F32)
        Sb = m.alloc((8, 512), BF16)
        qr = self.ring("sq", 2, (8, 128), BF16)
        kr = self.ring("sk", 2, (8, 128), BF16)
        vr = self.ring("sv", 2, D, BF16)
        gr = self.ring("sg", 2, D, BF16)
        spr2 = self.ring("ssp", 2, 1024, F32)
        ofr = self.ring("sof", 2, D, F32)
        pmr = self.ring("spm", 2, D, BF16)
        ebr = self.ring("seb", 4, 128, F32)
        enr = self.ring("sen", 4, 128, F32)
        qtr = self.ring("sqt", 4, (2, 128), BF16)
        ktr = self.ring("skt", 4, (2, 128), BF16)
        khr = self.ring("skh", 4, 128, BF16)
        khm = self.ring("skhm", 2, 256, BF16)
        atr = self.ring("sat", 2, 128, BF16)
        ostr = self.ring("sos", 2, 512, F32)
        onr = self.ring("son", 2, 512, F32)
        sgr = self.ring("ssg", 2, 512, F32)
        s4r = self.ring("ss4", 2, 2, F32)
        junk = m.alloc(512, BF16)
        pb = self.psring("spb", [0, 1])
        ptr_ = self.psring("sptr", [2], bf16=True)
        pA = self.psring("spA", [3])
        pO = self.psring("spO", [4, 5])
        pS = self.psring("spS", [6, 7])
        QTv = QT.rearrange("(c p) t -> p c t", p=128)
        KTv = KT.rearrange("(c p) t -> p c t", p=128)
        for dr in range(2):
            self.memset("pool", Sf, 0.0, ["Sf"])
            self.memset("pool", Sb, 0.0, ["Sb"])
            if dr == 0:
                order = list(range(NT))
            else:
                order = [1, 0] + list(range(NT - 1, 1, -1))
            endc = 127 if dr == 0 else 0
            def lds(tt_, dr=dr):
                rows = slice(tt_ * 128, (tt_ + 1) * 128)
                q_, qk = qr()
                k_, kk = kr()
                v_, vk = vr()
                sp_, spk = spr2()
                self.dma("sp", q_, QTv[:, 0:8, rows], ["QK"], [qk])
                self.dma("sp", k_, KTv[:, 0:8, rows], ["QK"], [kk])
                self.dma("sp", v_, Vm[rows, :], ["VG"], [vk])
                self.dma("sp", sp_, SP[dr, rows, :], ["SPd"], [spk])
                g_ = gk = None
                if dr == 1:
                    g_, gk = gr()
                    of_, ofk = ofr()
                    self.dma("sp", g_, Gm[rows, :], ["VG"], [gk])
                    self.dma("sp", of_, OF[rows, :], ["OFd"], [ofk])
                else:
                    of_, ofk = ofr()
                return (rows, q_, qk, k_, kk, v_, vk, sp_, spk, g_, gk, of_, ofk)
            nld = lds(order[0])
            for oi, tt_ in enumerate(order):
                (rows, q_, qk, k_, kk, v_, vk, sp_, spk, g_, gk, of_, ofk) = nld
                if oi + 1 < len(order):
                    nld = lds(order[oi + 1])
                if dr == 1:
                    pm_, pmk = pmr()
                for h in range(4):
                    qt, qtk = qtr()
                    kt, ktk = ktr()
                    kh, khk = khm()
                    ebs = []
                    for dc in range(2):
                        c = h * 2 + dc
                        b_, bk = pb()
                        self.mm(b_[:, 0:128], sp_[:, c * 128:(c + 1) * 128], tri[dr], True, True, [spk, "gtri"], [bk])
                        eb, ebk = ebr()
                        en, enk = enr()
                        self.act(eb, b_[:, 0:128], AF.Exp, [bk], [ebk])
                        self.act(en, b_[:, 0:128], AF.Exp, [bk], [enk], scale=-1.0)
                        self.tt("dve", qt[:, dc, :], q_[:, c, :], eb, ALU.mult, [qk, ebk], [qtk])
                        self.tt("pool", kt[:, dc, :], k_[:, c, :], en, ALU.mult, [kk, enk], [ktk])
                        khT, khTk = khr()
                        self.ts("dve", khT, kt[:, dc, :], eb[:, endc:endc + 1], None, ALU.mult, None, [ktk, ebk], [khTk])
                        p_, pk = ptr_()
                        self.tr(p_[:, 0:128], khT, ident, [khTk, "ident"], [pk])
                        self.copy("act", kh[:, dc * 128:(dc + 1) * 128], p_[:, 0:128], [pk], [khk])
                        ebs.append((eb, ebk))
                    a_, ak = pA()
                    for dc in range(2):
                        self.mm(a_[:, 0:128], kt[:, dc, :], qt[:, dc, :], dc == 0, dc == 1, [ktk, qtk], [ak])
                    at, atk = atr()
                    self.tt("dve", at, a_[:, 0:128], mask[dr], ALU.mult, [ak, "gmask"], [atk])
                    o_, ok = pO()
                    self.mm(o_, at, v_[:, h * 512:(h + 1) * 512], True, False, [atk, vk], [ok])
                    for dc in range(2):
                        self.mm(o_, qt[:, dc, :], Sb[:, h * 2 + dc, :], False, dc == 1, [qtk, "Sb"], [ok])
                    for dc in range(2):
                        c = h * 2 + dc
                        s_, sk = pS()
                        self.mm(s_, kh[:, dc * 128:(dc + 1) * 128], v_[:, h * 512:(h + 1) * 512], True, True, [khk, vk], [sk])
                        eb, ebk = ebs[dc]
                        self.stt(Sf[:, c, :], Sf[:, c, :], eb[:, endc:endc + 1], s_, ALU.mult, ALU.add, ["Sf", ebk, sk, ok], ["Sf"])
                        self.copy("act", Sb[:, c, :], Sf[:, c, :], ["Sf"], ["Sb"])
                    if dr == 0:
                        self.copy("act", of_[:, h * 512:(h + 1) * 512], o_, [ok], [ofk])
                    else:
                        os_, osk = ostr()
                        self.tt("dve", os_, o_, of_[:, h * 512:(h + 1) * 512], ALU.add, [ok, ofk], [osk])
                        s4, s4k = s4r()
                        self.act(junk, os_, AF.Square, [osk], ["gjunk", s4k], accum_out=s4[:, 0:1])
                        self.ts("dve", s4[:, 0:1], s4[:, 0:1], 1.0 / 512, EPS, ALU.mult, ALU.add, [s4k], [s4k])
                        self.rsqrt(s4[:, 0:1], s4k)
                        on, onk = onr()
                        self.stt(on, os_, s4[:, 0:1], onb, ALU.mult, ALU.mult, [osk, s4k, "onb"], [onk])
                        sg, sgk = sgr()
                        self.act(sg, g_[:, h * 512:(h + 1) * 512], AF.Silu, [gk], [sgk])
                        self.tt("pool", pm_[:, h * 512:(h + 1) * 512], on, sg, ALU.mult, [onk, sgk], [pmk])
                if dr == 0:
                    self.dma("sp", OF[rows, :], of_, [ofk], ["OFd"])
                else:
                    self.dma("sp", self.PM[rows, :], pm_, [pmk], ["PM"])
        self.end_phase()

    def phase_moe(self, i, ctx_on):
        cfg = self.cfg
        m = self.mem
        E = cfg.E
        NTOK = cfg.NTOK
        LAT = cfg.LAT
        aff = m.alloc(NTOK, F32, parts=E)
        r = self.norm_setup(i, "ffn", ctx_on)
        hT = m.alloc((16, 1024), BF16)
        rw = m.alloc((16, E), BF16)
        self.load_w(rw, "rw", self.din["router_w"][i], E)
        ones = m.alloc(E, BF16, parts=E)
        self.memset("pool", ones, 1.0, ["ones"])
        ex = self.ring("mex", 2, 512, BF16, parts=E)
        exf = self.ring("mexf", 2, 512, F32, parts=E)
        rc = self.ring("mrc", 2, 512, F32, parts=E)
        pring = self.psring("mp", [0, 1])
        pring2 = self.psring("mp2", [2, 3])
        for blk in self.cur_blocks:
            row0, ntok, v = blk
            self.norm_block(r, blk, hT, "hT", hm_store=True)
            for m0 in range(0, ntok, 512):
                mw = min(512, ntok - m0)
                pt, pk = pring()
                for k in range(16):
                    self.mm(pt[0:E, 0:mw], rw[:, k, :], hT[:, k, m0:m0 + mw], k == 0, k == 15, ["rw", "hT"], [pk])
                ef, efk = exf()
                self.act(ef[:, 0:mw], pt[0:E, 0:mw], AF.Exp, [pk], [efk])
                e_, ek = ex()
                self.copy("dve", e_[:, 0:mw], ef[:, 0:mw], [efk], [ek])
                lo, lk = ex()
                self.tt("dve", lo[:, 0:mw], ef[:, 0:mw], e_[:, 0:mw], ALU.subtract, [efk, ek], [lk])
                p2, pk2 = pring2()
                self.mm(p2[0:E, 0:mw], ones, e_[:, 0:mw], True, False, ["ones", ek], [pk2])
                self.mm(p2[0:E, 0:mw], ones, lo[:, 0:mw], False, True, ["ones", lk], [pk2])
                rr, rk = rc()
                self.S.op("dve", lambda e, rr=rr, p2=p2, mw=mw: e.reciprocal(out=rr[:, 0:mw], in_=p2[0:E, 0:mw]), [pk2], [rk])
                self.tt("dve", aff[:, row0 + m0:row0 + m0 + mw], ef[:, 0:mw], rr[:, 0:mw], ALU.mult, [efk, rk], ["aff"])
        self.barrier()
        segs = [(0, CTX, LAT, cfg.cap_l)]
        if ctx_on:
            segs.append((1, 0, CTX, cfg.cap_c))
        res = {}
        for (sid, c0, n, cap) in segs:
            vals = m.alloc(cap, F32, parts=E)
            idx = m.alloc(cap, U32, parts=E)
            src = aff[:, c0:c0 + n]
            vk, ik = "tv%d" % sid, "ti%d" % sid
            for rr_ in range(cap // 8):
                sl = slice(rr_ * 8, rr_ * 8 + 8)
                self.S.op("dve", lambda e, vals=vals, sl=sl, src=src: e.max(out=vals[:, sl], in_=src), ["aff"], [vk])
                self.S.op("dve", lambda e, vals=vals, idx=idx, sl=sl, src=src: e.max_index(out=idx[:, sl], in_max=vals[:, sl], in_values=src),
                          ["aff", vk], [ik])
                self.S.op("dve", lambda e, vals=vals, sl=sl, src=src: e.match_replace(out=src, in_to_replace=vals[:, sl], in_values=src, imm_value=-1.0),
                          [vk, ik], ["aff"])
            self.dma("sp", self.gate_d[sid, :, 0:cap], vals, [vk], ["gate_d"])
            self.dma("sp", self.idx_d[sid, :, 0:cap], idx, [ik], ["idx_d"])
            res[sid] = (c0, n, cap)
        self.end_phase()
        res2 = {}
        for sid in sorted(res):
            c0, n, cap = res[sid]
            gs = min(128, cap)
            ng = cap // gs
            idxT = m.alloc((E, ng), U32)
            gateT = m.alloc((E, ng), F32)
            for e_ in range(E):
                self.dma("sp", idxT[0:gs, e_, :], self.idx_d[sid, e_, 0:cap].rearrange("(g p) -> p g", p=gs), (), ["idxT%d" % sid],
                         allow_slow_non_contiguous=True)
                self.dma("sp", gateT[0:gs, e_, :], self.gate_d[sid, e_, 0:cap].rearrange("(g p) -> p g", p=gs), (), ["gateT%d" % sid],
                         allow_slow_non_contiguous=True)
            res2[sid] = (idxT, gateT, gs, ng, c0, n)
        res = res2
        groups = []
        slot = 0
        for sid in sorted(res):
            idxT, gateT, gs, ng, c0, n = res[sid]
            for g in range(ng):
                groups.append((sid, g, gs, slot))
                slot += gs
        NS = slot
        ident = m.alloc(128, BF16)
        self.dma("sp", ident, self.din["ident"], (), ["ident"])
        r = {"ident": ident, "ptr": self.psring("mtr", [6, 7], bf16=True)}
        g2 = {0: self.load_gate_bc(i, "ffn", 0, "g2L")}
        if ctx_on:
            g2[1] = self.load_gate_bc(i, "ffn", 1, "g2C")
        XT = m.alloc((16, NS), BF16)
        zT = m.alloc((12, NS), BF16)
        FB = 256
        wgr = self.ring("wg", 2, (16, FB), BF16)
        wur = self.ring("wu", 2, (16, FB), BF16)
        wd = m.alloc((12, D), BF16)
        xgr = self.ring("xg", 2, D, BF16)
        ysr = self.ring("ys", 2, D, F32)
        sar = self.ring("sa", 2, 512, F32)
        pA = self.psring("pA", [0, 1])
        pU = self.psring("pU", [2, 3])
        pY = self.psring("pY", [4, 5])
        mtiles = []
        nl = cfg.cap_l
        for s0 in range(0, nl, 512):
            mtiles.append((s0, min(512, nl - s0)))
        if ctx_on:
            mtiles.append((nl, NS - nl))
        pre = None
        for e in range(E):
            for (sid, g, gs, s0) in groups:
                idxT, gateT, _, _, c0, n = res[sid]
                xg, xk = xgr()
                srcrows = self.Hm
                ia = idxT[0:gs, e, g:g + 1]
                self.S.dma_custom("pool", lambda en, xg=xg, gs=gs, srcrows=srcrows, ia=ia, c0=c0: en.indirect_dma_start(
                    out=xg[0:gs, :], out_offset=None, in_=srcrows, in_offset=bass.IndirectOffsetOnAxis(ap=ia, axis=0),
                    element_offset=c0 * D),
                    ["Hm", "idxT%d" % sid], [xk])
                self.transpose_tile(r, xg, xk, gs, XT, "XT", s0)
            wdv = self.din["exp_w_down"][i, e].rearrange("(c p) n -> p c n", p=128)
            for fb in range(FF // FB):
                if fb == 0 and pre is not None:
                    wg, wgk, wu, wuk = pre
                    pre = None
                else:
                    wg, wgk = wgr()
                    wu, wuk = wur()
                    self.load_w(wg, wgk, self.din["exp_w_gate"][i, e][:, fb * FB:(fb + 1) * FB], FB)
                    self.load_w(wu, wuk, self.din["exp_w_up"][i, e][:, fb * FB:(fb + 1) * FB], FB)
                if fb == 1:
                    for c0_ in range(0, 12, 3):
                        self.S.dma("pool", wd[:, c0_:c0_ + 3, :], wdv[:, c0_:c0_ + 3, :], (), ["wd"])
                for c in range(FB // 128):
                    fc = fb * (FB // 128) + c
                    for (s0, w_) in mtiles:
                        a_, ak = pA()
                        u_, uk = pU()
                        for k in range(16):
                            self.mm(a_[:, 0:w_], wg[:, k, c * 128:(c + 1) * 128], XT[:, k, s0:s0 + w_], k == 0, k == 15, [wgk, "XT"], [ak])
                        for k in range(16):
                            self.mm(u_[:, 0:w_], wu[:, k, c * 128:(c + 1) * 128], XT[:, k, s0:s0 + w_], k == 0, k == 15, [wuk, "XT"], [uk])
                        sa, sk = sar()
                        self.act(sa[:, 0:w_], a_[:, 0:w_], AF.Silu, [ak], [sk])
                        self.tt("dve", zT[:, fc, s0:s0 + w_], sa[:, 0:w_], u_[:, 0:w_], ALU.mult, [sk, uk], ["zT"])
            if e + 1 < E:
                wg, wgk = wgr()
                wu, wuk = wur()
                self.load_w(wg, wgk, self.din["exp_w_gate"][i, e + 1][:, 0:FB], FB)
                self.load_w(wu, wuk, self.din["exp_w_up"][i, e + 1][:, 0:FB], FB)
                pre = (wg, wgk, wu, wuk)
            for (sid, g, gs, s0) in groups:
                idxT, gateT, _, _, c0, n = res[sid]
                gbc, gbk = g2[sid]
                ys, yk = ysr()
                for db in range(4):
                    y_, ypk = pY()
                    for c in range(12):
                        self.mm(y_[0:gs, :], zT[:, c, s0:s0 + gs], wd[:, c, db * 512:(db + 1) * 512], c == 0, c == 11, ["zT", "wd"], [ypk])
                    self.stt(ys[0:gs, db * 512:(db + 1) * 512], y_[0:gs, :], gateT[0:gs, e, g:g + 1], gbc[0:gs, db * 512:(db + 1) * 512],
                             ALU.mult, ALU.mult, [ypk, "gateT%d" % sid, gbk], [yk])
                dst = self.X
                ia = idxT[0:gs, e, g:g + 1]
                self.S.dma_custom("pool", lambda en, ys=ys, gs=gs, dst=dst, ia=ia, c0=c0: en.indirect_dma_start(
                    out=dst, out_offset=bass.IndirectOffsetOnAxis(ap=ia, axis=0), in_=ys[0:gs, :], in_offset=None, compute_op=ALU.add,
                    element_offset=c0 * D),
                    [yk, "idxT%d" % sid], ["X"])
        self.end_phase()

    def phase_final(self):
        m = self.mem
        g = m.alloc(D, F32)
        self.dma("sp", g, self.din["final_norm"][0, :].partition_broadcast(128), (), ["fg"])
        xring = self.ring("fx", 2, D, F32)
        oring = self.ring("fo", 2, D, F32)
        ss = self.ring("fss", 2, 1, F32)
        junk = m.alloc(D, BF16)
        nt_ = self.cfg.LAT // 128

        def ld(t):
            xs_, xk_ = xring()
            self.dma("sp", xs_, self.X[CTX + t * 128:CTX + (t + 1) * 128, :], ["X"], [xk_])
            return xs_, xk_
        nxt = ld(0)
        for t in range(nt_):
            xs, xk = nxt
            if t + 1 < nt_:
                nxt = ld(t + 1)
            s, sk = ss()
            self.act(junk, xs, AF.Square, [xk], ["fjunk", sk], accum_out=s)
            self.ts("dve", s, s, 1.0 / D, EPS, ALU.mult, ALU.add, [sk], [sk])
            self.rsqrt(s, sk)
            o, ok = oring()
            self.stt(o, xs, s[:, 0:1], g, ALU.mult, ALU.mult, [xk, sk, "fg"], [ok])
            self.dma("sp", self.out[t * 128:(t + 1) * 128, :], o, [ok], ["out"])


def prep_inputs(inp, cfg):
    f32 = np.float32
    d = {}
    d["xin"] = np.ascontiguousarray(inp["x"][0], dtype=f32)
    d["ctxin"] = np.ascontiguousarray(inp["ctx"][0], dtype=f32)
    cl = np.asarray(inp["c"][0], f32).reshape(16, 128).T
    cc = np.asarray(inp["c_ctx"], f32).reshape(16, 128).T
    d["ccols"] = np.ascontiguousarray(np.stack([cl, cc], axis=2).reshape(128, 32))
    for k in ("mod_w", "mod_b", "norm_mix", "norm_ffn", "router_w", "exp_w_gate", "exp_w_up", "exp_w_down", "conv_w_out"):
        d[k] = np.ascontiguousarray(inp[k], dtype=f32)
    d["final_norm"] = np.asarray(inp["final_norm"], f32).reshape(1, D)
    w = np.asarray(inp["conv_w_in"], f32)
    nA = w.shape[0]
    parts = [w[:, :, s * D:(s + 1) * D].reshape(nA, D, 16, 1, 128) for s in range(3)]
    d["conv_w_in_r"] = np.ascontiguousarray(np.concatenate(parts, axis=3).reshape(nA, D, 3 * D))
    dw = np.asarray(inp["conv_w_dw"], f32).reshape(nA, 3, 16, 128)
    d["conv_dw_cols"] = np.ascontiguousarray(dw.transpose(0, 3, 1, 2).reshape(nA, 128, 48))
    d["ident"] = np.eye(128, dtype=f32).astype(ml_dtypes.bfloat16)
    if cfg.nB:
        d["diff_w_qkv"] = np.ascontiguousarray(inp["diff_w_qkv"], dtype=f32)
        d["diff_lambda"] = np.asarray(inp["diff_lambda"], f32).reshape(cfg.nB, 1, 512)
        d["diff_subln"] = np.asarray(inp["diff_subln"], f32)
        d["diff_w_out"] = np.ascontiguousarray(inp["diff_w_out"], dtype=f32)
        n = np.arange(cfg.LAT)
        pos = [n // 64, n % 64]
        inv = 10000.0 ** (-np.arange(32, dtype=np.float64) / 32)
        C = np.ones((128, cfg.NTOK), np.float64)
        Sn = np.zeros((128, cfg.NTOK), np.float64)
        P = np.zeros((128, 128), np.float64)
        for dd in range(128):
            sct, within = dd // 64, dd % 64
            ang = pos[sct].astype(np.float64) * inv[within % 32]
            ang = (pos[sct].astype(np.float32) * inv[within % 32].astype(np.float32)).astype(np.float64)
            C[dd, CTX:] = np.cos(ang)
            Sn[dd, CTX:] = np.sin(ang)
            if within < 32:
                P[dd + 32, dd] = -1.0
            else:
                P[dd - 32, dd] = 1.0
        d["ropeC"] = C.astype(f32)
        d["ropeS"] = Sn.astype(f32)
        d["ropeP"] = P.astype(f32).astype(ml_dtypes.bfloat16)
    if cfg.nC:
        for k in ("gla_w_in", "gla_gate_w1", "gla_gate_w2", "gla_gate_b", "gla_onorm", "gla_w_out"):
            d[k] = np.ascontiguousarray(inp[k], dtype=f32)
        jj, ii = np.meshgrid(np.arange(128), np.arange(128), indexing="ij")
        mk = np.stack([(jj <= ii), (jj >= ii)]).astype(f32)
        d["gmask"] = mk
        d["gtri"] = (mk * (-1.0 / 16.0)).astype(f32)
    return d


def kernel(**inputs):
    cfg = Cfg()
    prog = Prog(cfg)
    nc = prog.build()
    d = prep_inputs(inputs, cfg)
    d = {k: v for k, v in d.items() if k in prog.din}
    res = run_bass_kernel_spmd(nc, [d], core_ids=[0])
    out = np.asarray(res.results[0]["out"], dtype=np.float32)
    return out.reshape(1, cfg.LAT, D)
```
